# Optimizing a Trainium2 kernel written in Bass

```python
import jax, jax.numpy as jnp
from jax import lax
import numpy as np

D_MODEL = 2048
BATCH = 4
SEQ = 4096
DEPTH = 1

MOBA_HEADS = 8
MOBA_HEAD_DIM = 128
MOBA_BLOCK = 256
MOBA_TOPK = 3
MOBA_Q_CHUNK = 32
SWA_HEADS = 16
SWA_KV_HEADS = 2
SWA_HEAD_DIM = 64
SWA_WINDOW = 128
D_FF = 5632
ROPE_THETA = 10000.0
EPS = 1e-6
N_ADA = 9

MOBA_W = MOBA_HEADS * MOBA_HEAD_DIM
SWA_QW = SWA_HEADS * SWA_HEAD_DIM
SWA_KVW = SWA_KV_HEADS * SWA_HEAD_DIM
IN_COLS = 3 * MOBA_W + SWA_QW + 2 * SWA_KVW + 2 * D_MODEL

kernel_name = "hybrid_moba_swa_sink_macaron_adaln"


def rms_norm(x, g):
    xf = x.astype(jnp.float32)
    y = xf * lax.rsqrt(jnp.mean(xf * xf, axis=-1, keepdims=True) + EPS)
    return (y * g.astype(jnp.float32)).astype(x.dtype)


def modulate(h, shift, scale):
    return h * (1.0 + scale[:, None, :]) + shift[:, None, :]


def rope(x, pos):
    half = x.shape[-1] // 2
    inv = ROPE_THETA ** (-jnp.arange(half, dtype=jnp.float32) / half)
    ang = pos.astype(jnp.float32)[:, None] * inv[None, :]
    cos = jnp.cos(ang)[None, :, None, :]
    sin = jnp.sin(ang)[None, :, None, :]
    x1 = x[..., :half].astype(jnp.float32)
    x2 = x[..., half:].astype(jnp.float32)
    return jnp.concatenate([x1 * cos - x2 * sin, x2 * cos + x1 * sin], axis=-1).astype(x.dtype)


def swiglu(h, w_gate, w_up, w_down):
    return jnp.einsum('bsf,fd->bsd', jax.nn.silu(jnp.einsum('bsd,df->bsf', h, w_gate)) * jnp.einsum('bsd,df->bsf', h, w_up), w_down)


def moba_attention(q, k, v):
    B, S, H, dh = q.shape
    BLK, C = MOBA_BLOCK, MOBA_Q_CHUNK
    nb = -(-S // BLK)
    Sp = nb * BLK
    K = min(MOBA_TOPK, nb)
    pad = ((0, 0), (0, Sp - S), (0, 0), (0, 0))
    qp, kp, vp = [jnp.pad(t, pad).transpose(0, 2, 1, 3) for t in (q, k, v)]
    kb = kp.reshape(B, H, nb, BLK, dh)
    vb = vp.reshape(B, H, nb, BLK, dh)
    kmean = jnp.mean(kb.astype(jnp.float32), axis=3).astype(q.dtype)
    n_chunks = Sp // C
    qc = qp.reshape(B, H, n_chunks, C, dh).transpose(2, 0, 1, 3, 4)
    scale = dh ** -0.5
    neg = jnp.finfo(jnp.float32).min
    b_ix = jnp.arange(B)[:, None, None, None]
    h_ix = jnp.arange(H)[None, :, None, None]

    def chunk_fn(args):
        ci, qi = args
        q_pos = ci * C + jnp.arange(C)
        own = (ci * C) // BLK
        gs = jnp.einsum('bhcd,bhnd->bhcn', qi, kmean, preferred_element_type=jnp.float32)
        gs = jnp.where(jnp.arange(nb) < own, gs, neg)
        _, sel = lax.top_k(gs, K)
        sel_valid = jnp.arange(K) < own
        ksel = kb[b_ix, h_ix, sel]
        vsel = vb[b_ix, h_ix, sel]
        s_sel = jnp.einsum('bhcd,bhckjd->bhckj', qi, ksel, preferred_element_type=jnp.float32) * scale
        s_sel = jnp.where(sel_valid[:, None], s_sel, neg).reshape(B, H, C, K * BLK)
        kown = lax.dynamic_index_in_dim(kb, own, axis=2, keepdims=False)
        vown = lax.dynamic_index_in_dim(vb, own, axis=2, keepdims=False)
        s_own = jnp.einsum('bhcd,bhjd->bhcj', qi, kown, preferred_element_type=jnp.float32) * scale
        k_pos = own * BLK + jnp.arange(BLK)
        s_own = jnp.where(k_pos[None, :] <= q_pos[:, None], s_own, neg)
        p = jax.nn.softmax(jnp.concatenate([s_sel, s_own], axis=-1), axis=-1)
        p_sel = p[..., :K * BLK].reshape(B, H, C, K, BLK).astype(vb.dtype)
        p_own = p[..., K * BLK:].astype(vb.dtype)
        out = jnp.einsum('bhckj,bhckjd->bhcd', p_sel, vsel) + jnp.einsum('bhcj,bhjd->bhcd', p_own, vown)
        return out.astype(q.dtype)

    outs = lax.map(chunk_fn, (jnp.arange(n_chunks), qc))
    return outs.transpose(1, 0, 3, 2, 4).reshape(B, Sp, H, dh)[:, :S]


def sliding_window_attention(q, k, v, sinks):
    B, S, Hq, dh = q.shape
    Hkv = k.shape[2]
    G = Hq // Hkv
    W = SWA_WINDOW
    nb = S // W
    qb = q.reshape(B, nb, W, Hkv, G, dh)
    kb = k.reshape(B, nb, W, Hkv, dh)
    vb = v.reshape(B, nb, W, Hkv, dh)
    shift = ((0, 0), (1, 0), (0, 0), (0, 0), (0, 0))
    kcat = jnp.concatenate([jnp.pad(kb, shift)[:, :-1], kb], axis=2)
    vcat = jnp.concatenate([jnp.pad(vb, shift)[:, :-1], vb], axis=2)
    s = jnp.einsum('bnqkgd,bnjkd->bnkgqj', qb, kcat, preferred_element_type=jnp.float32) * (dh ** -0.5)
    qrel = W + jnp.arange(W)[:, None]
    krel = jnp.arange(2 * W)[None, :]
    band = (krel <= qrel) & (krel > qrel - W)
    valid = band[None] & ((jnp.arange(nb)[:, None, None] > 0) | (krel[None] >= W))
    s = jnp.where(valid[None, :, None, None], s, jnp.finfo(jnp.float32).min)
    sink = jnp.broadcast_to(sinks.astype(jnp.float32).reshape(Hkv, G)[None, None, :, :, None, None], s.shape[:-1] + (1,))
    p = jax.nn.softmax(jnp.concatenate([s, sink], axis=-1), axis=-1)[..., :-1].astype(v.dtype)
    out = jnp.einsum('bnkgqj,bnjkd->bnqkgd', p, vcat)
    return out.reshape(B, S, Hq, dh)


def setup_inputs(seed: int = 0) -> dict:
    key = jax.random.key(seed)
    ks = jax.random.split(key, 24)
    f32 = jnp.float32

    def nrm(k, shape, fan_in):
        return jax.random.normal(k, shape, f32) * (fan_in ** -0.5)

    def gain(k):
        return 1.0 + 0.02 * jax.random.normal(k, (DEPTH, D_MODEL), f32)

    return {
        "x": jax.random.normal(ks[0], (BATCH, SEQ, D_MODEL), f32),
        "c": jax.random.normal(ks[1], (BATCH, D_MODEL), f32),
        "w_ada": nrm(ks[2], (DEPTH, D_MODEL, N_ADA * D_MODEL), D_MODEL),
        "b_ada": 0.01 * jax.random.normal(ks[3], (DEPTH, N_ADA * D_MODEL), f32),
        "norm_ffn1": gain(ks[4]),
        "ffn1_gate": nrm(ks[5], (DEPTH, D_MODEL, D_FF), D_MODEL),
        "ffn1_up": nrm(ks[6], (DEPTH, D_MODEL, D_FF), D_MODEL),
        "ffn1_down": nrm(ks[7], (DEPTH, D_FF, D_MODEL), D_FF),
        "norm_mix": gain(ks[8]),
        "w_in": nrm(ks[9], (DEPTH, D_MODEL, IN_COLS), D_MODEL),
        "swa_sinks": jax.random.normal(ks[10], (DEPTH, SWA_HEADS), f32),
        "w_branch_moba": nrm(ks[11], (DEPTH, MOBA_W, D_MODEL), MOBA_W),
        "w_branch_swa": nrm(ks[12], (DEPTH, SWA_QW, D_MODEL), SWA_QW),
        "w_out": nrm(ks[13], (DEPTH, D_MODEL, D_MODEL), D_MODEL),
        "norm_ffn2": gain(ks[14]),
        "ffn2_gate": nrm(ks[15], (DEPTH, D_MODEL, D_FF), D_MODEL),
        "ffn2_up": nrm(ks[16], (DEPTH, D_MODEL, D_FF), D_MODEL),
        "ffn2_down": nrm(ks[17], (DEPTH, D_FF, D_MODEL), D_FF),
        "norm_final": 1.0 + 0.02 * jax.random.normal(ks[18], (D_MODEL,), f32),
    }


def reference(x, c, w_ada, b_ada, norm_ffn1, ffn1_gate, ffn1_up, ffn1_down, norm_mix, w_in, swa_sinks,
              w_branch_moba, w_branch_swa, w_out, norm_ffn2, ffn2_gate, ffn2_up, ffn2_down, norm_final):
    B, S, _ = x.shape
    pos = jnp.arange(S)
    split_at = list(np.cumsum([MOBA_W, MOBA_W, MOBA_W, SWA_QW, SWA_KVW, SWA_KVW, D_MODEL]))
    for l in range(DEPTH):
        mod = jnp.einsum('bd,de->be', jax.nn.silu(c), w_ada[l]) + b_ada[l]
        (sh1, sc1, g1, sh2, sc2, g2, sh3, sc3, g3) = jnp.split(mod, N_ADA, axis=-1)

        h = modulate(rms_norm(x, norm_ffn1[l]), sh1, sc1)
        x = x + 0.5 * g1[:, None, :] * swiglu(h, ffn1_gate[l], ffn1_up[l], ffn1_down[l])

        h = modulate(rms_norm(x, norm_mix[l]), sh2, sc2)
        proj = jnp.einsum('bsd,de->bse', h, w_in[l])
        qa, ka, va, qb, kb, vb, ga, gb = jnp.split(proj, split_at, axis=-1)
        qa = rope(qa.reshape(B, S, MOBA_HEADS, MOBA_HEAD_DIM), pos)
        ka = rope(ka.reshape(B, S, MOBA_HEADS, MOBA_HEAD_DIM), pos)
        va = va.reshape(B, S, MOBA_HEADS, MOBA_HEAD_DIM)
        qb = rope(qb.reshape(B, S, SWA_HEADS, SWA_HEAD_DIM), pos)
        kb = rope(kb.reshape(B, S, SWA_KV_HEADS, SWA_HEAD_DIM), pos)
        vb = vb.reshape(B, S, SWA_KV_HEADS, SWA_HEAD_DIM)
        ya = moba_attention(qa, ka, va).reshape(B, S, MOBA_W)
        yb = sliding_window_attention(qb, kb, vb, swa_sinks[l]).reshape(B, S, SWA_QW)
        merged = (jax.nn.sigmoid(ga) * jnp.einsum('bse,ed->bsd', ya, w_branch_moba[l])
                  + jax.nn.sigmoid(gb) * jnp.einsum('bse,ed->bsd', yb, w_branch_swa[l]))
        x = x + g2[:, None, :] * jnp.einsum('bsd,de->bse', merged, w_out[l])

        h = modulate(rms_norm(x, norm_ffn2[l]), sh3, sc3)
        x = x + 0.5 * g3[:, None, :] * swiglu(h, ffn2_gate[l], ffn2_up[l], ffn2_down[l])
    return rms_norm(x, norm_final)
```

```python
import numpy as np
import ml_dtypes
import concourse.bass as bass
import concourse.mybir as mybir
from concourse.bass_utils import run_bass_kernel_spmd

F32 = mybir.dt.float32
BF16 = mybir.dt.bfloat16
ALU = mybir.AluOpType
AF = mybir.ActivationFunctionType
AX = mybir.AxisListType
NEG = -1.0e30


class Cfg:
    def __init__(s, D=2048, FF=5632, HA=8, HB=16, KVB=2, SEQ=4096, BATCH=4, TT=1024, FSPLIT=2):
        s.D, s.FF, s.HA, s.HB, s.KVB, s.SEQ, s.BATCH, s.TT, s.FSPLIT = D, FF, HA, HB, KVB, SEQ, BATCH, TT, FSPLIT
        s.T = SEQ // 2
        s.ND = D // 128
        s.NF = FF // 128
        s.BLK, s.W, s.TOPK = 256, 128, 3
        s.G = HB // KVB
        s.oqa = 0
        s.oka = HA * 128
        s.ova = 2 * HA * 128
        s.oqb = 3 * HA * 128
        s.okb = s.oqb + HB * 64
        s.ovb = s.okb + KVB * 64
        s.oga = s.ovb + KVB * 64
        s.ogb = s.oga + D
        s.INC = s.ogb + D
        s.NSUB = TT // 512
        s.NQT = s.T // 128
        s.NKB = 2 * s.T // 256
        s.NKT = 2 * s.T // 128
        assert KVB == 2 and s.T % TT == 0 and TT % 512 == 0 and s.NF % FSPLIT == 0


class Buf:
    __slots__ = ("n", "w", "r")

    def __init__(s, n=""):
        s.n = n
        s.w = {}
        s.r = {}


class Op:
    __slots__ = ("eng", "fn", "deps", "sig", "val", "dsem", "dval", "waits", "isdma")


class Rec:
    NDS = 24

    def __init__(s):
        s.ops = []
        s.last = {}
        s.bar = None
        s.bar_done = set()
        s.ndma = 0
        s.dma_last = {}

    def add(s, eng, fn, reads=(), writes=(), dma=False):
        op = Op()
        op.eng, op.fn, op.isdma, op.sig, op.deps = eng, fn, dma, False, set()
        op.val = op.dsem = op.dval = None
        for b in reads:
            for k, v in b.w.items():
                if k == 'dma':
                    op.deps.update(v)
                elif k == eng and not dma and eng == 'pe':
                    pass
                else:
                    op.deps.add(v)
        for b in writes:
            for k, v in b.r.items():
                if k == 'dma':
                    op.deps.update(v)
                elif k == eng and not dma:
                    pass
                else:
                    op.deps.add(v)
            for k, v in b.w.items():
                if k == 'dma':
                    op.deps.update(v)
                elif k == eng and not dma:
                    pass
                else:
                    op.deps.add(v)
        if s.bar is not None and eng not in s.bar_done:
            op.deps.update(s.bar)
            s.bar_done.add(eng)
        if dma:
            op.dsem = s.ndma % s.NDS
            op.dval = 16 * (s.ndma // s.NDS + 1)
            s.ndma += 1
            prev = s.dma_last.get(op.dsem)
            if prev is not None:
                op.deps.add(prev)
            s.dma_last[op.dsem] = op
            op.sig = True
        for b in reads:
            if dma:
                b.r.setdefault('dma', []).append(op)
            else:
                b.r[eng] = op
        for b in writes:
            if b.r:
                b.w = {}
                b.r = {}
            if dma:
                b.w.setdefault('dma', []).append(op)
            else:
                b.w[eng] = op
        s.last[eng] = op
        s.ops.append(op)
        return op

    def barrier(s):
        s.bar = set(s.last.values()) | set(s.dma_last.values())
        s.bar_done = set()

    def finish(s, nc):
        for op in s.ops:
            for d in op.deps:
                d.sig = True
        cnt = {}
        for op in s.ops:
            if op.isdma:
                continue
            if op.sig:
                cnt[op.eng] = cnt.get(op.eng, 0) + 1
                op.val = cnt[op.eng]
        engs = ['pe', 'act', 'dve', 'pool', 'sp']
        esem = {e: nc.alloc_semaphore("sem_" + e) for e in engs if e != 'sp'}
        dsem = [nc.alloc_semaphore("dsem%d" % i) for i in range(s.NDS)]
        per = {e: [] for e in engs}
        for op in s.ops:
            per[op.eng].append(op)
        for e in engs:
            waited = {}
            for op in per[e]:
                need = {}
                for d in op.deps:
                    if d.isdma:
                        k, v = ('d', d.dsem), d.dval
                    else:
                        k, v = ('e', d.eng), d.val
                    if v > need.get(k, 0):
                        need[k] = v
                op.waits = []
                for k, v in need.items():
                    if v > waited.get(k, 0):
                        waited[k] = v
                        op.waits.append((k, v))
        dfinal = {}
        for op in s.ops:
            if op.isdma:
                dfinal[op.dsem] = op.dval

        def body_for(e):
            def body(eng):
                for op in per[e]:
                    for (k, v) in op.waits:
                        eng.wait_ge(dsem[k[1]] if k[0] == 'd' else esem[k[1]], v)
                    ins = op.fn(eng)
                    if op.isdma:
                        ins.then_inc(dsem[op.dsem], 16)
                    elif op.sig:
                        ins.then_inc(esem[e], 1)
                if e == 'sp':
                    for k, v in dfinal.items():
                        eng.wait_ge(dsem[k], v)
            return body

        with nc.Block() as blk:
            blk.tensor(body_for('pe'))
            blk.scalar(body_for('act'))
            blk.vector(body_for('dve'))
            blk.gpsimd(body_for('pool'))
            blk.sync(body_for('sp'))


class Builder:
    def __init__(s, cfg):
        s.c = cfg
        s.nc = bass.Bass("TRN2", target_bir_lowering=False)
        s.R = Rec()
        s.dram = {}
        s.nbuf = 0
        s.cast_rr = 0

    def din(s, name, shape, dt=F32):
        t = s.nc.dram_tensor(name, list(shape), dt, kind="ExternalInput").ap()
        s.dram[name] = t
        return t

    def dscr(s, name, shape, dt):
        return s.nc.dram_tensor(name, list(shape), dt, kind="Internal").ap()

    def buf(s, n=""):
        return Buf(n)

    def mm(s, out, lhsT, rhs, start, stop, reads, writes):
        s.R.add('pe', lambda e: e.matmul(out, lhsT, rhs, start=start, stop=stop), reads, writes)

    def tr(s, out, in_, ident, reads, writes):
        s.R.add('pe', lambda e: e.transpose(out, in_, ident), reads, writes)

    def dma(s, out, in_, reads, writes):
        s.R.add('sp', lambda e: e.dma_start(out=out, in_=in_), reads, writes, dma=True)

    def act(s, out, in_, func, reads, writes, bias=None, scale=None, accum_out=None):
        kw = {}
        if bias is not None:
            kw['bias'] = bias
        if scale is not None:
            kw['scale'] = scale
        if accum_out is not None:
            kw['accum_out'] = accum_out
        s.R.add('act', lambda e: e.activation(out, in_, func, **kw), reads, writes)

    def tt(s, eng, out, in0, in1, op, reads, writes):
        s.R.add(eng, lambda e: e.tensor_tensor(out, in0, in1, op), reads, writes)

    def ts(s, eng, out, in0, s1, s2, op0, op1, reads, writes):
        if op1 is None:
            s.R.add(eng, lambda e: e.tensor_scalar(out, in0, s1, None, op0), reads, writes)
        else:
            s.R.add(eng, lambda e: e.tensor_scalar(out, in0, s1, s2, op0, op1), reads, writes)

    def stt(s, out, in0, scalar, in1, op0, op1, reads, writes):
        s.R.add('dve', lambda e: e.scalar_tensor_tensor(out, in0, scalar, in1, op0, op1), reads, writes)

    def cp(s, eng, out, in_, reads, writes):
        if eng == 'act':
            s.R.add('act', lambda e: e.activation(out, in_, AF.Copy), reads, writes)
        else:
            s.R.add(eng, lambda e: e.tensor_copy(out, in_), reads, writes)

    def memset(s, eng, ap, val, writes):
        s.R.add(eng, lambda e: e.memset(ap, val), (), writes)

    def init_psum(s):
        s.psb = [(s.nc.alloc_psum_tensor("psb%d" % i, [128, 512], F32), Buf("ps%d" % i)) for i in range(8)]
        s.psi = 0

    def ps(s):
        t = s.psb[s.psi % 8]
        s.psi += 1
        return t

    def init_arena(s, nbytes):
        s.arena = s.nc.alloc_sbuf_tensor("arena", [128, nbytes // 4], F32)
        s.arena_n = nbytes // 4
        s.aoff = 0

    def phase(s):
        s.R.barrier()
        s.aoff = 0

    def at(s, shape, dt):
        n = int(np.prod(shape))
        nw = (n * (2 if dt == BF16 else 4) + 3) // 4
        nw = (nw + 7) // 8 * 8
        assert s.aoff + nw <= s.arena_n, ("arena overflow", s.aoff, nw, s.arena_n)
        ap = s.arena[:, s.aoff:s.aoff + nw]
        s.aoff += nw
        if dt == BF16:
            ap = ap.bitcast(BF16)
        ap = ap[:, 0:n]
        if len(shape) == 2:
            ap = ap.rearrange("p (a b) -> p a b", a=shape[0])
        elif len(shape) == 3:
            ap = ap.rearrange("p (a b c) -> p a b c", a=shape[0], b=shape[1])
        return ap, Buf()

    def init_wstream(s, nslot=12, nstage=4):
        nc = s.nc
        s.wring = [(nc.alloc_sbuf_tensor("wr%d" % i, [128, 8, 256], BF16), Buf()) for i in range(nslot)]
        s.wstage = [(nc.alloc_sbuf_tensor("ws%d" % i, [128, 8, 256], F32), Buf()) for i in range(nstage)]
        s.wri = 0
        s.wsi = 0
        s.wring_owner = [None] * nslot
        s.wblk_id = 0

    def load_wblock(s, W, r0, nk, col0, ncols):
        units = []
        k = 0
        s.wblk_id += 1
        while k < nk:
            nku = min(8, nk - k)
            st, stb = s.wstage[s.wsi % len(s.wstage)]
            s.wsi += 1
            si = s.wri % len(s.wring)
            s.wri += 1
            assert s.wring_owner[si] is None, "weight ring too small for prefetch depth"
            s.wring_owner[si] = s.wblk_id
            sl, slb = s.wring[si]
            src = W[(r0 + k) * 128:(r0 + k + nku) * 128, col0:col0 + ncols].rearrange("(c p) n -> p c n", p=128)
            s.dma(st[:, 0:nku, 0:ncols], src, [], [stb])
            eng = ('pool', 'act', 'pool', 'dve')[s.cast_rr % 4]
            s.cast_rr += 1
            s.cp(eng, sl[:, 0:nku, 0:ncols], st[:, 0:nku, 0:ncols], [stb], [slb])
            units.append((sl, slb, nku, si))
            k += nku
        return units

    def wl(s, units, kc, c0, n=128):
        u = units[kc // 8]
        return u[0][:, kc % 8, c0:c0 + n], u[1]

    def release(s, units):
        for u in units:
            s.wring_owner[u[3]] = None


class WPipe:
    def __init__(s, B, specs, pf):
        s.B, s.specs, s.pf, s.loaded, s.nxt = B, specs, pf, {}, 0

    def get(s, i):
        while s.nxt < len(s.specs) and s.nxt <= i + s.pf:
            s.loaded[s.nxt] = s.B.load_wblock(*s.specs[s.nxt])
            s.nxt += 1
        return s.loaded[i]

    def done(s, i):
        s.B.release(s.loaded.pop(i))


def build(cfg):
    B = Builder(cfg)
    nc = B.nc
    c = cfg
    D, FF, ND, NF, T, TT, NSUB = c.D, c.FF, c.ND, c.NF, c.T, c.TT, c.NSUB
    HA, HB, KVB, G = c.HA, c.HB, c.KVB, c.G
    NQT, NKB, NKT = c.NQT, c.NKB, c.NKT
    T2 = 2 * T

    xT_own = B.din("xT_own", [D, T])
    xT_ctx = B.din("xT_ctx", [D, T])
    cT = B.din("cT", [128, ND])
    w_ada = B.din("w_ada", [D, 9 * D])
    b_adaT = B.din("b_adaT", [128, 9 * ND])
    gains = B.din("gains", [128, 4 * ND])
    f1g = B.din("f1g", [D, FF]); f1u = B.din("f1u", [D, FF]); f1d = B.din("f1d", [FF, D])
    w_in = B.din("w_in", [D, c.INC])
    sinks = B.din("sinks", [128, HB])
    w_bm = B.din("w_bm", [HA * 128, D]); w_bs = B.din("w_bs", [HB * 64, D]); w_out = B.din("w_out", [D, D])
    f2g = B.din("f2g", [D, FF]); f2u = B.din("f2u", [D, FF]); f2d = B.din("f2d", [FF, D])
    ropeA = B.din("ropeA", [2, 128, T2])
    ropeB = B.din("ropeB", [2, 128, T2])
    cmask = B.din("cmask", [128, 4 * 128])
    vbias = B.din("vbias", [128, 2 * NQT * NKB])
    outT = nc.dram_tensor("outT", [D, T], F32, kind="ExternalOutput").ap()

    S_x1 = B.dscr("S_x1", [D, T], F32); S_x1c = B.dscr("S_x1c", [D, T], F32)
    S_x2 = B.dscr("S_x2", [D, T], F32); S_x3 = B.dscr("S_x3", [D, T], F32)
    S_tmp = B.dscr("S_tmp", [D, T], F32)
    QA_d = B.dscr("QA_d", [HA * 128, T], BF16)
    KA_d = B.dscr("KA_d", [HA * 128, T2], BF16)
    VA_d = B.dscr("VA_d", [T2, HA * 128], BF16)
    QB_d = B.dscr("QB_d", [64, NQT, HB, 128], BF16)
    KB_d = B.dscr("KB_d", [64, KVB, T2], BF16)
    VB_d = B.dscr("VB_d", [T2, KVB * 64], BF16)
    SGA_d = B.dscr("SGA_d", [D, T], BF16); SGB_d = B.dscr("SGB_d", [D, T], BF16)
    YA_d = B.dscr("YA_d", [HA * 128, T], BF16); YB_d = B.dscr("YB_d", [HB * 64, T], BF16)
    db = {k: Buf(k) for k in ["x1", "x1c", "x2", "x3", "tmp", "qa", "ka", "va", "qb", "kb", "vb", "sga", "sgb", "ya", "yb", "out"]}

    B.init_psum()
    B.init_wstream(12, 4)
    consts = nc.alloc_sbuf_tensor("consts", [128, 4 * 128], F32); consts_b = Buf()
    ones_bf = nc.alloc_sbuf_tensor("ones_bf", [128, 128], BF16); ones_b = Buf()
    MOD = nc.alloc_sbuf_tensor("MOD", [128, 9 * ND], F32); MOD_b = Buf()
    GN = nc.alloc_sbuf_tensor("GN", [128, 4 * ND], F32); GN_b = Buf()
    AB = nc.alloc_sbuf_tensor("AB", [128, 3, 3, ND], F32); AB_b = Buf()
    VBI = nc.alloc_sbuf_tensor("VBI", [128, 2 * NQT * NKB], F32); VBI_b = Buf()
    ESK = nc.alloc_sbuf_tensor("ESK", [128, HB], F32); ESK_b = Buf()
    EPS = nc.alloc_sbuf_tensor("EPS", [128, 1], F32); EPS_b = Buf()
    B.init_arena(118 * 1024)

    TRI = consts[:, 0:128]; MASKP = consts[:, 128:256]; MASKP0 = consts[:, 256:384]; IDENT = consts[:, 384:512]

    B.dma(consts[:], cmask[:, :], [], [consts_b])
    B.dma(GN[:], gains[:, :], [], [GN_b])
    B.dma(VBI[:], vbias[:, :], [], [VBI_b])
    B.dma(ESK[:], sinks[:, :], [], [ESK_b])
    B.memset('dve', ones_bf[:], 1.0, [ones_b])
    B.memset('dve', EPS[:], 1e-6, [EPS_b])
    B.act(ESK[:], ESK[:], AF.Exp, [ESK_b], [ESK_b])

    B.phase()
    csb, csb_b = B.at([ND], F32)
    cbf, cbf_b = B.at([ND], BF16)
    bad, bad_b = B.at([9 * ND], F32)
    B.dma(csb, cT[:, :], [], [csb_b])
    B.dma(bad, b_adaT[:, :], [], [bad_b])
    B.act(cbf, csb, AF.Silu, [csb_b], [cbf_b])
    psm, psm_b = B.ps()
    specs = [(w_ada, 0, ND, cb * 256, 256) for cb in range(9 * D // 256)]
    wp = WPipe(B, specs, 2)
    for cb in range(len(specs)):
        u = wp.get(cb)
        for cc in range(2):
            j = cb * 2 + cc
            for kc in range(ND):
                l, lb = B.wl(u, kc, cc * 128)
                B.mm(psm[:, j:j + 1], l, cbf[:, kc:kc + 1], kc == 0, kc == ND - 1, [lb, cbf_b], [psm_b])
        wp.done(cb)
    B.tt('dve', MOD[:], psm[:, 0:9 * ND], bad, ALU.add, [psm_b, bad_b], [MOD_b])
    for i in range(3):
        sh = MOD[:, (3 * i) * ND:(3 * i + 1) * ND]
        sc = MOD[:, (3 * i + 1) * ND:(3 * i + 2) * ND]
        gg = MOD[:, (3 * i + 2) * ND:(3 * i + 3) * ND]
        B.stt(AB[:, i, 0, :], sc, 1.0, GN[:, i * ND:(i + 1) * ND], ALU.add, ALU.mult, [MOD_b, GN_b], [AB_b])
        B.cp('dve', AB[:, i, 1, :], sh, [MOD_b], [AB_b])
        B.ts('dve', AB[:, i, 2, :], gg, (1.0 if i == 1 else 0.5), None, ALU.mult, None, [MOD_b], [AB_b])

    def norm_apply(src, src_b, t0, hT, hT_b, Acol, Bcol, xr, sq, rstd, rstd_b, tmpf, out_dram=None, out_b=None):
        ssb = [B.ps() for _ in range(NSUB)]
        for dc in range(ND):
            xt, xb = xr[dc % len(xr)]
            B.dma(xt, src[dc * 128:(dc + 1) * 128, t0:t0 + TT], [src_b], [xb])
            st, sb_ = sq[dc % len(sq)]
            B.act(st, xt, AF.Square, [xb], [sb_])
            for su in range(NSUB):
                B.mm(ssb[su][0][:, :], ones_bf[:], st[:, su * 512:(su + 1) * 512], dc == 0, dc == ND - 1,
                     [ones_b, sb_], [ssb[su][1]])
        for su in range(NSUB):
            r = rstd[:, su * 512:(su + 1) * 512]
            B.act(r, ssb[su][0][:, :], AF.Sqrt, [ssb[su][1], EPS_b], [rstd_b], bias=EPS[:, 0:1], scale=1.0 / D)
            B.R.add('dve', lambda e, r=r: e.reciprocal(r, r), [rstd_b], [rstd_b])
        for dc in range(ND):
            xt, xb = xr[dc % len(xr)]
            B.dma(xt, src[dc * 128:(dc + 1) * 128, t0:t0 + TT], [src_b], [xb])
            for su in range(NSUB):
                tf, tfb = tmpf[(dc * NSUB + su) % len(tmpf)]
                sl = slice(su * 512, (su + 1) * 512)
                B.tt('dve', tf, xt[:, sl], rstd[:, sl], ALU.mult, [xb, rstd_b], [tfb])
                if out_dram is None:
                    B.act(hT[:, dc, sl], tf, AF.Identity, [tfb, AB_b], [hT_b[dc]],
                          bias=Bcol[:, dc:dc + 1], scale=Acol[:, dc:dc + 1])
                else:
                    B.act(tf, tf, AF.Identity, [tfb, GN_b], [tfb], scale=Acol[:, dc:dc + 1])
                    B.dma(out_dram[dc * 128:(dc + 1) * 128, t0 + su * 512:t0 + (su + 1) * 512], tf, [tfb], [out_b])

    def ffn_phase(src, src_b, dst, dst_b, ph, Wg, Wu, Wd, ntiles):
        B.phase()
        hT, _ = B.at([ND, TT], BF16)
        hT_b = [Buf() for _ in range(ND)]
        NFH = NF // c.FSPLIT
        aT, _ = B.at([NFH, TT], BF16)
        aT_b = [Buf() for _ in range(NFH)]
        xr = [B.at([TT], F32) for _ in range(4)]
        sq = [B.at([TT], BF16) for _ in range(2)]
        rstd, rstd_b = B.at([TT], F32)
        tmpf = [B.at([512], F32) for _ in range(4)]
        xo = [B.at([512], F32) for _ in range(4)]
        Acol, Bcol, Gcol = AB[:, ph, 0, :], AB[:, ph, 1, :], AB[:, ph, 2, :]
        for ti in range(ntiles):
            t0 = ti * TT
            norm_apply(src, src_b, t0, hT, hT_b, Acol, Bcol, xr, sq, rstd, rstd_b, tmpf)
            for fs in range(c.FSPLIT):
                specs = []
                for fb in range(NFH // 2):
                    col0 = (fs * NFH + fb * 2) * 128
                    specs.append((Wg, 0, ND, col0, 256))
                    specs.append((Wu, 0, ND, col0, 256))
                wp = WPipe(B, specs, 3)
                for fb in range(NFH // 2):
                    ug = wp.get(2 * fb)
                    uu = wp.get(2 * fb + 1)
                    for cc in range(2):
                        fl = fb * 2 + cc
                        for su in range(NSUB):
                            sl = slice(su * 512, (su + 1) * 512)
                            pg, pgb = B.ps()
                            pu, pub = B.ps()
                            for kc in range(ND):
                                l, lb = B.wl(ug, kc, cc * 128)
                                B.mm(pg[:, :], l, hT[:, kc, sl], kc == 0, kc == ND - 1, [lb, hT_b[kc]], [pgb])
                            for kc in range(ND):
                                l, lb = B.wl(uu, kc, cc * 128)
                                B.mm(pu[:, :], l, hT[:, kc, sl], kc == 0, kc == ND - 1, [lb, hT_b[kc]], [pub])
                            tf, tfb = tmpf[(fl * NSUB + su) % len(tmpf)]
                            B.act(tf, pg[:, :], AF.Silu, [pgb], [tfb])
                            B.tt('dve', aT[:, fl, sl], tf, pu[:, :], ALU.mult, [tfb, pub], [aT_b[fl]])
                    wp.done(2 * fb)
                    wp.done(2 * fb + 1)
                xin, xin_b = (src, src_b) if fs == 0 else (S_tmp, db["tmp"])
                xout, xout_b = (dst, dst_b) if fs == c.FSPLIT - 1 else (S_tmp, db["tmp"])
                specs = [(Wd, fs * NFH, NFH, db_ * 256, 256) for db_ in range(ND // 2)]
                wp = WPipe(B, specs, 1)
                for db_ in range(ND // 2):
                    u = wp.get(db_)
                    for cc in range(2):
                        dc = db_ * 2 + cc
                        for su in range(NSUB):
                            sl = slice(t0 + su * 512, t0 + (su + 1) * 512)
                            xt, xtb = xo[(dc * NSUB + su) % len(xo)]
                            B.dma(xt, xin[dc * 128:(dc + 1) * 128, sl], [xin_b], [xtb])
                            pd, pdb = B.ps()
                            for kc in range(NFH):
                                l, lb = B.wl(u, kc, cc * 128)
                                B.mm(pd[:, :], l, aT[:, kc, su * 512:(su + 1) * 512], kc == 0, kc == NFH - 1,
                                     [lb, aT_b[kc]], [pdb])
                            B.stt(xt, pd[:, :], Gcol[:, dc:dc + 1], xt, ALU.mult, ALU.add, [pdb, xtb, AB_b], [xtb])
                            B.dma(xout[dc * 128:(dc + 1) * 128, sl], xt, [xtb], [xout_b])
                    wp.done(db_)

    ffn_phase(xT_ctx, Buf(), S_x1c, db["x1c"], 0, f1g, f1u, f1d, T // TT)
    ffn_phase(xT_own, Buf(), S_x1, db["x1"], 0, f1g, f1u, f1d, T // TT)

    def proj_phase(src, src_b, is_ctx):
        B.phase()
        hT, _ = B.at([ND, TT], BF16)
        hT_b = [Buf() for _ in range(ND)]
        xr = [B.at([TT], F32) for _ in range(4)]
        sq = [B.at([TT], BF16) for _ in range(2)]
        rstd, rstd_b = B.at([TT], F32)
        tmpf = [B.at([512], F32) for _ in range(4)]
        rp = [B.at([2, 512], F32) for _ in range(4)]
        xs = [B.at([512], F32) for _ in range(3)]
        ra = [B.at([512], F32) for _ in range(3)]
        rb = [B.at([512], F32) for _ in range(3)]
        ev = [B.at([512], BF16) for _ in range(4)]
        evi = [0]
        Acol, Bcol = AB[:, 1, 0, :], AB[:, 1, 1, :]
        loc0 = 0 if is_ctx else T
        for ti in range(T // TT):
            t0 = ti * TT
            norm_apply(src, src_b, t0, hT, hT_b, Acol, Bcol, xr, sq, rstd, rstd_b, tmpf)
            blocks = []
            if not is_ctx:
                for i in range(HA // 2):
                    blocks.append(("qa", c.oqa + i * 256, 256, i * 2))
            for i in range(HA // 2):
                blocks.append(("ka", c.oka + i * 256, 256, i * 2))
            for i in range(HA // 2):
                blocks.append(("va", c.ova + i * 256, 256, i * 2))
            if not is_ctx:
                for i in range(HB * 64 // 256):
                    blocks.append(("qb", c.oqb + i * 256, 256, i * 2))
            blocks.append(("kb", c.okb, 128, 0))
            blocks.append(("vb", c.ovb, 128, 0))
            if not is_ctx:
                for i in range(ND // 2):
                    blocks.append(("ga", c.oga + i * 256, 256, i * 2))
                for i in range(ND // 2):
                    blocks.append(("gb", c.ogb + i * 256, 256, i * 2))
            wp = WPipe(B, [(w_in, 0, ND, b[1], b[2]) for b in blocks], 3)
            ropeslices = {}
            for su in range(NSUB):
                for which, tab in ((0, ropeA), (1, ropeB)):
                    r_, rb_ = rp[su * 2 + which] if NSUB * 2 <= len(rp) else rp[which]
                    lo = loc0 + t0 + su * 512
                    B.dma(r_, tab[:, :, lo:lo + 512].rearrange("a p n -> p a n"), [], [rb_])
                    ropeslices[(su, which)] = (r_, rb_)
            for bi, (kind, col0, ncols, ch0) in enumerate(blocks):
                u = wp.get(bi)
                if kind in ("va", "vb"):
                    for tk in range(TT // 128):
                        pv, pvb = B.ps()
                        for kc in range(ND):
                            r_, rb_ = B.wl(u, kc, 0, ncols)
                            B.mm(pv[:, 0:ncols], hT[:, kc, tk * 128:(tk + 1) * 128], r_, kc == 0, kc == ND - 1,
                                 [rb_, hT_b[kc]], [pvb])
                        e, eb = ev[evi[0] % len(ev)]
                        evi[0] += 1
                        B.cp('act', e[:, 0:ncols], pv[:, 0:ncols], [pvb], [eb])
                        tok = loc0 + t0 + tk * 128
                        if kind == "va":
                            B.dma(VA_d[tok:tok + 128, col0 - c.ova:col0 - c.ova + ncols], e[:, 0:ncols], [eb], [db["va"]])
                        else:
                            B.dma(VB_d[tok:tok + 128, 0:ncols], e[:, 0:ncols], [eb], [db["vb"]])
                    wp.done(bi)
                    continue
                for cc in range(ncols // 128):
                    ch = ch0 + cc
                    for su in range(NSUB):
                        sl = slice(su * 512, (su + 1) * 512)
                        pp, ppb = B.ps()
                        for kc in range(ND):
                            l, lb = B.wl(u, kc, cc * 128)
                            B.mm(pp[:, :], l, hT[:, kc, sl], kc == 0, kc == ND - 1, [lb, hT_b[kc]], [ppb])
                        tok = t0 + su * 512
                        if kind in ("ga", "gb"):
                            e, eb = ev[evi[0] % len(ev)]
                            evi[0] += 1
                            B.act(e, pp[:, :], AF.Sigmoid, [ppb], [eb])
                            dd, ddb = (SGA_d, db["sga"]) if kind == "ga" else (SGB_d, db["sgb"])
                            B.dma(dd[ch * 128:(ch + 1) * 128, tok:tok + 512], e, [eb], [ddb])
                            continue
                        isA = kind in ("qa", "ka")
                        rt, rtb = ropeslices[(su, 0 if isA else 1)]
                        x_, xb_ = xs[evi[0] % 3]
                        a_, ab_ = ra[evi[0] % 3]
                        b_, bb_ = rb[evi[0] % 3]
                        B.cp('act', x_, pp[:, :], [ppb], [xb_])
                        B.tt('pool', a_, x_, rt[:, 0, :], ALU.mult, [xb_, rtb], [ab_])
                        hs = 64 if isA else 32
                        for g0 in range(0, 128, 2 * hs):
                            B.tt('pool', b_[g0:g0 + hs, :], x_[g0 + hs:g0 + 2 * hs, :], rt[g0 + hs:g0 + 2 * hs, 1, :],
                                 ALU.mult, [xb_, rtb], [bb_])
                            B.tt('pool', b_[g0 + hs:g0 + 2 * hs, :], x_[g0:g0 + hs, :], rt[g0:g0 + hs, 1, :],
                                 ALU.mult, [xb_, rtb], [bb_])
                        e, eb = ev[evi[0] % len(ev)]
                        evi[0] += 1
                        if isA:
                            B.tt('dve', e, a_, b_, ALU.add, [ab_, bb_], [eb])
                            if kind == "qa":
                                B.dma(QA_d[ch * 128:(ch + 1) * 128, tok:tok + 512], e, [eb], [db["qa"]])
                            else:
                                B.dma(KA_d[ch * 128:(ch + 1) * 128, loc0 + tok:loc0 + tok + 512], e, [eb], [db["ka"]])
                        else:
                            B.tt('dve', e[0:64, 0:512], a_[0:64, :], b_[0:64, :], ALU.add, [ab_, bb_], [eb])
                            e2, eb2 = ev[evi[0] % len(ev)]
                            evi[0] += 1
                            B.tt('dve', e2[0:64, 0:512], a_[64:128, :], b_[64:128, :], ALU.add, [ab_, bb_], [eb2])
                            for hh, (ee, eeb) in enumerate(((e, eb), (e2, eb2))):
                                if kind == "qb":
                                    head = ch * 2 + hh
                                    B.dma(QB_d[:, tok // 128:tok // 128 + 4, head, :],
                                          ee[0:64, 0:512].rearrange("p (a b) -> p a b", a=4), [eeb], [db["qb"]])
                                else:
                                    B.dma(KB_d[:, hh, loc0 + tok:loc0 + tok + 512], ee[0:64, 0:512], [eeb], [db["kb"]])
                wp.done(bi)

    proj_phase(S_x1c, db["x1c"], True)
    proj_phase(S_x1, db["x1"], False)

    B.phase()
    scaleA = 128.0 ** -0.5
    KT = [B.at([T2], BF16) for _ in range(2)]
    V1 = [B.at([NKT, 132], BF16) for _ in range(2)]
    QT = [B.at([T], BF16) for _ in range(2)]
    kmf, kmf_b = B.at([NKB], F32)
    kmT, kmT_b = B.at([NKB], BF16)
    gsm = [B.at([NKB], F32) for _ in range(2)]
    top8 = [B.at([8], F32) for _ in range(2)]
    SEL, SEL_b = B.at([NQT, NKB], F32)
    Pt = [B.at([256], BF16) for _ in range(6)]
    acc = [B.at([132], F32) for _ in range(4)]
    rec = [B.at([1], F32) for _ in range(2)]
    yt = [B.at([128], F32) for _ in range(2)]
    ytT = [B.at([256], BF16) for _ in range(2)]
    for vv, vb_ in V1:
        B.memset('pool', vv[:, :, 128:129], 1.0, [vb_])
    pti = [0]
    VBb = VBI[:, 0:NQT * NKB].rearrange("p (a b) -> p a b", a=NQT)
    VBv = VBI[:, NQT * NKB:2 * NQT * NKB].rearrange("p (a b) -> p a b", a=NQT)

    def load_head(h):
        kt, ktb = KT[h % 2]
        v1, v1b = V1[h % 2]
        qt, qtb = QT[h % 2]
        B.dma(kt, KA_d[h * 128:(h + 1) * 128, :], [db["ka"]], [ktb])
        B.dma(v1[:, :, 0:128], VA_d[:, h * 128:(h + 1) * 128].rearrange("(n p) d -> p n d", p=128), [db["va"]], [v1b])
        B.dma(qt, QA_d[h * 128:(h + 1) * 128, :], [db["qa"]], [qtb])

    load_head(0)
    for h in range(HA):
        if h + 1 < HA:
            load_head(h + 1)
        kt, ktb = KT[h % 2]
        v1, v1b = V1[h % 2]
        qt, qtb = QT[h % 2]
        B.R.add('dve', lambda e, kt=kt: e.tensor_reduce(kmf, kt.rearrange("p (n k) -> p n k", k=256), AX.X, ALU.add),
                [ktb], [kmf_b])
        B.ts('dve', kmT, kmf, 1.0 / 256, None, ALU.mult, None, [kmf_b], [kmT_b])
        for q in range(NQT):
            pg, pgb = B.ps()
            B.mm(pg[:, 0:NKB], qt[:, q * 128:(q + 1) * 128], kmT, True, True, [qtb, kmT_b], [pgb])
            g_, gb_ = gsm[q % 2]
            t8, t8b = top8[q % 2]
            B.tt('dve', g_, pg[:, 0:NKB], VBb[:, q, :], ALU.add, [pgb, VBI_b], [gb_])
            B.R.add('dve', lambda e, t8=t8, g_=g_: e.max(t8, g_), [gb_], [t8b])
            B.ts('dve', g_, g_, t8[:, c.TOPK - 1:c.TOPK], None, ALU.is_ge, None, [gb_, t8b], [gb_])
            B.tt('dve', SEL[:, q, :], g_, VBv[:, q, :], ALU.mult, [gb_, VBI_b], [SEL_b])
        for j in range(T // 256):
            nb = T // 256 + j
            q0 = j * 256
            a0, a0b = acc[(2 * j) % 4]
            a1, a1b = acc[(2 * j + 1) % 4]
            accs = ((a0, a0b), (a1, a1b))
            k0 = 2 * nb
            s0, s0b = B.ps()
            B.mm(s0[:, 0:256], kt[:, k0 * 128:(k0 + 1) * 128], qt[:, q0:q0 + 256], True, True, [ktb, qtb], [s0b])
            p0, p0b = Pt[pti[0] % 6]; pti[0] += 1
            B.act(p0, s0[:, 0:256], AF.Exp, [s0b], [p0b], scale=scaleA)
            B.tt('pool', p0[:, 0:128], p0[:, 0:128], TRI, ALU.mult, [p0b, consts_b], [p0b])
            s1, s1b = B.ps()
            B.mm(s1[:, 0:128], kt[:, (k0 + 1) * 128:(k0 + 2) * 128], qt[:, q0 + 128:q0 + 256], True, True, [ktb, qtb], [s1b])
            p1, p1b = Pt[pti[0] % 6]; pti[0] += 1
            B.act(p1[:, 0:128], s1[:, 0:128], AF.Exp, [s1b], [p1b], scale=scaleA)
            B.tt('pool', p1[:, 0:128], p1[:, 0:128], TRI, ALU.mult, [p1b, consts_b], [p1b])
            o0, o0b = B.ps()
            B.mm(o0[:, 0:129], p0[:, 0:128], v1[:, k0, 0:129], True, True, [p0b, v1b], [o0b])
            B.cp('dve', a0[:, 0:129], o0[:, 0:129], [o0b], [a0b])
            o1, o1b = B.ps()
            B.mm(o1[:, 0:129], p0[:, 128:256], v1[:, k0, 0:129], True, False, [p0b, v1b], [o1b])
            B.mm(o1[:, 0:129], p1[:, 0:128], v1[:, k0 + 1, 0:129], False, True, [p1b, v1b], [o1b])
            B.cp('dve', a1[:, 0:129], o1[:, 0:129], [o1b], [a1b])
            for n in range(nb):
                pk = []
                for kk in range(2):
                    ktile = 2 * n + kk
                    sp_, spb = B.ps()
                    B.mm(sp_[:, 0:256], kt[:, ktile * 128:(ktile + 1) * 128], qt[:, q0:q0 + 256], True, True, [ktb, qtb], [spb])
                    pp_, ppb = Pt[pti[0] % 6]; pti[0] += 1
                    B.act(pp_, sp_[:, 0:256], AF.Exp, [spb], [ppb], scale=scaleA)
                    pk.append((pp_, ppb, ktile))
                for qs in range(2):
                    oo, oob = B.ps()
                    for kk in range(2):
                        pp_, ppb, ktile = pk[kk]
                        B.mm(oo[:, 0:129], pp_[:, qs * 128:(qs + 1) * 128], v1[:, ktile, 0:129], kk == 0, kk == 1, [ppb, v1b], [oob])
                    aa, aab = accs[qs]
                    B.stt(aa[:, 0:129], oo[:, 0:129], SEL[:, 2 * j + qs, n:n + 1], aa[:, 0:129], ALU.mult, ALU.add,
                          [oob, SEL_b, aab], [aab])
            yT, yTb = ytT[j % 2]
            for qs in range(2):
                aa, aab = accs[qs]
                r_, rb_ = rec[qs]
                y_, yb_ = yt[qs]
                B.R.add('dve', lambda e, r_=r_, aa=aa: e.reciprocal(r_, aa[:, 128:129]), [aab], [rb_])
                B.ts('dve', y_, aa[:, 0:128], r_[:, 0:1], None, ALU.mult, None, [aab, rb_], [yb_])
                pt_, ptb = B.ps()
                B.tr(pt_[:, 0:128], y_, IDENT, [yb_, consts_b], [ptb])
                B.cp('act', yT[:, qs * 128:(qs + 1) * 128], pt_[:, 0:128], [ptb], [yTb])
            B.dma(YA_d[h * 128:(h + 1) * 128, q0:q0 + 256], yT, [yTb], [db["ya"]])

    B.phase()
    scaleB = 64.0 ** -0.5
    KBT, KBT_b = B.at([KVB, T2], BF16)
    VB1, VB1_b = B.at([NKT, KVB, 66], BF16)
    QBT = [B.at([HB, 128], BF16) for _ in range(2)]
    Pb = [B.at([512], BF16) for _ in range(4)]
    den = [B.at([4], F32) for _ in range(2)]
    ybt = [B.at([HB * 64], F32) for _ in range(2)]
    ybT = [B.at([HB * 64 // 128, 128], BF16) for _ in range(2)]
    B.dma(KBT[0:64], KB_d[:, :, :], [db["kb"]], [KBT_b])
    B.memset('pool', VB1[:, :, :, 64:65], 1.0, [VB1_b])
    for g in range(KVB):
        B.dma(VB1[:, :, g, 0:64], VB_d[:, g * 64:(g + 1) * 64].rearrange("(n p) d -> p n d", p=128), [db["vb"]], [VB1_b])
    HG = min(4, G)
    pbi = [0]
    for i in range(NQT):
        qb_, qbb = QBT[i % 2]
        B.dma(qb_[0:64], QB_d[:, i, :, :], [db["qb"]], [qbb])
        cur = T // 128 + i
        prv = cur - 1
        y_, yb_ = ybt[i % 2]
        for g in range(KVB):
            for hg in range(G // HG):
                h0 = g * G + hg * HG
                rhs = qb_[0:64, h0:h0 + HG, :]
                ps_ = []
                for (ktile, msk) in ((prv, MASKP0 if i == 0 else MASKP), (cur, TRI)):
                    sp_, spb = B.ps()
                    B.mm(sp_[:, 0:HG * 128], KBT[0:64, g, ktile * 128:(ktile + 1) * 128], rhs, True, True, [KBT_b, qbb], [spb])
                    pp_, ppb = Pb[pbi[0] % 4]; pbi[0] += 1
                    B.act(pp_[:, 0:HG * 128], sp_[:, 0:HG * 128], AF.Exp, [spb], [ppb], scale=scaleB)
                    for hh in range(HG):
                        B.tt('pool', pp_[:, hh * 128:(hh + 1) * 128], pp_[:, hh * 128:(hh + 1) * 128], msk, ALU.mult,
                             [ppb, consts_b], [ppb])
                    ps_.append((pp_, ppb, ktile))
                oo, oob = B.ps()
                for hh in range(HG):
                    for kk in range(2):
                        pp_, ppb, ktile = ps_[kk]
                        B.mm(oo[:, hh * 65:hh * 65 + 65], pp_[:, hh * 128:(hh + 1) * 128], VB1[:, ktile, g, 0:65],
                             kk == 0, kk == 1, [ppb, VB1_b], [oob])
                d_, db_ = den[(g * (G // HG) + hg) % 2]
                ov = oo[:, 0:HG * 65].rearrange("p (h d) -> p h d", h=HG)
                B.tt('dve', d_[:, 0:HG], ov[:, :, 64], ESK[:, h0:h0 + HG], ALU.add, [oob, ESK_b], [db_])
                B.R.add('dve', lambda e, d_=d_: e.reciprocal(d_[:, 0:HG], d_[:, 0:HG]), [db_], [db_])
                for hh in range(HG):
                    B.ts('dve', y_[:, (h0 + hh) * 64:(h0 + hh + 1) * 64], ov[:, hh, 0:64], d_[:, hh:hh + 1], None,
                         ALU.mult, None, [oob, db_], [yb_])
        yT, yTb = ybT[i % 2]
        for ch in range(HB * 64 // 128):
            pt_, ptb = B.ps()
            B.tr(pt_[:, 0:128], y_[:, ch * 128:(ch + 1) * 128], IDENT, [yb_, consts_b], [ptb])
            B.cp('act', yT[:, ch, :], pt_[:, 0:128], [ptb], [yTb])
        B.dma(YB_d[:, i * 128:(i + 1) * 128].rearrange("(c p) n -> p c n", p=128), yT, [yTb], [db["yb"]])

    B.phase()
    NCA, NCB = HA, HB * 64 // 128
    yaT, yaT_b = B.at([NCA, TT], BF16)
    ybT2, ybT2_b = B.at([NCB, TT], BF16)
    mT, _ = B.at([ND, TT], BF16)
    mT_b = [Buf() for _ in range(ND)]
    sg = [B.at([2, 512], BF16) for _ in range(3)]
    tm = [B.at([2, 512], F32) for _ in range(3)]
    xo = [B.at([512], F32) for _ in range(4)]
    Gcol = AB[:, 1, 2, :]
    k_i = [0]
    for ti in range(T // TT):
        t0 = ti * TT
        B.dma(yaT, YA_d[:, t0:t0 + TT].rearrange("(c p) n -> p c n", p=128), [db["ya"]], [yaT_b])
        B.dma(ybT2, YB_d[:, t0:t0 + TT].rearrange("(c p) n -> p c n", p=128), [db["yb"]], [ybT2_b])
        specs = []
        for d2 in range(ND // 2):
            specs.append((w_bm, 0, NCA, d2 * 256, 256))
            specs.append((w_bs, 0, NCB, d2 * 256, 256))
        wp = WPipe(B, specs, 3)
        for d2 in range(ND // 2):
            um = wp.get(2 * d2)
            us = wp.get(2 * d2 + 1)
            for cc in range(2):
                dc = d2 * 2 + cc
                for su in range(NSUB):
                    sl = slice(su * 512, (su + 1) * 512)
                    tok = t0 + su * 512
                    s_, sb_ = sg[k_i[0] % 3]
                    t_, tb_ = tm[k_i[0] % 3]
                    k_i[0] += 1
                    B.dma(s_[:, 0, :], SGA_d[dc * 128:(dc + 1) * 128, tok:tok + 512], [db["sga"]], [sb_])
                    B.dma(s_[:, 1, :], SGB_d[dc * 128:(dc + 1) * 128, tok:tok + 512], [db["sgb"]], [sb_])
                    pm, pmb = B.ps()
                    pq, pqb = B.ps()
                    for kc in range(NCA):
                        l, lb = B.wl(um, kc, cc * 128)
                        B.mm(pm[:, :], l, yaT[:, kc, sl], kc == 0, kc == NCA - 1, [lb, yaT_b], [pmb])
                    for kc in range(NCB):
                        l, lb = B.wl(us, kc, cc * 128)
                        B.mm(pq[:, :], l, ybT2[:, kc, sl], kc == 0, kc == NCB - 1, [lb, ybT2_b], [pqb])
                    B.tt('dve', t_[:, 0, :], pm[:, :], s_[:, 0, :], ALU.mult, [pmb, sb_], [tb_])
                    B.tt('dve', t_[:, 1, :], pq[:, :], s_[:, 1, :], ALU.mult, [pqb, sb_], [tb_])
                    B.tt('pool', mT[:, dc, sl], t_[:, 0, :], t_[:, 1, :], ALU.add, [tb_], [mT_b[dc]])
            wp.done(2 * d2)
            wp.done(2 * d2 + 1)
        specs = [(w_out, 0, ND, d2 * 256, 256) for d2 in range(ND // 2)]
        wp = WPipe(B, specs, 2)
        for d2 in range(ND // 2):
            u = wp.get(d2)
            for cc in range(2):
                dc = d2 * 2 + cc
                for su in range(NSUB):
                    sl = slice(t0 + su * 512, t0 + (su + 1) * 512)
                    xt, xtb = xo[(dc * NSUB + su) % len(xo)]
                    B.dma(xt, S_x1[dc * 128:(dc + 1) * 128, sl], [db["x1"]], [xtb])
                    pd, pdb = B.ps()
                    for kc in range(ND):
                        l, lb = B.wl(u, kc, cc * 128)
                        B.mm(pd[:, :], l, mT[:, kc, su * 512:(su + 1) * 512], kc == 0, kc == ND - 1, [lb, mT_b[kc]], [pdb])
                    B.stt(xt, pd[:, :], Gcol[:, dc:dc + 1], xt, ALU.mult, ALU.add, [pdb, xtb, AB_b], [xtb])
                    B.dma(S_x2[dc * 128:(dc + 1) * 128, sl], xt, [xtb], [db["x2"]])
            wp.done(d2)

    ffn_phase(S_x2, db["x2"], S_x3, db["x3"], 2, f2g, f2u, f2d, T // TT)
    B.phase()
    xr = [B.at([TT], F32) for _ in range(4)]
    sq = [B.at([TT], BF16) for _ in range(2)]
    rstd, rstd_b = B.at([TT], F32)
    tmpf = [B.at([512], F32) for _ in range(4)]
    for ti in range(T // TT):
        norm_apply(S_x3, db["x3"], ti * TT, None, None, GN[:, 3 * ND:4 * ND], None, xr, sq, rstd, rstd_b, tmpf,
                   out_dram=outT, out_b=db["out"])
    B.R.finish(nc)
    return nc


def rope_tables(cfg, half):
    T = cfg.T
    pos_ctx = np.arange(0, T, dtype=np.float64)
    pos_own = np.arange(half * T, (half + 1) * T, dtype=np.float64)
    pos = np.concatenate([pos_ctx, pos_own])
    out = []
    for hs in (64, 32):
        inv = 10000.0 ** (-np.arange(hs, dtype=np.float64) / hs)
        ang = (pos.astype(np.float32)[None, :] * inv.astype(np.float32)[:, None]).astype(np.float32)
        cos = np.cos(ang).astype(np.float32)
        sin = np.sin(ang).astype(np.float32)
        reps = 128 // (2 * hs)
        cosT = np.concatenate([cos, cos] * reps, axis=0)
        sinT = np.concatenate([sin, -sin] * reps, axis=0)
        out.append(np.stack([cosT, sinT]).astype(np.float32))
    return out


def const_masks(cfg, half):
    k = np.arange(128)[:, None]
    q = np.arange(128)[None, :]
    tri = (k <= q).astype(np.float32)
    mp = (k > q).astype(np.float32)
    mp0 = mp * float(half)
    ident = np.eye(128, dtype=np.float32)
    cm = np.concatenate([tri, mp, mp0, ident], axis=1)
    NQT, NKB = cfg.NQT, cfg.NKB
    valid = np.zeros((NQT, NKB), np.float32)
    for qt in range(NQT):
        j = qt // 2
        for n in range(NKB):
            if n < NKB // 2:
                valid[qt, n] = float(half)
            elif n < NKB // 2 + j:
                valid[qt, n] = 1.0
    bias = np.where(valid > 0, 0.0, NEG).astype(np.float32)
    vb = np.concatenate([bias.reshape(-1), valid.reshape(-1)])[None, :].repeat(128, 0).astype(np.float32)
    return cm, vb


def colT(v, n):
    return np.ascontiguousarray(np.asarray(v, np.float32).reshape(n, 128).T)


def make_in_maps(cfg, inp):
    D, ND, T = cfg.D, cfg.ND, cfg.T
    f = lambda a: np.ascontiguousarray(np.asarray(a, np.float32))
    x = np.asarray(inp["x"], np.float32)
    shared = {
        "w_ada": f(inp["w_ada"][0]), "b_adaT": colT(inp["b_ada"][0], 9 * ND),
        "gains": np.concatenate([colT(inp["norm_ffn1"][0], ND), colT(inp["norm_mix"][0], ND),
                                 colT(inp["norm_ffn2"][0], ND), colT(inp["norm_final"], ND)], axis=1),
        "f1g": f(inp["ffn1_gate"][0]), "f1u": f(inp["ffn1_up"][0]), "f1d": f(inp["ffn1_down"][0]),
        "w_in": f(inp["w_in"][0]),
        "sinks": np.ascontiguousarray(np.broadcast_to(np.asarray(inp["swa_sinks"][0], np.float32)[None, :], (128, cfg.HB))),
        "w_bm": f(inp["w_branch_moba"][0]), "w_bs": f(inp["w_branch_swa"][0]), "w_out": f(inp["w_out"][0]),
        "f2g": f(inp["ffn2_gate"][0]), "f2u": f(inp["ffn2_up"][0]), "f2d": f(inp["ffn2_down"][0]),
    }
    maps = []
    for core in range(2 * cfg.BATCH):
        b, half = core // 2, core % 2
        rA, rB = rope_tables(cfg, half)
        cm, vb = const_masks(cfg, half)
        m = dict(shared)
        m["xT_own"] = np.ascontiguousarray(x[b, half * T:(half + 1) * T, :].T)
        m["xT_ctx"] = np.ascontiguousarray(x[b, 0:T, :].T)
        m["cT"] = colT(inp["c"][b], ND)
        m["ropeA"], m["ropeB"], m["cmask"], m["vbias"] = rA, rB, cm, vb
        maps.append(m)
    return maps


def assemble(cfg, results):
    out = np.empty((cfg.BATCH, cfg.SEQ, cfg.D), np.float32)
    for core, r in enumerate(results):
        b, half = core // 2, core % 2
        out[b, half * cfg.T:(half + 1) * cfg.T, :] = np.asarray(r["outT"], np.float32).T
    return out


def kernel(**inputs):
    cfg = Cfg()
    nc = build(cfg)
    maps = make_in_maps(cfg, inputs)
    res = run_bass_kernel_spmd(nc, maps, core_ids=list(range(8)))
    return assemble(cfg, res.results)
```

```python
import numpy as np
import ml_dtypes
import concourse.bass as bass
import concourse.mybir as mybir
from concourse.bass_utils import run_bass_kernel_spmd

F32 = mybir.dt.float32
BF16 = mybir.dt.bfloat16
ALU = mybir.AluOpType
AF = mybir.ActivationFunctionType
AX = mybir.AxisListType
NEG = -1.0e30


class Cfg:
    def __init__(s, D=2048, FF=5632, HA=8, HB=16, KVB=2, SEQ=4096, BATCH=4, TT=1024, FSPLIT=2):
        s.D, s.FF, s.HA, s.HB, s.KVB, s.SEQ, s.BATCH, s.TT, s.FSPLIT = D, FF, HA, HB, KVB, SEQ, BATCH, TT, FSPLIT
        s.T = SEQ // 2
        s.ND = D // 128
        s.NF = FF // 128
        s.BLK, s.W, s.TOPK = 256, 128, 3
        s.G = HB // KVB
        s.oqa = 0
        s.oka = HA * 128
        s.ova = 2 * HA * 128
        s.oqb = 3 * HA * 128
        s.okb = s.oqb + HB * 64
        s.ovb = s.okb + KVB * 64
        s.oga = s.ovb + KVB * 64
        s.ogb = s.oga + D
        s.INC = s.ogb + D
        s.NSUB = TT // 512
        s.NQT = s.T // 128
        s.NKB = 2 * s.T // 256
        s.NKT = 2 * s.T // 128
        assert KVB == 2 and s.T % TT == 0 and TT % 512 == 0 and s.NF % FSPLIT == 0


class Buf:
    __slots__ = ("n", "w", "r")

    def __init__(s, n=""):
        s.n = n
        s.w = {}
        s.r = {}


class Op:
    __slots__ = ("eng", "fn", "deps", "sig", "val", "dsem", "dval", "waits", "isdma")


class Rec:
    NDS = 24
    QPOOL = {'sp': (0, 16), 'act': (16, 8)}

    def __init__(s):
        s.ops = []
        s.last = {}
        s.bar = None
        s.bar_done = set()
        s.ndma = 0
        s.dma_last = {}
        s.qcnt = {}

    def add(s, eng, fn, reads=(), writes=(), dma=False):
        op = Op()
        op.eng, op.fn, op.isdma, op.sig, op.deps = eng, fn, dma, False, set()
        op.val = op.dsem = op.dval = None
        for b in reads:
            for k, v in b.w.items():
                if k == 'dma':
                    op.deps.update(v)
                elif k == eng and not dma and eng == 'pe':
                    pass
                else:
                    op.deps.add(v)
        for b in writes:
            for k, v in b.r.items():
                if k == 'dma':
                    op.deps.update(v)
                elif k == eng and not dma:
                    pass
                else:
                    op.deps.add(v)
            for k, v in b.w.items():
                if k == 'dma':
                    op.deps.update(v)
                elif k == eng and not dma:
                    pass
                else:
                    op.deps.add(v)
        if s.bar is not None and eng not in s.bar_done:
            op.deps.update(s.bar)
            s.bar_done.add(eng)
        if dma:
            base, cnt_ = s.QPOOL[eng]
            nq = s.qcnt.get(eng, 0)
            s.qcnt[eng] = nq + 1
            op.dsem = base + nq % cnt_
            op.dval = 16 * (nq // cnt_ + 1)
            s.ndma += 1
            prev = s.dma_last.get(op.dsem)
            if prev is not None:
                op.deps.add(prev)
            s.dma_last[op.dsem] = op
            op.sig = True
        for b in reads:
            if dma:
                b.r.setdefault('dma', []).append(op)
            else:
                b.r[eng] = op
        for b in writes:
            if b.r:
                b.w = {}
                b.r = {}
            if dma:
                b.w.setdefault('dma', []).append(op)
            else:
                b.w[eng] = op
        if not dma:
            s.last[eng] = op
        s.ops.append(op)
        return op

    def barrier(s):
        s.bar = set(s.last.values()) | set(s.dma_last.values())
        s.bar_done = set()

    def finish(s, nc):
        for op in s.ops:
            for d in op.deps:
                d.sig = True
        cnt = {}
        for op in s.ops:
            if op.isdma:
                continue
            if op.sig:
                cnt[op.eng] = cnt.get(op.eng, 0) + 1
                op.val = cnt[op.eng]
        engs = ['pe', 'act', 'dve', 'pool', 'sp']
        esem = {e: nc.alloc_semaphore("sem_" + e) for e in engs if e != 'sp'}
        dsem = [nc.alloc_semaphore("dsem%d" % i) for i in range(s.NDS)]
        per = {e: [] for e in engs}
        for op in s.ops:
            per[op.eng].append(op)
        for e in engs:
            waited = {}
            for op in per[e]:
                need = {}
                for d in op.deps:
                    if d.isdma:
                        k, v = ('d', d.dsem), d.dval
                    else:
                        k, v = ('e', d.eng), d.val
                    if v > need.get(k, 0):
                        need[k] = v
                op.waits = []
                for k, v in need.items():
                    if v > waited.get(k, 0):
                        waited[k] = v
                        op.waits.append((k, v))
        dfinal = {}
        for op in s.ops:
            if op.isdma:
                dfinal[op.dsem] = op.dval

        def body_for(e):
            def body(eng):
                for op in per[e]:
                    for (k, v) in op.waits:
                        eng.wait_ge(dsem[k[1]] if k[0] == 'd' else esem[k[1]], v)
                    ins = op.fn(eng)
                    if op.isdma:
                        ins.then_inc(dsem[op.dsem], 16)
                    elif op.sig:
                        ins.then_inc(esem[e], 1)
                if e == 'sp':
                    for k, v in dfinal.items():
                        eng.wait_ge(dsem[k], v)
                    for e2 in esem:
                        if cnt.get(e2, 0) > 0:
                            eng.wait_ge(esem[e2], cnt[e2])
            return body

        with nc.Block() as blk:
            blk.tensor(body_for('pe'))
            blk.scalar(body_for('act'))
            blk.vector(body_for('dve'))
            blk.gpsimd(body_for('pool'))
            blk.sync(body_for('sp'))


class Builder:
    def __init__(s, cfg):
        s.c = cfg
        s.nc = bass.Bass("TRN2", target_bir_lowering=False)
        s.R = Rec()
        s.dram = {}
        s.nbuf = 0
        s.cast_rr = 0

    def din(s, name, shape, dt=F32):
        t = s.nc.dram_tensor(name, list(shape), dt, kind="ExternalInput").ap()
        s.dram[name] = t
        return t

    def dscr(s, name, shape, dt):
        return s.nc.dram_tensor(name, list(shape), dt, kind="Internal").ap()

    def buf(s, n=""):
        return Buf(n)

    def mm(s, out, lhsT, rhs, start, stop, reads, writes):
        s.R.add('pe', lambda e: e.matmul(out, lhsT, rhs, start=start, stop=stop), reads, writes)

    def tr(s, out, in_, ident, reads, writes):
        s.R.add('pe', lambda e: e.transpose(out, in_, ident), reads, writes)

    def dma(s, out, in_, reads, writes, q='sp'):
        s.R.add(q, lambda e: e.dma_start(out=out, in_=in_), reads, writes, dma=True)

    def ld(s, out, in_, reads, writes):
        s.dma(out, in_, reads, writes, q='act')

    def act(s, out, in_, func, reads, writes, bias=None, scale=None, accum_out=None):
        kw = {}
        if bias is not None:
            kw['bias'] = bias
        if scale is not None:
            kw['scale'] = scale
        if accum_out is not None:
            kw['accum_out'] = accum_out
        s.R.add('act', lambda e: e.activation(out, in_, func, **kw), reads, writes)

    def tt(s, eng, out, in0, in1, op, reads, writes):
        s.R.add(eng, lambda e: e.tensor_tensor(out, in0, in1, op), reads, writes)

    def ts(s, eng, out, in0, s1, s2, op0, op1, reads, writes):
        if op1 is None:
            s.R.add(eng, lambda e: e.tensor_scalar(out, in0, s1, None, op0), reads, writes)
        else:
            s.R.add(eng, lambda e: e.tensor_scalar(out, in0, s1, s2, op0, op1), reads, writes)

    def stt(s, out, in0, scalar, in1, op0, op1, reads, writes):
        s.R.add('dve', lambda e: e.scalar_tensor_tensor(out, in0, scalar, in1, op0, op1), reads, writes)

    def cp(s, eng, out, in_, reads, writes):
        if eng == 'act':
            s.R.add('act', lambda e: e.activation(out, in_, AF.Copy), reads, writes)
        else:
            s.R.add(eng, lambda e: e.tensor_copy(out, in_), reads, writes)

    def memset(s, eng, ap, val, writes):
        s.R.add(eng, lambda e: e.memset(ap, val), (), writes)

    def init_psum(s):
        s.psb = [(s.nc.alloc_psum_tensor("psb%d" % i, [128, 512], F32), Buf("ps%d" % i)) for i in range(8)]
        s.psi = 0

    def ps(s):
        t = s.psb[s.psi % 8]
        s.psi += 1
        return t

    def init_arena(s, nbytes):
        s.arena = s.nc.alloc_sbuf_tensor("arena", [128, nbytes // 4], F32)
        s.arena_n = nbytes // 4
        s.aoff = 0

    def phase(s):
        s.R.barrier()
        s.aoff = 0

    def at(s, shape, dt):
        n = int(np.prod(shape))
        nw = (n * (2 if dt == BF16 else 4) + 3) // 4
        nw = (nw + 7) // 8 * 8
        assert s.aoff + nw <= s.arena_n, ("arena overflow", s.aoff, nw, s.arena_n)
        ap = s.arena[:, s.aoff:s.aoff + nw]
        s.aoff += nw
        if dt == BF16:
            ap = ap.bitcast(BF16)
        ap = ap[:, 0:n]
        if len(shape) == 2:
            ap = ap.rearrange("p (a b) -> p a b", a=shape[0])
        elif len(shape) == 3:
            ap = ap.rearrange("p (a b c) -> p a b c", a=shape[0], b=shape[1])
        return ap, Buf()

    def init_wstream(s, nslot=12, nstage=4):
        nc = s.nc
        s.wring = [(nc.alloc_sbuf_tensor("wr%d" % i, [128, 8, 256], BF16), Buf()) for i in range(nslot)]
        s.wstage = [(nc.alloc_sbuf_tensor("ws%d" % i, [128, 8, 256], F32), Buf()) for i in range(nstage)]
        s.wri = 0
        s.wsi = 0
        s.wring_owner = [None] * nslot
        s.wblk_id = 0

    def wl(s, units, kc, c0, n=128):
        u = units[kc // 8]
        return u[0][:, kc % 8, c0:c0 + n], u[1]


class WPipe:
    AHEAD = 2

    def __init__(s, B, specs):
        s.B, s.specs = B, specs
        s.units, s.bunits = [], []
        for bi, (W, r0, nk, col0, ncols) in enumerate(specs):
            k, us = 0, []
            while k < nk:
                nku = min(8, nk - k)
                us.append(len(s.units))
                s.units.append((bi, W, r0 + k, nku, col0, ncols))
                k += nku
            s.bunits.append(us)
        s.nd = s.ncs = 0
        s.st, s.rg = {}, {}

    def pump(s, i):
        B = s.B
        NST = len(B.wstage)
        while True:
            if s.nd < len(s.units) and s.nd < s.ncs + NST:
                bi, W, r, nku, col0, ncols = s.units[s.nd]
                st, stb = B.wstage[B.wsi % NST]
                B.wsi += 1
                src = W[r * 128:(r + nku) * 128, col0:col0 + ncols].rearrange("(c p) n -> p c n", p=128)
                B.dma(st[:, 0:nku, 0:ncols], src, [], [stb])
                s.st[s.nd] = (st, stb)
                s.nd += 1
                continue
            if s.ncs < s.nd and s.units[s.ncs][0] <= i + s.AHEAD:
                si = B.wri % len(B.wring)
                if B.wring_owner[si] is None:
                    bi, W, r, nku, col0, ncols = s.units[s.ncs]
                    B.wri += 1
                    B.wring_owner[si] = 1
                    sl, slb = B.wring[si]
                    st, stb = s.st.pop(s.ncs)
                    B.cp('act', sl[:, 0:nku, 0:ncols], st[:, 0:nku, 0:ncols], [stb], [slb])
                    s.rg[s.ncs] = (sl, slb, nku, si)
                    s.ncs += 1
                    continue
            break

    def get(s, i):
        s.pump(i)
        us = s.bunits[i]
        assert all(u in s.rg for u in us), "weight ring stalled"
        return [s.rg[u] for u in us]

    def done(s, i):
        for u in s.bunits[i]:
            s.B.wring_owner[s.rg.pop(u)[3]] = None


def build(cfg):
    B = Builder(cfg)
    nc = B.nc
    c = cfg
    D, FF, ND, NF, T, TT, NSUB = c.D, c.FF, c.ND, c.NF, c.T, c.TT, c.NSUB
    HA, HB, KVB, G = c.HA, c.HB, c.KVB, c.G
    NQT, NKB, NKT = c.NQT, c.NKB, c.NKT
    T2 = 2 * T

    xT_own = B.din("xT_own", [D, T])
    xT_ctx = B.din("xT_ctx", [D, T])
    cT = B.din("cT", [128, ND])
    w_ada = B.din("w_ada", [D, 9 * D])
    b_adaT = B.din("b_adaT", [128, 9 * ND])
    gains = B.din("gains", [128, 4 * ND])
    f1g = B.din("f1g", [D, FF]); f1u = B.din("f1u", [D, FF]); f1d = B.din("f1d", [FF, D])
    w_in = B.din("w_in", [D, c.INC])
    sinks = B.din("sinks", [128, HB])
    w_bm = B.din("w_bm", [HA * 128, D]); w_bs = B.din("w_bs", [HB * 64, D]); w_out = B.din("w_out", [D, D])
    f2g = B.din("f2g", [D, FF]); f2u = B.din("f2u", [D, FF]); f2d = B.din("f2d", [FF, D])
    ropeA = B.din("ropeA", [2, 128, T2])
    ropeB = B.din("ropeB", [2, 128, T2])
    cmask = B.din("cmask", [128, 4 * 128])
    vbias = B.din("vbias", [128, 2 * NQT * NKB])
    outT = nc.dram_tensor("outT", [D, T], F32, kind="ExternalOutput").ap()

    S_x1 = B.dscr("S_x1", [D, T], F32); S_x1c = B.dscr("S_x1c", [D, T], F32)
    S_x2 = B.dscr("S_x2", [D, T], F32); S_x3 = B.dscr("S_x3", [D, T], F32)
    S_tmp = B.dscr("S_tmp", [D, T], F32)
    QA_d = B.dscr("QA_d", [HA * 128, T], BF16)
    KA_d = B.dscr("KA_d", [HA * 128, T2], BF16)
    VA_d = B.dscr("VA_d", [T2, HA * 128], BF16)
    QB_d = B.dscr("QB_d", [64, NQT, HB, 128], BF16)
    KB_d = B.dscr("KB_d", [64, KVB, T2], BF16)
    VB_d = B.dscr("VB_d", [T2, KVB * 64], BF16)
    SGA_d = B.dscr("SGA_d", [D, T], BF16); SGB_d = B.dscr("SGB_d", [D, T], BF16)
    YA_d = B.dscr("YA_d", [HA * 128, T], BF16); YB_d = B.dscr("YB_d", [HB * 64, T], BF16)
    db = {k: Buf(k) for k in ["x1", "x1c", "x2", "x3", "tmp", "qa", "ka", "va", "qb", "kb", "vb", "sga", "sgb", "ya", "yb", "out"]}

    B.init_psum()
    B.init_wstream(12, 4)
    consts = nc.alloc_sbuf_tensor("consts", [128, 4 * 128], F32); consts_b = Buf()
    ones_bf = nc.alloc_sbuf_tensor("ones_bf", [128, 128], BF16); ones_b = Buf()
    MOD = nc.alloc_sbuf_tensor("MOD", [128, 9 * ND], F32); MOD_b = Buf()
    GN = nc.alloc_sbuf_tensor("GN", [128, 4 * ND], F32); GN_b = Buf()
    AB = nc.alloc_sbuf_tensor("AB", [128, 3, 3, ND], F32); AB_b = Buf()
    VBI = nc.alloc_sbuf_tensor("VBI", [128, 2 * NQT * NKB], F32); VBI_b = Buf()
    ESK = nc.alloc_sbuf_tensor("ESK", [128, HB], F32); ESK_b = Buf()
    EPS = nc.alloc_sbuf_tensor("EPS", [128, 1], F32); EPS_b = Buf()
    B.init_arena(118 * 1024)

    TRI = consts[:, 0:128]; MASKP = consts[:, 128:256]; MASKP0 = consts[:, 256:384]; IDENT = consts[:, 384:512]

    B.dma(consts[:], cmask[:, :], [], [consts_b])
    B.dma(GN[:], gains[:, :], [], [GN_b])
    B.dma(VBI[:], vbias[:, :], [], [VBI_b])
    B.dma(ESK[:], sinks[:, :], [], [ESK_b])
    B.memset('dve', ones_bf[:], 1.0, [ones_b])
    B.memset('dve', EPS[:], 1e-6, [EPS_b])
    B.act(ESK[:], ESK[:], AF.Exp, [ESK_b], [ESK_b])

    B.phase()
    csb, csb_b = B.at([ND], F32)
    cbf, cbf_b = B.at([ND], BF16)
    bad, bad_b = B.at([9 * ND], F32)
    B.dma(csb, cT[:, :], [], [csb_b])
    B.dma(bad, b_adaT[:, :], [], [bad_b])
    B.act(cbf, csb, AF.Silu, [csb_b], [cbf_b])
    psm, psm_b = B.ps()
    specs = [(w_ada, 0, ND, cb * 256, 256) for cb in range(9 * D // 256)]
    wp = WPipe(B, specs)
    for cb in range(len(specs)):
        u = wp.get(cb)
        for cc in range(2):
            j = cb * 2 + cc
            for kc in range(ND):
                l, lb = B.wl(u, kc, cc * 128)
                B.mm(psm[:, j:j + 1], l, cbf[:, kc:kc + 1], kc == 0, kc == ND - 1, [lb, cbf_b], [psm_b])
        wp.done(cb)
    B.tt('dve', MOD[:], psm[:, 0:9 * ND], bad, ALU.add, [psm_b, bad_b], [MOD_b])
    for i in range(3):
        sh = MOD[:, (3 * i) * ND:(3 * i + 1) * ND]
        sc = MOD[:, (3 * i + 1) * ND:(3 * i + 2) * ND]
        gg = MOD[:, (3 * i + 2) * ND:(3 * i + 3) * ND]
        B.stt(AB[:, i, 0, :], sc, 1.0, GN[:, i * ND:(i + 1) * ND], ALU.add, ALU.mult, [MOD_b, GN_b], [AB_b])
        B.cp('dve', AB[:, i, 1, :], sh, [MOD_b], [AB_b])
        B.ts('dve', AB[:, i, 2, :], gg, (1.0 if i == 1 else 0.5), None, ALU.mult, None, [MOD_b], [AB_b])

    def norm_apply(src, src_b, t0, hT, hT_b, Acol, Bcol, xr, sq, rstd, rstd_b, tmpf, out_dram=None, out_b=None):
        ssb = [B.ps() for _ in range(NSUB)]
        for dc in range(ND):
            xt, xb = xr[dc % len(xr)]
            B.ld(xt, src[dc * 128:(dc + 1) * 128, t0:t0 + TT], [src_b], [xb])
            st, sb_ = sq[dc % len(sq)]
            B.act(st, xt, AF.Square, [xb], [sb_])
            for su in range(NSUB):
                B.mm(ssb[su][0][:, :], ones_bf[:], st[:, su * 512:(su + 1) * 512], dc == 0, dc == ND - 1,
                     [ones_b, sb_], [ssb[su][1]])
        for su in range(NSUB):
            r = rstd[:, su * 512:(su + 1) * 512]
            B.act(r, ssb[su][0][:, :], AF.Sqrt, [ssb[su][1], EPS_b], [rstd_b], bias=EPS[:, 0:1], scale=1.0 / D)
            B.R.add('dve', lambda e, r=r: e.reciprocal(r, r), [rstd_b], [rstd_b])
        for dc in range(ND):
            xt, xb = xr[dc % len(xr)]
            B.ld(xt, src[dc * 128:(dc + 1) * 128, t0:t0 + TT], [src_b], [xb])
            for su in range(NSUB):
                tf, tfb = tmpf[(dc * NSUB + su) % len(tmpf)]
                sl = slice(su * 512, (su + 1) * 512)
                B.tt('dve', tf, xt[:, sl], rstd[:, sl], ALU.mult, [xb, rstd_b], [tfb])
                if out_dram is None:
                    B.act(hT[:, dc, sl], tf, AF.Identity, [tfb, AB_b], [hT_b[dc]],
                          bias=Bcol[:, dc:dc + 1], scale=Acol[:, dc:dc + 1])
                else:
                    B.act(tf, tf, AF.Identity, [tfb, GN_b], [tfb], scale=Acol[:, dc:dc + 1])
                    B.dma(out_dram[dc * 128:(dc + 1) * 128, t0 + su * 512:t0 + (su + 1) * 512], tf, [tfb], [out_b])

    def ffn_phase(src, src_b, dst, dst_b, ph, Wg, Wu, Wd, ntiles):
        B.phase()
        hT, _ = B.at([ND, TT], BF16)
        hT_b = [Buf() for _ in range(ND)]
        NFH = NF // c.FSPLIT
        aT, _ = B.at([NFH, TT], BF16)
        aT_b = [Buf() for _ in range(NFH)]
        xr = [B.at([TT], F32) for _ in range(4)]
        sq = [B.at([TT], BF16) for _ in range(2)]
        rstd, rstd_b = B.at([TT], F32)
        tmpf = [B.at([512], F32) for _ in range(4)]
        xo = [B.at([512], F32) for _ in range(4)]
        Acol, Bcol, Gcol = AB[:, ph, 0, :], AB[:, ph, 1, :], AB[:, ph, 2, :]
        specs, gu_i, dn_i = [], {}, {}
        for ti in range(ntiles):
            for fs in range(c.FSPLIT):
                for fb in range(NFH // 2):
                    col0 = (fs * NFH + fb * 2) * 128
                    gu_i[(ti, fs, fb)] = len(specs)
                    specs.append((Wg, 0, ND, col0, 256))
                    specs.append((Wu, 0, ND, col0, 256))
                for db_ in range(ND // 2):
                    dn_i[(ti, fs, db_)] = len(specs)
                    specs.append((Wd, fs * NFH, NFH, db_ * 256, 256))
        wp = WPipe(B, specs)
        wp.pump(0)
        for ti in range(ntiles):
            t0 = ti * TT
            norm_apply(src, src_b, t0, hT, hT_b, Acol, Bcol, xr, sq, rstd, rstd_b, tmpf)
            for fs in range(c.FSPLIT):
                for fb in range(NFH // 2):
                    bi = gu_i[(ti, fs, fb)]
                    ug = wp.get(bi)
                    uu = wp.get(bi + 1)
                    for cc in range(2):
                        fl = fb * 2 + cc
                        for su in range(NSUB):
                            sl = slice(su * 512, (su + 1) * 512)
                            pg, pgb = B.ps()
                            pu, pub = B.ps()
                            for kc in range(ND):
                                l, lb = B.wl(ug, kc, cc * 128)
                                B.mm(pg[:, :], l, hT[:, kc, sl], kc == 0, kc == ND - 1, [lb, hT_b[kc]], [pgb])
                            for kc in range(ND):
                                l, lb = B.wl(uu, kc, cc * 128)
                                B.mm(pu[:, :], l, hT[:, kc, sl], kc == 0, kc == ND - 1, [lb, hT_b[kc]], [pub])
                            tf, tfb = tmpf[(fl * NSUB + su) % len(tmpf)]
                            B.act(tf, pg[:, :], AF.Silu, [pgb], [tfb])
                            B.tt('dve', aT[:, fl, sl], tf, pu[:, :], ALU.mult, [tfb, pub], [aT_b[fl]])
                    wp.done(bi)
                    wp.done(bi + 1)
                xin, xin_b = (src, src_b) if fs == 0 else (S_tmp, db["tmp"])
                xout, xout_b = (dst, dst_b) if fs == c.FSPLIT - 1 else (S_tmp, db["tmp"])
                for db_ in range(ND // 2):
                    bi = dn_i[(ti, fs, db_)]
                    u = wp.get(bi)
                    for cc in range(2):
                        dc = db_ * 2 + cc
                        for su in range(NSUB):
                            sl = slice(t0 + su * 512, t0 + (su + 1) * 512)
                            xt, xtb = xo[(dc * NSUB + su) % len(xo)]
                            B.ld(xt, xin[dc * 128:(dc + 1) * 128, sl], [xin_b], [xtb])
                            pd, pdb = B.ps()
                            for kc in range(NFH):
                                l, lb = B.wl(u, kc, cc * 128)
                                B.mm(pd[:, :], l, aT[:, kc, su * 512:(su + 1) * 512], kc == 0, kc == NFH - 1,
                                     [lb, aT_b[kc]], [pdb])
                            B.stt(xt, pd[:, :], Gcol[:, dc:dc + 1], xt, ALU.mult, ALU.add, [pdb, xtb, AB_b], [xtb])
                            B.dma(xout[dc * 128:(dc + 1) * 128, sl], xt, [xtb], [xout_b])
                    wp.done(bi)

    ffn_phase(xT_ctx, Buf(), S_x1c, db["x1c"], 0, f1g, f1u, f1d, T // TT)
    ffn_phase(xT_own, Buf(), S_x1, db["x1"], 0, f1g, f1u, f1d, T // TT)

    def proj_phase(src, src_b, is_ctx):
        B.phase()
        hT, _ = B.at([ND, TT], BF16)
        hT_b = [Buf() for _ in range(ND)]
        xr = [B.at([TT], F32) for _ in range(4)]
        sq = [B.at([TT], BF16) for _ in range(2)]
        rstd, rstd_b = B.at([TT], F32)
        tmpf = [B.at([512], F32) for _ in range(4)]
        rp = [B.at([2, 512], F32) for _ in range(4)]
        xs = [B.at([512], F32) for _ in range(3)]
        ra = [B.at([512], F32) for _ in range(3)]
        rb = [B.at([512], F32) for _ in range(3)]
        ev = [B.at([512], BF16) for _ in range(4)]
        evi = [0]
        Acol, Bcol = AB[:, 1, 0, :], AB[:, 1, 1, :]
        loc0 = 0 if is_ctx else T
        for ti in range(T // TT):
            t0 = ti * TT
            norm_apply(src, src_b, t0, hT, hT_b, Acol, Bcol, xr, sq, rstd, rstd_b, tmpf)
            blocks = []
            if not is_ctx:
                for i in range(HA // 2):
                    blocks.append(("qa", c.oqa + i * 256, 256, i * 2))
            for i in range(HA // 2):
                blocks.append(("ka", c.oka + i * 256, 256, i * 2))
            for i in range(HA // 2):
                blocks.append(("va", c.ova + i * 256, 256, i * 2))
            if not is_ctx:
                for i in range(HB * 64 // 256):
                    blocks.append(("qb", c.oqb + i * 256, 256, i * 2))
            blocks.append(("kb", c.okb, 128, 0))
            blocks.append(("vb", c.ovb, 128, 0))
            if not is_ctx:
                for i in range(ND // 2):
                    blocks.append(("ga", c.oga + i * 256, 256, i * 2))
                for i in range(ND // 2):
                    blocks.append(("gb", c.ogb + i * 256, 256, i * 2))
            wp = WPipe(B, [(w_in, 0, ND, b[1], b[2]) for b in blocks])
            ropeslices = {}
            for su in range(NSUB):
                for which, tab in ((0, ropeA), (1, ropeB)):
                    r_, rb_ = rp[su * 2 + which] if NSUB * 2 <= len(rp) else rp[which]
                    lo = loc0 + t0 + su * 512
                    B.ld(r_, tab[:, :, lo:lo + 512].rearrange("a p n -> p a n"), [], [rb_])
                    ropeslices[(su, which)] = (r_, rb_)
            for bi, (kind, col0, ncols, ch0) in enumerate(blocks):
                u = wp.get(bi)
                if kind in ("va", "vb"):
                    for tk in range(TT // 128):
                        pv, pvb = B.ps()
                        for kc in range(ND):
                            r_, rb_ = B.wl(u, kc, 0, ncols)
                            B.mm(pv[:, 0:ncols], hT[:, kc, tk * 128:(tk + 1) * 128], r_, kc == 0, kc == ND - 1,
                                 [rb_, hT_b[kc]], [pvb])
                        e, eb = ev[evi[0] % len(ev)]
                        evi[0] += 1
                        B.cp('act', e[:, 0:ncols], pv[:, 0:ncols], [pvb], [eb])
                        tok = loc0 + t0 + tk * 128
                        if kind == "va":
                            B.dma(VA_d[tok:tok + 128, col0 - c.ova:col0 - c.ova + ncols], e[:, 0:ncols], [eb], [db["va"]])
                        else:
                            B.dma(VB_d[tok:tok + 128, 0:ncols], e[:, 0:ncols], [eb], [db["vb"]])
                    wp.done(bi)
                    continue
                for cc in range(ncols // 128):
                    ch = ch0 + cc
                    for su in range(NSUB):
                        sl = slice(su * 512, (su + 1) * 512)
                        pp, ppb = B.ps()
                        for kc in range(ND):
                            l, lb = B.wl(u, kc, cc * 128)
                            B.mm(pp[:, :], l, hT[:, kc, sl], kc == 0, kc == ND - 1, [lb, hT_b[kc]], [ppb])
                        tok = t0 + su * 512
                        if kind in ("ga", "gb"):
                            e, eb = ev[evi[0] % len(ev)]
                            evi[0] += 1
                            B.act(e, pp[:, :], AF.Sigmoid, [ppb], [eb])
                            dd, ddb = (SGA_d, db["sga"]) if kind == "ga" else (SGB_d, db["sgb"])
                            B.dma(dd[ch * 128:(ch + 1) * 128, tok:tok + 512], e, [eb], [ddb])
                            continue
                        isA = kind in ("qa", "ka")
                        rt, rtb = ropeslices[(su, 0 if isA else 1)]
                        x_, xb_ = xs[evi[0] % 3]
                        a_, ab_ = ra[evi[0] % 3]
                        b_, bb_ = rb[evi[0] % 3]
                        B.cp('act', x_, pp[:, :], [ppb], [xb_])
                        B.tt('pool', a_, x_, rt[:, 0, :], ALU.mult, [xb_, rtb], [ab_])
                        hs = 64 if isA else 32
                        for g0 in range(0, 128, 2 * hs):
                            B.tt('pool', b_[g0:g0 + hs, :], x_[g0 + hs:g0 + 2 * hs, :], rt[g0 + hs:g0 + 2 * hs, 1, :],
                                 ALU.mult, [xb_, rtb], [bb_])
                            B.tt('pool', b_[g0 + hs:g0 + 2 * hs, :], x_[g0:g0 + hs, :], rt[g0:g0 + hs, 1, :],
                                 ALU.mult, [xb_, rtb], [bb_])
                        e, eb = ev[evi[0] % len(ev)]
                        evi[0] += 1
                        if isA:
                            B.tt('dve', e, a_, b_, ALU.add, [ab_, bb_], [eb])
                            if kind == "qa":
                                B.dma(QA_d[ch * 128:(ch + 1) * 128, tok:tok + 512], e, [eb], [db["qa"]])
                            else:
                                B.dma(KA_d[ch * 128:(ch + 1) * 128, loc0 + tok:loc0 + tok + 512], e, [eb], [db["ka"]])
                        else:
                            B.tt('dve', e[0:64, 0:512], a_[0:64, :], b_[0:64, :], ALU.add, [ab_, bb_], [eb])
                            e2, eb2 = ev[evi[0] % len(ev)]
                            evi[0] += 1
                            B.tt('dve', e2[0:64, 0:512], a_[64:128, :], b_[64:128, :], ALU.add, [ab_, bb_], [eb2])
                            for hh, (ee, eeb) in enumerate(((e, eb), (e2, eb2))):
                                if kind == "qb":
                                    head = ch * 2 + hh
                                    B.dma(QB_d[:, tok // 128:tok // 128 + 4, head, :],
                                          ee[0:64, 0:512].rearrange("p (a b) -> p a b", a=4), [eeb], [db["qb"]])
                                else:
                                    B.dma(KB_d[:, hh, loc0 + tok:loc0 + tok + 512], ee[0:64, 0:512], [eeb], [db["kb"]])
                wp.done(bi)

    proj_phase(S_x1c, db["x1c"], True)
    proj_phase(S_x1, db["x1"], False)

    B.phase()
    scaleA = 128.0 ** -0.5
    KT = [B.at([T2], BF16) for _ in range(2)]
    V1 = [B.at([NKT, 132], BF16) for _ in range(2)]
    QT = [B.at([T], BF16) for _ in range(2)]
    kmf, kmf_b = B.at([NKB], F32)
    kmT, kmT_b = B.at([NKB], BF16)
    gsm = [B.at([NKB], F32) for _ in range(2)]
    top8 = [B.at([8], F32) for _ in range(2)]
    SEL, SEL_b = B.at([NQT, NKB], F32)
    Pt = [B.at([256], BF16) for _ in range(6)]
    acc = [B.at([132], F32) for _ in range(4)]
    rec = [B.at([1], F32) for _ in range(2)]
    yt = [B.at([128], F32) for _ in range(2)]
    ytT = [B.at([256], BF16) for _ in range(2)]
    for vv, vb_ in V1:
        B.memset('pool', vv[:, :, 128:129], 1.0, [vb_])
    pti = [0]
    VBb = VBI[:, 0:NQT * NKB].rearrange("p (a b) -> p a b", a=NQT)
    VBv = VBI[:, NQT * NKB:2 * NQT * NKB].rearrange("p (a b) -> p a b", a=NQT)

    def load_head(h):
        kt, ktb = KT[h % 2]
        v1, v1b = V1[h % 2]
        qt, qtb = QT[h % 2]
        B.ld(kt, KA_d[h * 128:(h + 1) * 128, :], [db["ka"]], [ktb])
        B.ld(v1[:, :, 0:128], VA_d[:, h * 128:(h + 1) * 128].rearrange("(n p) d -> p n d", p=128), [db["va"]], [v1b])
        B.ld(qt, QA_d[h * 128:(h + 1) * 128, :], [db["qa"]], [qtb])

    load_head(0)
    for h in range(HA):
        if h + 1 < HA:
            load_head(h + 1)
        kt, ktb = KT[h % 2]
        v1, v1b = V1[h % 2]
        qt, qtb = QT[h % 2]
        B.R.add('dve', lambda e, kt=kt: e.tensor_reduce(kmf, kt.rearrange("p (n k) -> p n k", k=256), AX.X, ALU.add),
                [ktb], [kmf_b])
        B.ts('dve', kmT, kmf, 1.0 / 256, None, ALU.mult, None, [kmf_b], [kmT_b])
        for q in range(NQT):
            pg, pgb = B.ps()
            B.mm(pg[:, 0:NKB], qt[:, q * 128:(q + 1) * 128], kmT, True, True, [qtb, kmT_b], [pgb])
            g_, gb_ = gsm[q % 2]
            t8, t8b = top8[q % 2]
            B.tt('dve', g_, pg[:, 0:NKB], VBb[:, q, :], ALU.add, [pgb, VBI_b], [gb_])
            B.R.add('dve', lambda e, t8=t8, g_=g_: e.max(t8, g_), [gb_], [t8b])
            B.ts('dve', g_, g_, t8[:, c.TOPK - 1:c.TOPK], None, ALU.is_ge, None, [gb_, t8b], [gb_])
            B.tt('dve', SEL[:, q, :], g_, VBv[:, q, :], ALU.mult, [gb_, VBI_b], [SEL_b])
        for j in range(T // 256):
            nb = T // 256 + j
            q0 = j * 256
            a0, a0b = acc[(2 * j) % 4]
            a1, a1b = acc[(2 * j + 1) % 4]
            accs = ((a0, a0b), (a1, a1b))
            k0 = 2 * nb
            s0, s0b = B.ps()
            B.mm(s0[:, 0:256], kt[:, k0 * 128:(k0 + 1) * 128], qt[:, q0:q0 + 256], True, True, [ktb, qtb], [s0b])
            p0, p0b = Pt[pti[0] % 6]; pti[0] += 1
            B.act(p0, s0[:, 0:256], AF.Exp, [s0b], [p0b], scale=scaleA)
            B.tt('pool', p0[:, 0:128], p0[:, 0:128], TRI, ALU.mult, [p0b, consts_b], [p0b])
            s1, s1b = B.ps()
            B.mm(s1[:, 0:128], kt[:, (k0 + 1) * 128:(k0 + 2) * 128], qt[:, q0 + 128:q0 + 256], True, True, [ktb, qtb], [s1b])
            p1, p1b = Pt[pti[0] % 6]; pti[0] += 1
            B.act(p1[:, 0:128], s1[:, 0:128], AF.Exp, [s1b], [p1b], scale=scaleA)
            B.tt('pool', p1[:, 0:128], p1[:, 0:128], TRI, ALU.mult, [p1b, consts_b], [p1b])
            o0, o0b = B.ps()
            B.mm(o0[:, 0:129], p0[:, 0:128], v1[:, k0, 0:129], True, True, [p0b, v1b], [o0b])
            B.cp('dve', a0[:, 0:129], o0[:, 0:129], [o0b], [a0b])
            o1, o1b = B.ps()
            B.mm(o1[:, 0:129], p0[:, 128:256], v1[:, k0, 0:129], True, False, [p0b, v1b], [o1b])
            B.mm(o1[:, 0:129], p1[:, 0:128], v1[:, k0 + 1, 0:129], False, True, [p1b, v1b], [o1b])
            B.cp('dve', a1[:, 0:129], o1[:, 0:129], [o1b], [a1b])
            for n in range(nb):
                pk = []
                sp_, spb = B.ps()
                for kk in range(2):
                    ktile = 2 * n + kk
                    B.mm(sp_[:, kk * 256:(kk + 1) * 256], kt[:, ktile * 128:(ktile + 1) * 128], qt[:, q0:q0 + 256], True, True,
                         [ktb, qtb], [spb])
                for kk in range(2):
                    pp_, ppb = Pt[pti[0] % 6]; pti[0] += 1
                    B.act(pp_, sp_[:, kk * 256:(kk + 1) * 256], AF.Exp, [spb], [ppb], scale=scaleA)
                    pk.append((pp_, ppb, 2 * n + kk))
                oo, oob = B.ps()
                for qs in range(2):
                    for kk in range(2):
                        pp_, ppb, ktile = pk[kk]
                        B.mm(oo[:, qs * 256:qs * 256 + 129], pp_[:, qs * 128:(qs + 1) * 128], v1[:, ktile, 0:129], kk == 0, kk == 1,
                             [ppb, v1b], [oob])
                for qs in range(2):
                    aa, aab = accs[qs]
                    B.stt(aa[:, 0:129], oo[:, qs * 256:qs * 256 + 129], SEL[:, 2 * j + qs, n:n + 1], aa[:, 0:129], ALU.mult, ALU.add,
                          [oob, SEL_b, aab], [aab])
            yT, yTb = ytT[j % 2]
            for qs in range(2):
                aa, aab = accs[qs]
                r_, rb_ = rec[qs]
                y_, yb_ = yt[qs]
                B.R.add('dve', lambda e, r_=r_, aa=aa: e.reciprocal(r_, aa[:, 128:129]), [aab], [rb_])
                B.ts('dve', y_, aa[:, 0:128], r_[:, 0:1], None, ALU.mult, None, [aab, rb_], [yb_])
                pt_, ptb = B.ps()
                B.tr(pt_[:, 0:128], y_, IDENT, [yb_, consts_b], [ptb])
                B.cp('act', yT[:, qs * 128:(qs + 1) * 128], pt_[:, 0:128], [ptb], [yTb])
            B.dma(YA_d[h * 128:(h + 1) * 128, q0:q0 + 256], yT, [yTb], [db["ya"]])

    B.phase()
    scaleB = 64.0 ** -0.5
    KBT, KBT_b = B.at([KVB, T2], BF16)
    VB1, VB1_b = B.at([NKT, KVB, 66], BF16)
    QBT = [B.at([HB, 128], BF16) for _ in range(2)]
    Pb = [B.at([512], BF16) for _ in range(4)]
    den = [B.at([4], F32) for _ in range(2)]
    ybt = [B.at([HB * 64], F32) for _ in range(2)]
    ybT = [B.at([HB * 64 // 128, 128], BF16) for _ in range(2)]
    B.dma(KBT[0:64], KB_d[:, :, :], [db["kb"]], [KBT_b])
    B.memset('pool', VB1[:, :, :, 64:65], 1.0, [VB1_b])
    for g in range(KVB):
        B.dma(VB1[:, :, g, 0:64], VB_d[:, g * 64:(g + 1) * 64].rearrange("(n p) d -> p n d", p=128), [db["vb"]], [VB1_b])
    HG = min(4, G)
    pbi = [0]
    for i in range(NQT):
        qb_, qbb = QBT[i % 2]
        B.ld(qb_[0:64], QB_d[:, i, :, :], [db["qb"]], [qbb])
        cur = T // 128 + i
        prv = cur - 1
        y_, yb_ = ybt[i % 2]
        for g in range(KVB):
            for hg in range(G // HG):
                h0 = g * G + hg * HG
                rhs = qb_[0:64, h0:h0 + HG, :]
                ps_ = []
                for (ktile, msk) in ((prv, MASKP0 if i == 0 else MASKP), (cur, TRI)):
                    sp_, spb = B.ps()
                    B.mm(sp_[:, 0:HG * 128], KBT[0:64, g, ktile * 128:(ktile + 1) * 128], rhs, True, True, [KBT_b, qbb], [spb])
                    pp_, ppb = Pb[pbi[0] % 4]; pbi[0] += 1
                    B.act(pp_[:, 0:HG * 128], sp_[:, 0:HG * 128], AF.Exp, [spb], [ppb], scale=scaleB)
                    for hh in range(HG):
                        B.tt('pool', pp_[:, hh * 128:(hh + 1) * 128], pp_[:, hh * 128:(hh + 1) * 128], msk, ALU.mult,
                             [ppb, consts_b], [ppb])
                    ps_.append((pp_, ppb, ktile))
                oo, oob = B.ps()
                for hh in range(HG):
                    for kk in range(2):
                        pp_, ppb, ktile = ps_[kk]
                        B.mm(oo[:, hh * 65:hh * 65 + 65], pp_[:, hh * 128:(hh + 1) * 128], VB1[:, ktile, g, 0:65],
                             kk == 0, kk == 1, [ppb, VB1_b], [oob])
                d_, db_ = den[(g * (G // HG) + hg) % 2]
                ov = oo[:, 0:HG * 65].rearrange("p (h d) -> p h d", h=HG)
                B.tt('dve', d_[:, 0:HG], ov[:, :, 64], ESK[:, h0:h0 + HG], ALU.add, [oob, ESK_b], [db_])
                B.R.add('dve', lambda e, d_=d_: e.reciprocal(d_[:, 0:HG], d_[:, 0:HG]), [db_], [db_])
                for hh in range(HG):
                    B.ts('dve', y_[:, (h0 + hh) * 64:(h0 + hh + 1) * 64], ov[:, hh, 0:64], d_[:, hh:hh + 1], None,
                         ALU.mult, None, [oob, db_], [yb_])
        yT, yTb = ybT[i % 2]
        for ch in range(HB * 64 // 128):
            pt_, ptb = B.ps()
            B.tr(pt_[:, 0:128], y_[:, ch * 128:(ch + 1) * 128], IDENT, [yb_, consts_b], [ptb])
            B.cp('act', yT[:, ch, :], pt_[:, 0:128], [ptb], [yTb])
        B.dma(YB_d[:, i * 128:(i + 1) * 128].rearrange("(c p) n -> p c n", p=128), yT, [yTb], [db["yb"]])

    B.phase()
    NCA, NCB = HA, HB * 64 // 128
    yaT, yaT_b = B.at([NCA, TT], BF16)
    ybT2, ybT2_b = B.at([NCB, TT], BF16)
    mT, _ = B.at([ND, TT], BF16)
    mT_b = [Buf() for _ in range(ND)]
    sg = [B.at([2, 512], BF16) for _ in range(3)]
    tm = [B.at([2, 512], F32) for _ in range(3)]
    xo = [B.at([512], F32) for _ in range(4)]
    Gcol = AB[:, 1, 2, :]
    k_i = [0]
    for ti in range(T // TT):
        t0 = ti * TT
        B.ld(yaT, YA_d[:, t0:t0 + TT].rearrange("(c p) n -> p c n", p=128), [db["ya"]], [yaT_b])
        B.ld(ybT2, YB_d[:, t0:t0 + TT].rearrange("(c p) n -> p c n", p=128), [db["yb"]], [ybT2_b])
        specs = []
        for d2 in range(ND // 2):
            specs.append((w_bm, 0, NCA, d2 * 256, 256))
            specs.append((w_bs, 0, NCB, d2 * 256, 256))
        wp = WPipe(B, specs)
        for d2 in range(ND // 2):
            um = wp.get(2 * d2)
            us = wp.get(2 * d2 + 1)
            for cc in range(2):
                dc = d2 * 2 + cc
                for su in range(NSUB):
                    sl = slice(su * 512, (su + 1) * 512)
                    tok = t0 + su * 512
                    s_, sb_ = sg[k_i[0] % 3]
                    t_, tb_ = tm[k_i[0] % 3]
                    k_i[0] += 1
                    B.ld(s_[:, 0, :], SGA_d[dc * 128:(dc + 1) * 128, tok:tok + 512], [db["sga"]], [sb_])
                    B.ld(s_[:, 1, :], SGB_d[dc * 128:(dc + 1) * 128, tok:tok + 512], [db["sgb"]], [sb_])
                    pm, pmb = B.ps()
                    pq, pqb = B.ps()
                    for kc in range(NCA):
                        l, lb = B.wl(um, kc, cc * 128)
                        B.mm(pm[:, :], l, yaT[:, kc, sl], kc == 0, kc == NCA - 1, [lb, yaT_b], [pmb])
                    for kc in range(NCB):
                        l, lb = B.wl(us, kc, cc * 128)
                        B.mm(pq[:, :], l, ybT2[:, kc, sl], kc == 0, kc == NCB - 1, [lb, ybT2_b], [pqb])
                    B.tt('dve', t_[:, 0, :], pm[:, :], s_[:, 0, :], ALU.mult, [pmb, sb_], [tb_])
                    B.tt('dve', t_[:, 1, :], pq[:, :], s_[:, 1, :], ALU.mult, [pqb, sb_], [tb_])
                    B.tt('pool', mT[:, dc, sl], t_[:, 0, :], t_[:, 1, :], ALU.add, [tb_], [mT_b[dc]])
            wp.done(2 * d2)
            wp.done(2 * d2 + 1)
        specs = [(w_out, 0, ND, d2 * 256, 256) for d2 in range(ND // 2)]
        wp = WPipe(B, specs)
        for d2 in range(ND // 2):
            u = wp.get(d2)
            for cc in range(2):
                dc = d2 * 2 + cc
                for su in range(NSUB):
                    sl = slice(t0 + su * 512, t0 + (su + 1) * 512)
                    xt, xtb = xo[(dc * NSUB + su) % len(xo)]
                    B.ld(xt, S_x1[dc * 128:(dc + 1) * 128, sl], [db["x1"]], [xtb])
                    pd, pdb = B.ps()
                    for kc in range(ND):
                        l, lb = B.wl(u, kc, cc * 128)
                        B.mm(pd[:, :], l, mT[:, kc, su * 512:(su + 1) * 512], kc == 0, kc == ND - 1, [lb, mT_b[kc]], [pdb])
                    B.stt(xt, pd[:, :], Gcol[:, dc:dc + 1], xt, ALU.mult, ALU.add, [pdb, xtb, AB_b], [xtb])
                    B.dma(S_x2[dc * 128:(dc + 1) * 128, sl], xt, [xtb], [db["x2"]])
            wp.done(d2)

    ffn_phase(S_x2, db["x2"], S_x3, db["x3"], 2, f2g, f2u, f2d, T // TT)
    B.phase()
    xr = [B.at([TT], F32) for _ in range(4)]
    sq = [B.at([TT], BF16) for _ in range(2)]
    rstd, rstd_b = B.at([TT], F32)
    tmpf = [B.at([512], F32) for _ in range(4)]
    for ti in range(T // TT):
        norm_apply(S_x3, db["x3"], ti * TT, None, None, GN[:, 3 * ND:4 * ND], None, xr, sq, rstd, rstd_b, tmpf,
                   out_dram=outT, out_b=db["out"])
    B.R.finish(nc)
    return nc


def rope_tables(cfg, half):
    T = cfg.T
    pos_ctx = np.arange(0, T, dtype=np.float64)
    pos_own = np.arange(half * T, (half + 1) * T, dtype=np.float64)
    pos = np.concatenate([pos_ctx, pos_own])
    out = []
    for hs in (64, 32):
        inv = 10000.0 ** (-np.arange(hs, dtype=np.float64) / hs)
        ang = (pos.astype(np.float32)[None, :] * inv.astype(np.float32)[:, None]).astype(np.float32)
        cos = np.cos(ang).astype(np.float32)
        sin = np.sin(ang).astype(np.float32)
        reps = 128 // (2 * hs)
        cosT = np.concatenate([cos, cos] * reps, axis=0)
        sinT = np.concatenate([sin, -sin] * reps, axis=0)
        out.append(np.stack([cosT, sinT]).astype(np.float32))
    return out


def const_masks(cfg, half):
    k = np.arange(128)[:, None]
    q = np.arange(128)[None, :]
    tri = (k <= q).astype(np.float32)
    mp = (k > q).astype(np.float32)
    mp0 = mp * float(half)
    ident = np.eye(128, dtype=np.float32)
    cm = np.concatenate([tri, mp, mp0, ident], axis=1)
    NQT, NKB = cfg.NQT, cfg.NKB
    valid = np.zeros((NQT, NKB), np.float32)
    for qt in range(NQT):
        j = qt // 2
        for n in range(NKB):
            if n < NKB // 2:
                valid[qt, n] = float(half)
            elif n < NKB // 2 + j:
                valid[qt, n] = 1.0
    bias = np.where(valid > 0, 0.0, NEG).astype(np.float32)
    vb = np.concatenate([bias.reshape(-1), valid.reshape(-1)])[None, :].repeat(128, 0).astype(np.float32)
    return cm, vb


def colT(v, n):
    return np.ascontiguousarray(np.asarray(v, np.float32).reshape(n, 128).T)


def make_in_maps(cfg, inp):
    D, ND, T = cfg.D, cfg.ND, cfg.T
    f = lambda a: np.ascontiguousarray(np.asarray(a, np.float32))
    x = np.asarray(inp["x"], np.float32)
    shared = {
        "w_ada": f(inp["w_ada"][0]), "b_adaT": colT(inp["b_ada"][0], 9 * ND),
        "gains": np.concatenate([colT(inp["norm_ffn1"][0], ND), colT(inp["norm_mix"][0], ND),
                                 colT(inp["norm_ffn2"][0], ND), colT(inp["norm_final"], ND)], axis=1),
        "f1g": f(inp["ffn1_gate"][0]), "f1u": f(inp["ffn1_up"][0]), "f1d": f(inp["ffn1_down"][0]),
        "w_in": f(inp["w_in"][0]),
        "sinks": np.ascontiguousarray(np.broadcast_to(np.asarray(inp["swa_sinks"][0], np.float32)[None, :], (128, cfg.HB))),
        "w_bm": f(inp["w_branch_moba"][0]), "w_bs": f(inp["w_branch_swa"][0]), "w_out": f(inp["w_out"][0]),
        "f2g": f(inp["ffn2_gate"][0]), "f2u": f(inp["ffn2_up"][0]), "f2d": f(inp["ffn2_down"][0]),
    }
    maps = []
    for core in range(2 * cfg.BATCH):
        b, half = core // 2, core % 2
        rA, rB = rope_tables(cfg, half)
        cm, vb = const_masks(cfg, half)
        m = dict(shared)
        m["xT_own"] = np.ascontiguousarray(x[b, half * T:(half + 1) * T, :].T)
        m["xT_ctx"] = np.ascontiguousarray(x[b, 0:T, :].T)
        m["cT"] = colT(inp["c"][b], ND)
        m["ropeA"], m["ropeB"], m["cmask"], m["vbias"] = rA, rB, cm, vb
        maps.append(m)
    return maps


def assemble(cfg, results):
    out = np.empty((cfg.BATCH, cfg.SEQ, cfg.D), np.float32)
    for core, r in enumerate(results):
        b, half = core // 2, core % 2
        out[b, half * cfg.T:(half + 1) * cfg.T, :] = np.asarray(r["outT"], np.float32).T
    return out


def kernel(**inputs):
    cfg = Cfg()
    nc = build(cfg)
    maps = make_in_maps(cfg, inputs)
    res = run_bass_kernel_spmd(nc, maps, core_ids=list(range(8)))
    return assemble(cfg, res.results)
```

```python
import numpy as np
import ml_dtypes
import concourse.bass as bass
import concourse.mybir as mybir
from concourse.bass_utils import run_bass_kernel_spmd

F32 = mybir.dt.float32
BF16 = mybir.dt.bfloat16
ALU = mybir.AluOpType
AF = mybir.ActivationFunctionType
AX = mybir.AxisListType
NEG = -1.0e30


class Cfg:
    def __init__(s, D=2048, FF=5632, HA=8, HB=16, KVB=2, SEQ=4096, BATCH=4, TT=1024, FSPLIT=2):
        s.D, s.FF, s.HA, s.HB, s.KVB, s.SEQ, s.BATCH, s.TT, s.FSPLIT = D, FF, HA, HB, KVB, SEQ, BATCH, TT, FSPLIT
        s.T = SEQ // 2
        s.ND = D // 128
        s.NF = FF // 128
        s.BLK, s.W, s.TOPK = 256, 128, 3
        s.G = HB // KVB
        s.oqa = 0
        s.oka = HA * 128
        s.ova = 2 * HA * 128
        s.oqb = 3 * HA * 128
        s.okb = s.oqb + HB * 64
        s.ovb = s.okb + KVB * 64
        s.oga = s.ovb + KVB * 64
        s.ogb = s.oga + D
        s.INC = s.ogb + D
        s.NSUB = TT // 512
        s.NQT = s.T // 128
        s.NKB = 2 * s.T // 256
        s.NKT = 2 * s.T // 128
        assert KVB == 2 and s.T % TT == 0 and TT % 512 == 0 and s.NF % FSPLIT == 0


class Buf:
    __slots__ = ("n", "w", "r")

    def __init__(s, n=""):
        s.n = n
        s.w = {}
        s.r = {}


class Op:
    __slots__ = ("eng", "fn", "deps", "sig", "val", "dsem", "dval", "waits", "isdma")


class Rec:
    NDS = 24
    QPOOL = {'sp': (0, 16), 'act': (16, 8)}

    def __init__(s):
        s.ops = []
        s.last = {}
        s.bar = None
        s.bar_done = set()
        s.ndma = 0
        s.dma_last = {}
        s.qcnt = {}

    def add(s, eng, fn, reads=(), writes=(), dma=False):
        op = Op()
        op.eng, op.fn, op.isdma, op.sig, op.deps = eng, fn, dma, False, set()
        op.val = op.dsem = op.dval = None
        for b in reads:
            for k, v in b.w.items():
                if k == 'dma':
                    op.deps.update(v)
                elif k == eng and not dma and eng == 'pe':
                    pass
                else:
                    op.deps.add(v)
        for b in writes:
            for k, v in b.r.items():
                if k == 'dma':
                    op.deps.update(v)
                elif k == eng and not dma:
                    pass
                else:
                    op.deps.add(v)
            for k, v in b.w.items():
                if k == 'dma':
                    op.deps.update(v)
                elif k == eng and not dma:
                    pass
                else:
                    op.deps.add(v)
        if s.bar is not None and eng not in s.bar_done:
            op.deps.update(s.bar)
            s.bar_done.add(eng)
        if dma:
            base, cnt_ = s.QPOOL[eng]
            nq = s.qcnt.get(eng, 0)
            s.qcnt[eng] = nq + 1
            op.dsem = base + nq % cnt_
            op.dval = 16 * (nq // cnt_ + 1)
            s.ndma += 1
            prev = s.dma_last.get(op.dsem)
            if prev is not None:
                op.deps.add(prev)
            s.dma_last[op.dsem] = op
            op.sig = True
        for b in reads:
            if dma:
                b.r.setdefault('dma', []).append(op)
            else:
                b.r[eng] = op
        for b in writes:
            if b.r:
                b.w = {}
                b.r = {}
            if dma:
                b.w.setdefault('dma', []).append(op)
            else:
                b.w[eng] = op
        if not dma:
            s.last[eng] = op
        s.ops.append(op)
        return op

    def barrier(s):
        s.bar = set(s.last.values()) | set(s.dma_last.values())
        s.bar_done = set()

    def finish(s, nc):
        for op in s.ops:
            for d in op.deps:
                d.sig = True
        cnt = {}
        for op in s.ops:
            if op.isdma:
                continue
            if op.sig:
                cnt[op.eng] = cnt.get(op.eng, 0) + 1
                op.val = cnt[op.eng]
        engs = ['pe', 'act', 'dve', 'pool', 'sp']
        esem = {e: nc.alloc_semaphore("sem_" + e) for e in engs if e != 'sp'}
        dsem = [nc.alloc_semaphore("dsem%d" % i) for i in range(s.NDS)]
        per = {e: [] for e in engs}
        for op in s.ops:
            per[op.eng].append(op)
        for e in engs:
            waited = {}
            for op in per[e]:
                need = {}
                for d in op.deps:
                    if d.isdma:
                        k, v = ('d', d.dsem), d.dval
                    else:
                        k, v = ('e', d.eng), d.val
                    if v > need.get(k, 0):
                        need[k] = v
                op.waits = []
                for k, v in need.items():
                    if v > waited.get(k, 0):
                        waited[k] = v
                        op.waits.append((k, v))
        dfinal = {}
        for op in s.ops:
            if op.isdma:
                dfinal[op.dsem] = op.dval

        def body_for(e):
            def body(eng):
                for op in per[e]:
                    for (k, v) in op.waits:
                        eng.wait_ge(dsem[k[1]] if k[0] == 'd' else esem[k[1]], v)
                    ins = op.fn(eng)
                    if op.isdma:
                        ins.then_inc(dsem[op.dsem], 16)
                    elif op.sig:
                        ins.then_inc(esem[e], 1)
                if e == 'sp':
                    for k, v in dfinal.items():
                        eng.wait_ge(dsem[k], v)
                    for e2 in esem:
                        if cnt.get(e2, 0) > 0:
                            eng.wait_ge(esem[e2], cnt[e2])
            return body

        with nc.Block() as blk:
            blk.tensor(body_for('pe'))
            blk.scalar(body_for('act'))
            blk.vector(body_for('dve'))
            blk.gpsimd(body_for('pool'))
            blk.sync(body_for('sp'))


class Builder:
    def __init__(s, cfg):
        s.c = cfg
        s.nc = bass.Bass("TRN2", target_bir_lowering=False)
        s.R = Rec()
        s.dram = {}
        s.nbuf = 0
        s.cast_rr = 0

    def din(s, name, shape, dt=F32):
        t = s.nc.dram_tensor(name, list(shape), dt, kind="ExternalInput").ap()
        s.dram[name] = t
        return t

    def dscr(s, name, shape, dt):
        return s.nc.dram_tensor(name, list(shape), dt, kind="Internal").ap()

    def buf(s, n=""):
        return Buf(n)

    def mm(s, out, lhsT, rhs, start, stop, reads, writes):
        s.R.add('pe', lambda e: e.matmul(out, lhsT, rhs, start=start, stop=stop), reads, writes)

    def tr(s, out, in_, ident, reads, writes):
        s.R.add('pe', lambda e: e.transpose(out, in_, ident), reads, writes)

    def dma(s, out, in_, reads, writes, q='sp'):
        s.R.add(q, lambda e: e.dma_start(out=out, in_=in_), reads, writes, dma=True)

    def ld(s, out, in_, reads, writes):
        s.dma(out, in_, reads, writes, q='act')

    def act(s, out, in_, func, reads, writes, bias=None, scale=None, accum_out=None):
        kw = {}
        if bias is not None:
            kw['bias'] = bias
        if scale is not None:
            kw['scale'] = scale
        if accum_out is not None:
            kw['accum_out'] = accum_out
        s.R.add('act', lambda e: e.activation(out, in_, func, **kw), reads, writes)

    def tt(s, eng, out, in0, in1, op, reads, writes):
        s.R.add(eng, lambda e: e.tensor_tensor(out, in0, in1, op), reads, writes)

    def ts(s, eng, out, in0, s1, s2, op0, op1, reads, writes):
        if op1 is None:
            s.R.add(eng, lambda e: e.tensor_scalar(out, in0, s1, None, op0), reads, writes)
        else:
            s.R.add(eng, lambda e: e.tensor_scalar(out, in0, s1, s2, op0, op1), reads, writes)

    def stt(s, out, in0, scalar, in1, op0, op1, reads, writes):
        s.R.add('dve', lambda e: e.scalar_tensor_tensor(out, in0, scalar, in1, op0, op1), reads, writes)

    def cp(s, eng, out, in_, reads, writes):
        if eng == 'act':
            s.R.add('act', lambda e: e.activation(out, in_, AF.Copy), reads, writes)
        else:
            s.R.add(eng, lambda e: e.tensor_copy(out, in_), reads, writes)

    def memset(s, eng, ap, val, writes):
        s.R.add(eng, lambda e: e.memset(ap, val), (), writes)

    def init_psum(s):
        s.psb = [(s.nc.alloc_psum_tensor("psb%d" % i, [128, 512], F32), Buf("ps%d" % i)) for i in range(8)]
        s.psi = 0

    def ps(s):
        t = s.psb[s.psi % 8]
        s.psi += 1
        return t

    def init_arena(s, nbytes):
        s.arena = s.nc.alloc_sbuf_tensor("arena", [128, nbytes // 4], F32)
        s.arena_n = nbytes // 4
        s.aoff = 0

    def phase(s):
        s.R.barrier()
        s.aoff = 0

    def at(s, shape, dt):
        n = int(np.prod(shape))
        nw = (n * (2 if dt == BF16 else 4) + 3) // 4
        nw = (nw + 7) // 8 * 8
        assert s.aoff + nw <= s.arena_n, ("arena overflow", s.aoff, nw, s.arena_n)
        ap = s.arena[:, s.aoff:s.aoff + nw]
        s.aoff += nw
        if dt == BF16:
            ap = ap.bitcast(BF16)
        ap = ap[:, 0:n]
        if len(shape) == 2:
            ap = ap.rearrange("p (a b) -> p a b", a=shape[0])
        elif len(shape) == 3:
            ap = ap.rearrange("p (a b c) -> p a b c", a=shape[0], b=shape[1])
        return ap, Buf()

    def init_wstream(s, nslot=12, nstage=4):
        nc = s.nc
        s.wring = [(nc.alloc_sbuf_tensor("wr%d" % i, [128, 8, 256], BF16), Buf()) for i in range(nslot)]
        s.wstage = [(nc.alloc_sbuf_tensor("ws%d" % i, [128, 8, 256], F32), Buf()) for i in range(nstage)]
        s.wri = 0
        s.wsi = 0
        s.wring_owner = [None] * nslot
        s.wblk_id = 0

    def wl(s, units, kc, c0, n=128):
        u = units[kc // 8]
        return u[0][:, kc % 8, c0:c0 + n], u[1]


class WPipe:
    AHEAD = 2

    def __init__(s, B, specs):
        s.B, s.specs = B, specs
        s.units, s.bunits = [], []
        for bi, (W, r0, nk, col0, ncols) in enumerate(specs):
            k, us = 0, []
            while k < nk:
                nku = min(8, nk - k)
                us.append(len(s.units))
                s.units.append((bi, W, r0 + k, nku, col0, ncols))
                k += nku
            s.bunits.append(us)
        s.nd = s.ncs = 0
        s.st, s.rg = {}, {}

    def pump(s, i):
        B = s.B
        NST = len(B.wstage)
        while True:
            if s.nd < len(s.units) and s.nd < s.ncs + NST:
                bi, W, r, nku, col0, ncols = s.units[s.nd]
                st, stb = B.wstage[B.wsi % NST]
                B.wsi += 1
                src = W[r * 128:(r + nku) * 128, col0:col0 + ncols].rearrange("(c p) n -> p c n", p=128)
                B.dma(st[:, 0:nku, 0:ncols], src, [], [stb])
                s.st[s.nd] = (st, stb)
                s.nd += 1
                continue
            if s.ncs < s.nd and s.units[s.ncs][0] <= i + s.AHEAD:
                si = B.wri % len(B.wring)
                if B.wring_owner[si] is None:
                    bi, W, r, nku, col0, ncols = s.units[s.ncs]
                    B.wri += 1
                    B.wring_owner[si] = 1
                    sl, slb = B.wring[si]
                    st, stb = s.st.pop(s.ncs)
                    B.cp('act', sl[:, 0:nku, 0:ncols], st[:, 0:nku, 0:ncols], [stb], [slb])
                    s.rg[s.ncs] = (sl, slb, nku, si)
                    s.ncs += 1
                    continue
            break

    def get(s, i):
        s.pump(i)
        us = s.bunits[i]
        assert all(u in s.rg for u in us), "weight ring stalled"
        return [s.rg[u] for u in us]

    def done(s, i):
        for u in s.bunits[i]:
            s.B.wring_owner[s.rg.pop(u)[3]] = None


def build(cfg):
    B = Builder(cfg)
    nc = B.nc
    c = cfg
    D, FF, ND, NF, T, TT, NSUB = c.D, c.FF, c.ND, c.NF, c.T, c.TT, c.NSUB
    HA, HB, KVB, G = c.HA, c.HB, c.KVB, c.G
    NQT, NKB, NKT = c.NQT, c.NKB, c.NKT
    T2 = 2 * T

    xT_own = B.din("xT_own", [D, T])
    xT_ctx = B.din("xT_ctx", [D, T])
    cT = B.din("cT", [128, ND])
    w_ada = B.din("w_ada", [D, 9 * D])
    b_adaT = B.din("b_adaT", [128, 9 * ND])
    gains = B.din("gains", [128, 4 * ND])
    f1g = B.din("f1g", [D, FF]); f1u = B.din("f1u", [D, FF]); f1d = B.din("f1d", [FF, D])
    w_in = B.din("w_in", [D, c.INC])
    sinks = B.din("sinks", [128, HB])
    w_bm = B.din("w_bm", [HA * 128, D]); w_bs = B.din("w_bs", [HB * 64, D]); w_out = B.din("w_out", [D, D])
    f2g = B.din("f2g", [D, FF]); f2u = B.din("f2u", [D, FF]); f2d = B.din("f2d", [FF, D])
    ropeA = B.din("ropeA", [2, 128, T2])
    ropeB = B.din("ropeB", [2, 128, T2])
    cmask = B.din("cmask", [128, 4 * 128])
    vbias = B.din("vbias", [128, 2 * NQT * NKB])
    outT = nc.dram_tensor("outT", [D, T], F32, kind="ExternalOutput").ap()

    S_x1 = B.dscr("S_x1", [D, T], F32); S_x1c = B.dscr("S_x1c", [D, T], F32)
    S_x2 = B.dscr("S_x2", [D, T], F32); S_x3 = B.dscr("S_x3", [D, T], F32)
    S_tmp = B.dscr("S_tmp", [D, T], F32)
    QA_d = B.dscr("QA_d", [HA * 128, T], BF16)
    KA_d = B.dscr("KA_d", [HA * 128, T2], BF16)
    VA_d = B.dscr("VA_d", [T2, HA * 128], BF16)
    QB_d = B.dscr("QB_d", [64, NQT, HB, 128], BF16)
    KB_d = B.dscr("KB_d", [64, KVB, T2], BF16)
    VB_d = B.dscr("VB_d", [T2, KVB * 64], BF16)
    SGA_d = B.dscr("SGA_d", [D, T], BF16); SGB_d = B.dscr("SGB_d", [D, T], BF16)
    YA_d = B.dscr("YA_d", [HA * 128, T], BF16); YB_d = B.dscr("YB_d", [HB * 64, T], BF16)
    db = {k: Buf(k) for k in ["x1", "x1c", "x2", "x3", "tmp", "qa", "ka", "va", "qb", "kb", "vb", "sga", "sgb", "ya", "yb", "out"]}

    B.init_psum()
    B.init_wstream(12, 4)
    consts = nc.alloc_sbuf_tensor("consts", [128, 4 * 128], F32); consts_b = Buf()
    ones_bf = nc.alloc_sbuf_tensor("ones_bf", [128, 128], BF16); ones_b = Buf()
    MOD = nc.alloc_sbuf_tensor("MOD", [128, 9 * ND], F32); MOD_b = Buf()
    GN = nc.alloc_sbuf_tensor("GN", [128, 4 * ND], F32); GN_b = Buf()
    AB = nc.alloc_sbuf_tensor("AB", [128, 3, 3, ND], F32); AB_b = Buf()
    VBI = nc.alloc_sbuf_tensor("VBI", [128, 2 * NQT * NKB], F32); VBI_b = Buf()
    ESK = nc.alloc_sbuf_tensor("ESK", [128, HB], F32); ESK_b = Buf()
    EPS = nc.alloc_sbuf_tensor("EPS", [128, 1], F32); EPS_b = Buf()
    B.init_arena(118 * 1024)

    TRI = consts[:, 0:128]; MASKP = consts[:, 128:256]; MASKP0 = consts[:, 256:384]; IDENT = consts[:, 384:512]

    B.dma(consts[:], cmask[:, :], [], [consts_b])
    B.dma(GN[:], gains[:, :], [], [GN_b])
    B.dma(VBI[:], vbias[:, :], [], [VBI_b])
    B.dma(ESK[:], sinks[:, :], [], [ESK_b])
    B.memset('dve', ones_bf[:], 1.0, [ones_b])
    B.memset('dve', EPS[:], 1e-6, [EPS_b])
    B.act(ESK[:], ESK[:], AF.Exp, [ESK_b], [ESK_b])

    B.phase()
    csb, csb_b = B.at([ND], F32)
    cbf, cbf_b = B.at([ND], BF16)
    bad, bad_b = B.at([9 * ND], F32)
    B.dma(csb, cT[:, :], [], [csb_b])
    B.dma(bad, b_adaT[:, :], [], [bad_b])
    B.act(cbf, csb, AF.Silu, [csb_b], [cbf_b])
    psm, psm_b = B.ps()
    specs = [(w_ada, 0, ND, cb * 256, 256) for cb in range(9 * D // 256)]
    wp = WPipe(B, specs)
    for cb in range(len(specs)):
        u = wp.get(cb)
        for cc in range(2):
            j = cb * 2 + cc
            for kc in range(ND):
                l, lb = B.wl(u, kc, cc * 128)
                B.mm(psm[:, j:j + 1], l, cbf[:, kc:kc + 1], kc == 0, kc == ND - 1, [lb, cbf_b], [psm_b])
        wp.done(cb)
    B.tt('dve', MOD[:], psm[:, 0:9 * ND], bad, ALU.add, [psm_b, bad_b], [MOD_b])
    for i in range(3):
        sh = MOD[:, (3 * i) * ND:(3 * i + 1) * ND]
        sc = MOD[:, (3 * i + 1) * ND:(3 * i + 2) * ND]
        gg = MOD[:, (3 * i + 2) * ND:(3 * i + 3) * ND]
        B.stt(AB[:, i, 0, :], sc, 1.0, GN[:, i * ND:(i + 1) * ND], ALU.add, ALU.mult, [MOD_b, GN_b], [AB_b])
        B.cp('dve', AB[:, i, 1, :], sh, [MOD_b], [AB_b])
        B.ts('dve', AB[:, i, 2, :], gg, (1.0 if i == 1 else 0.5), None, ALU.mult, None, [MOD_b], [AB_b])

    def norm_apply(src, src_b, t0, hT, hT_b, Acol, Bcol, xr, sq, rstd, rstd_b, tmpf, out_dram=None, out_b=None):
        ssb = [B.ps() for _ in range(NSUB)]
        NX = len(xr)

        def ldx(dc):
            xt, xb = xr[dc % NX]
            B.ld(xt, src[dc * 128:(dc + 1) * 128, t0:t0 + TT], [src_b], [xb])

        for dc in range(min(NX - 1, ND)):
            ldx(dc)
        for dc in range(ND):
            if dc + NX - 1 < ND:
                ldx(dc + NX - 1)
            xt, xb = xr[dc % NX]
            st, sb_ = sq[dc % len(sq)]
            B.act(st, xt, AF.Square, [xb], [sb_])
            for su in range(NSUB):
                B.mm(ssb[su][0][:, :], ones_bf[:], st[:, su * 512:(su + 1) * 512], dc == 0, dc == ND - 1,
                     [ones_b, sb_], [ssb[su][1]])
        for dc in range(min(NX - 1, ND)):
            ldx(dc)
        for su in range(NSUB):
            r = rstd[:, su * 512:(su + 1) * 512]
            B.act(r, ssb[su][0][:, :], AF.Sqrt, [ssb[su][1], EPS_b], [rstd_b], bias=EPS[:, 0:1], scale=1.0 / D)
            B.R.add('dve', lambda e, r=r: e.reciprocal(r, r), [rstd_b], [rstd_b])
        for dc in range(ND):
            if dc + NX - 1 < ND:
                ldx(dc + NX - 1)
            xt, xb = xr[dc % NX]
            for su in range(NSUB):
                tf, tfb = tmpf[(dc * NSUB + su) % len(tmpf)]
                sl = slice(su * 512, (su + 1) * 512)
                B.tt('dve', tf, xt[:, sl], rstd[:, sl], ALU.mult, [xb, rstd_b], [tfb])
                if out_dram is None:
                    B.act(hT[:, dc, sl], tf, AF.Identity, [tfb, AB_b], [hT_b[dc]],
                          bias=Bcol[:, dc:dc + 1], scale=Acol[:, dc:dc + 1])
                else:
                    B.act(tf, tf, AF.Identity, [tfb, GN_b], [tfb], scale=Acol[:, dc:dc + 1])
                    B.dma(out_dram[dc * 128:(dc + 1) * 128, t0 + su * 512:t0 + (su + 1) * 512], tf, [tfb], [out_b])

    def ffn_phase(src, src_b, dst, dst_b, ph, Wg, Wu, Wd, ntiles):
        B.phase()
        hT, _ = B.at([ND, TT], BF16)
        hT_b = [Buf() for _ in range(ND)]
        NFH = NF // c.FSPLIT
        aT, _ = B.at([NFH, TT], BF16)
        aT_b = [Buf() for _ in range(NFH)]
        xr = [B.at([TT], F32) for _ in range(4)]
        sq = [B.at([TT], BF16) for _ in range(2)]
        rstd, rstd_b = B.at([TT], F32)
        tmpf = [B.at([512], F32) for _ in range(4)]
        xo = [B.at([512], F32) for _ in range(4)]
        Acol, Bcol, Gcol = AB[:, ph, 0, :], AB[:, ph, 1, :], AB[:, ph, 2, :]
        specs, gu_i, dn_i = [], {}, {}
        for ti in range(ntiles):
            for fs in range(c.FSPLIT):
                for fb in range(NFH // 2):
                    col0 = (fs * NFH + fb * 2) * 128
                    gu_i[(ti, fs, fb)] = len(specs)
                    specs.append((Wg, 0, ND, col0, 256))
                    specs.append((Wu, 0, ND, col0, 256))
                for db_ in range(ND // 2):
                    dn_i[(ti, fs, db_)] = len(specs)
                    specs.append((Wd, fs * NFH, NFH, db_ * 256, 256))
        wp = WPipe(B, specs)
        wp.pump(0)
        for ti in range(ntiles):
            t0 = ti * TT
            norm_apply(src, src_b, t0, hT, hT_b, Acol, Bcol, xr, sq, rstd, rstd_b, tmpf)
            for fs in range(c.FSPLIT):
                for fb in range(NFH // 2):
                    bi = gu_i[(ti, fs, fb)]
                    ug = wp.get(bi)
                    uu = wp.get(bi + 1)
                    for cc in range(2):
                        fl = fb * 2 + cc
                        for su in range(NSUB):
                            sl = slice(su * 512, (su + 1) * 512)
                            pg, pgb = B.ps()
                            pu, pub = B.ps()
                            for kc in range(ND):
                                l, lb = B.wl(ug, kc, cc * 128)
                                B.mm(pg[:, :], l, hT[:, kc, sl], kc == 0, kc == ND - 1, [lb, hT_b[kc]], [pgb])
                            for kc in range(ND):
                                l, lb = B.wl(uu, kc, cc * 128)
                                B.mm(pu[:, :], l, hT[:, kc, sl], kc == 0, kc == ND - 1, [lb, hT_b[kc]], [pub])
                            tf, tfb = tmpf[(fl * NSUB + su) % len(tmpf)]
                            B.act(tf, pg[:, :], AF.Silu, [pgb], [tfb])
                            B.tt('dve', aT[:, fl, sl], tf, pu[:, :], ALU.mult, [tfb, pub], [aT_b[fl]])
                    wp.done(bi)
                    wp.done(bi + 1)
                xin, xin_b = (src, src_b) if fs == 0 else (S_tmp, db["tmp"])
                xout, xout_b = (dst, dst_b) if fs == c.FSPLIT - 1 else (S_tmp, db["tmp"])
                for db_ in range(ND // 2):
                    bi = dn_i[(ti, fs, db_)]
                    u = wp.get(bi)
                    for cc in range(2):
                        dc = db_ * 2 + cc
                        for su in range(NSUB):
                            sl = slice(t0 + su * 512, t0 + (su + 1) * 512)
                            xt, xtb = xo[(dc * NSUB + su) % len(xo)]
                            B.ld(xt, xin[dc * 128:(dc + 1) * 128, sl], [xin_b], [xtb])
                            pd, pdb = B.ps()
                            for kc in range(NFH):
                                l, lb = B.wl(u, kc, cc * 128)
                                B.mm(pd[:, :], l, aT[:, kc, su * 512:(su + 1) * 512], kc == 0, kc == NFH - 1,
                                     [lb, aT_b[kc]], [pdb])
                            B.stt(xt, pd[:, :], Gcol[:, dc:dc + 1], xt, ALU.mult, ALU.add, [pdb, xtb, AB_b], [xtb])
                            B.dma(xout[dc * 128:(dc + 1) * 128, sl], xt, [xtb], [xout_b])
                    wp.done(bi)

    ffn_phase(xT_ctx, Buf(), S_x1c, db["x1c"], 0, f1g, f1u, f1d, T // TT)
    ffn_phase(xT_own, Buf(), S_x1, db["x1"], 0, f1g, f1u, f1d, T // TT)

    def proj_phase(src, src_b, is_ctx):
        B.phase()
        hT, _ = B.at([ND, TT], BF16)
        hT_b = [Buf() for _ in range(ND)]
        xr = [B.at([TT], F32) for _ in range(4)]
        sq = [B.at([TT], BF16) for _ in range(2)]
        rstd, rstd_b = B.at([TT], F32)
        tmpf = [B.at([512], F32) for _ in range(4)]
        rp = [B.at([2, 512], F32) for _ in range(4)]
        xs = [B.at([512], F32) for _ in range(3)]
        ra = [B.at([512], F32) for _ in range(3)]
        rb = [B.at([512], F32) for _ in range(3)]
        ev = [B.at([512], BF16) for _ in range(4)]
        evi = [0]
        Acol, Bcol = AB[:, 1, 0, :], AB[:, 1, 1, :]
        loc0 = 0 if is_ctx else T
        for ti in range(T // TT):
            t0 = ti * TT
            norm_apply(src, src_b, t0, hT, hT_b, Acol, Bcol, xr, sq, rstd, rstd_b, tmpf)
            blocks = []
            if not is_ctx:
                for i in range(HA // 2):
                    blocks.append(("qa", c.oqa + i * 256, 256, i * 2))
            for i in range(HA // 2):
                blocks.append(("ka", c.oka + i * 256, 256, i * 2))
            for i in range(HA // 2):
                blocks.append(("va", c.ova + i * 256, 256, i * 2))
            if not is_ctx:
                for i in range(HB * 64 // 256):
                    blocks.append(("qb", c.oqb + i * 256, 256, i * 2))
            blocks.append(("kb", c.okb, 128, 0))
            blocks.append(("vb", c.ovb, 128, 0))
            if not is_ctx:
                for i in range(ND // 2):
                    blocks.append(("ga", c.oga + i * 256, 256, i * 2))
                for i in range(ND // 2):
                    blocks.append(("gb", c.ogb + i * 256, 256, i * 2))
            wp = WPipe(B, [(w_in, 0, ND, b[1], b[2]) for b in blocks])
            ropeslices = {}
            for su in range(NSUB):
                for which, tab in ((0, ropeA), (1, ropeB)):
                    r_, rb_ = rp[su * 2 + which] if NSUB * 2 <= len(rp) else rp[which]
                    lo = loc0 + t0 + su * 512
                    B.ld(r_, tab[:, :, lo:lo + 512].rearrange("a p n -> p a n"), [], [rb_])
                    ropeslices[(su, which)] = (r_, rb_)
            for bi, (kind, col0, ncols, ch0) in enumerate(blocks):
                u = wp.get(bi)
                if kind in ("va", "vb"):
                    for tk in range(TT // 128):
                        pv, pvb = B.ps()
                        for kc in range(ND):
                            r_, rb_ = B.wl(u, kc, 0, ncols)
                            B.mm(pv[:, 0:ncols], hT[:, kc, tk * 128:(tk + 1) * 128], r_, kc == 0, kc == ND - 1,
                                 [rb_, hT_b[kc]], [pvb])
                        e, eb = ev[evi[0] % len(ev)]
                        evi[0] += 1
                        B.cp('act', e[:, 0:ncols], pv[:, 0:ncols], [pvb], [eb])
                        tok = loc0 + t0 + tk * 128
                        if kind == "va":
                            B.dma(VA_d[tok:tok + 128, col0 - c.ova:col0 - c.ova + ncols], e[:, 0:ncols], [eb], [db["va"]])
                        else:
                            B.dma(VB_d[tok:tok + 128, 0:ncols], e[:, 0:ncols], [eb], [db["vb"]])
                    wp.done(bi)
                    continue
                for cc in range(ncols // 128):
                    ch = ch0 + cc
                    for su in range(NSUB):
                        sl = slice(su * 512, (su + 1) * 512)
                        pp, ppb = B.ps()
                        for kc in range(ND):
                            l, lb = B.wl(u, kc, cc * 128)
                            B.mm(pp[:, :], l, hT[:, kc, sl], kc == 0, kc == ND - 1, [lb, hT_b[kc]], [ppb])
                        tok = t0 + su * 512
                        if kind in ("ga", "gb"):
                            e, eb = ev[evi[0] % len(ev)]
                            evi[0] += 1
                            B.act(e, pp[:, :], AF.Sigmoid, [ppb], [eb])
                            dd, ddb = (SGA_d, db["sga"]) if kind == "ga" else (SGB_d, db["sgb"])
                            B.dma(dd[ch * 128:(ch + 1) * 128, tok:tok + 512], e, [eb], [ddb])
                            continue
                        isA = kind in ("qa", "ka")
                        rt, rtb = ropeslices[(su, 0 if isA else 1)]
                        x_, xb_ = xs[evi[0] % 3]
                        a_, ab_ = ra[evi[0] % 3]
                        b_, bb_ = rb[evi[0] % 3]
                        B.cp('act', x_, pp[:, :], [ppb], [xb_])
                        B.tt('pool', a_, x_, rt[:, 0, :], ALU.mult, [xb_, rtb], [ab_])
                        hs = 64 if isA else 32
                        for g0 in range(0, 128, 2 * hs):
                            B.tt('pool', b_[g0:g0 + hs, :], x_[g0 + hs:g0 + 2 * hs, :], rt[g0 + hs:g0 + 2 * hs, 1, :],
                                 ALU.mult, [xb_, rtb], [bb_])
                            B.tt('pool', b_[g0 + hs:g0 + 2 * hs, :], x_[g0:g0 + hs, :], rt[g0:g0 + hs, 1, :],
                                 ALU.mult, [xb_, rtb], [bb_])
                        e, eb = ev[evi[0] % len(ev)]
                        evi[0] += 1
                        if isA:
                            B.tt('dve', e, a_, b_, ALU.add, [ab_, bb_], [eb])
                            if kind == "qa":
                                B.dma(QA_d[ch * 128:(ch + 1) * 128, tok:tok + 512], e, [eb], [db["qa"]])
                            else:
                                B.dma(KA_d[ch * 128:(ch + 1) * 128, loc0 + tok:loc0 + tok + 512], e, [eb], [db["ka"]])
                        else:
                            B.tt('dve', e[0:64, 0:512], a_[0:64, :], b_[0:64, :], ALU.add, [ab_, bb_], [eb])
                            e2, eb2 = ev[evi[0] % len(ev)]
                            evi[0] += 1
                            B.tt('dve', e2[0:64, 0:512], a_[64:128, :], b_[64:128, :], ALU.add, [ab_, bb_], [eb2])
                            for hh, (ee, eeb) in enumerate(((e, eb), (e2, eb2))):
                                if kind == "qb":
                                    head = ch * 2 + hh
                                    B.dma(QB_d[:, tok // 128:tok // 128 + 4, head, :],
                                          ee[0:64, 0:512].rearrange("p (a b) -> p a b", a=4), [eeb], [db["qb"]])
                                else:
                                    B.dma(KB_d[:, hh, loc0 + tok:loc0 + tok + 512], ee[0:64, 0:512], [eeb], [db["kb"]])
                wp.done(bi)

    proj_phase(S_x1c, db["x1c"], True)
    proj_phase(S_x1, db["x1"], False)

    B.phase()
    scaleA = 128.0 ** -0.5
    KT = [B.at([T2], BF16) for _ in range(2)]
    V1 = [B.at([NKT, 132], BF16) for _ in range(2)]
    QT = [B.at([T], BF16) for _ in range(2)]
    kmf, kmf_b = B.at([NKB], F32)
    kmT, kmT_b = B.at([NKB], BF16)
    gsm = [B.at([NKB], F32) for _ in range(2)]
    top8 = [B.at([8], F32) for _ in range(2)]
    SEL, SEL_b = B.at([NQT, NKB], F32)
    Pt = [B.at([256], BF16) for _ in range(6)]
    acc = [B.at([132], F32) for _ in range(4)]
    rec = [B.at([1], F32) for _ in range(2)]
    yt = [B.at([128], F32) for _ in range(2)]
    ytT = [B.at([256], BF16) for _ in range(2)]
    for vv, vb_ in V1:
        B.memset('pool', vv[:, :, 128:129], 1.0, [vb_])
    pti = [0]
    VBb = VBI[:, 0:NQT * NKB].rearrange("p (a b) -> p a b", a=NQT)
    VBv = VBI[:, NQT * NKB:2 * NQT * NKB].rearrange("p (a b) -> p a b", a=NQT)

    def load_head(h):
        kt, ktb = KT[h % 2]
        v1, v1b = V1[h % 2]
        qt, qtb = QT[h % 2]
        B.ld(kt, KA_d[h * 128:(h + 1) * 128, :], [db["ka"]], [ktb])
        B.ld(v1[:, :, 0:128], VA_d[:, h * 128:(h + 1) * 128].rearrange("(n p) d -> p n d", p=128), [db["va"]], [v1b])
        B.ld(qt, QA_d[h * 128:(h + 1) * 128, :], [db["qa"]], [qtb])

    load_head(0)
    for h in range(HA):
        if h + 1 < HA:
            load_head(h + 1)
        kt, ktb = KT[h % 2]
        v1, v1b = V1[h % 2]
        qt, qtb = QT[h % 2]
        B.R.add('dve', lambda e, kt=kt: e.tensor_reduce(kmf, kt.rearrange("p (n k) -> p n k", k=256), AX.X, ALU.add),
                [ktb], [kmf_b])
        B.ts('dve', kmT, kmf, 1.0 / 256, None, ALU.mult, None, [kmf_b], [kmT_b])
        for q in range(NQT):
            pg, pgb = B.ps()
            B.mm(pg[:, 0:NKB], qt[:, q * 128:(q + 1) * 128], kmT, True, True, [qtb, kmT_b], [pgb])
            g_, gb_ = gsm[q % 2]
            t8, t8b = top8[q % 2]
            B.tt('dve', g_, pg[:, 0:NKB], VBb[:, q, :], ALU.add, [pgb, VBI_b], [gb_])
            B.R.add('dve', lambda e, t8=t8, g_=g_: e.max(t8, g_), [gb_], [t8b])
            B.ts('dve', g_, g_, t8[:, c.TOPK - 1:c.TOPK], None, ALU.is_ge, None, [gb_, t8b], [gb_])
            B.tt('dve', SEL[:, q, :], g_, VBv[:, q, :], ALU.mult, [gb_, VBI_b], [SEL_b])
        for j in range(T // 256):
            nb = T // 256 + j
            q0 = j * 256
            a0, a0b = acc[(2 * j) % 4]
            a1, a1b = acc[(2 * j + 1) % 4]
            accs = ((a0, a0b), (a1, a1b))
            k0 = 2 * nb
            s0, s0b = B.ps()
            B.mm(s0[:, 0:256], kt[:, k0 * 128:(k0 + 1) * 128], qt[:, q0:q0 + 256], True, True, [ktb, qtb], [s0b])
            p0, p0b = Pt[pti[0] % 6]; pti[0] += 1
            B.act(p0, s0[:, 0:256], AF.Exp, [s0b], [p0b], scale=scaleA)
            B.tt('pool', p0[:, 0:128], p0[:, 0:128], TRI, ALU.mult, [p0b, consts_b], [p0b])
            s1, s1b = B.ps()
            B.mm(s1[:, 0:128], kt[:, (k0 + 1) * 128:(k0 + 2) * 128], qt[:, q0 + 128:q0 + 256], True, True, [ktb, qtb], [s1b])
            p1, p1b = Pt[pti[0] % 6]; pti[0] += 1
            B.act(p1[:, 0:128], s1[:, 0:128], AF.Exp, [s1b], [p1b], scale=scaleA)
            B.tt('pool', p1[:, 0:128], p1[:, 0:128], TRI, ALU.mult, [p1b, consts_b], [p1b])
            o0, o0b = B.ps()
            B.mm(o0[:, 0:129], p0[:, 0:128], v1[:, k0, 0:129], True, True, [p0b, v1b], [o0b])
            B.cp('dve', a0[:, 0:129], o0[:, 0:129], [o0b], [a0b])
            o1, o1b = B.ps()
            B.mm(o1[:, 0:129], p0[:, 128:256], v1[:, k0, 0:129], True, False, [p0b, v1b], [o1b])
            B.mm(o1[:, 0:129], p1[:, 0:128], v1[:, k0 + 1, 0:129], False, True, [p1b, v1b], [o1b])
            B.cp('dve', a1[:, 0:129], o1[:, 0:129], [o1b], [a1b])
            for n in range(nb):
                pk = []
                sp_, spb = B.ps()
                for kk in range(2):
                    ktile = 2 * n + kk
                    B.mm(sp_[:, kk * 256:(kk + 1) * 256], kt[:, ktile * 128:(ktile + 1) * 128], qt[:, q0:q0 + 256], True, True,
                         [ktb, qtb], [spb])
                for kk in range(2):
                    pp_, ppb = Pt[pti[0] % 6]; pti[0] += 1
                    B.act(pp_, sp_[:, kk * 256:(kk + 1) * 256], AF.Exp, [spb], [ppb], scale=scaleA)
                    pk.append((pp_, ppb, 2 * n + kk))
                oo, oob = B.ps()
                for qs in range(2):
                    for kk in range(2):
                        pp_, ppb, ktile = pk[kk]
                        B.mm(oo[:, qs * 256:qs * 256 + 129], pp_[:, qs * 128:(qs + 1) * 128], v1[:, ktile, 0:129], kk == 0, kk == 1,
                             [ppb, v1b], [oob])
                for qs in range(2):
                    aa, aab = accs[qs]
                    B.stt(aa[:, 0:129], oo[:, qs * 256:qs * 256 + 129], SEL[:, 2 * j + qs, n:n + 1], aa[:, 0:129], ALU.mult, ALU.add,
                          [oob, SEL_b, aab], [aab])
            yT, yTb = ytT[j % 2]
            for qs in range(2):
                aa, aab = accs[qs]
                r_, rb_ = rec[qs]
                y_, yb_ = yt[qs]
                B.R.add('dve', lambda e, r_=r_, aa=aa: e.reciprocal(r_, aa[:, 128:129]), [aab], [rb_])
                B.ts('dve', y_, aa[:, 0:128], r_[:, 0:1], None, ALU.mult, None, [aab, rb_], [yb_])
                pt_, ptb = B.ps()
                B.tr(pt_[:, 0:128], y_, IDENT, [yb_, consts_b], [ptb])
                B.cp('act', yT[:, qs * 128:(qs + 1) * 128], pt_[:, 0:128], [ptb], [yTb])
            B.dma(YA_d[h * 128:(h + 1) * 128, q0:q0 + 256], yT, [yTb], [db["ya"]])

    B.phase()
    scaleB = 64.0 ** -0.5
    KBT, KBT_b = B.at([KVB, T2], BF16)
    VB1, VB1_b = B.at([NKT, KVB, 66], BF16)
    QBT = [B.at([HB, 128], BF16) for _ in range(2)]
    Pb = [B.at([512], BF16) for _ in range(4)]
    den = [B.at([4], F32) for _ in range(2)]
    ybt = [B.at([HB * 64], F32) for _ in range(2)]
    ybT = [B.at([HB * 64 // 128, 128], BF16) for _ in range(2)]
    B.dma(KBT[0:64], KB_d[:, :, :], [db["kb"]], [KBT_b])
    B.memset('pool', VB1[:, :, :, 64:65], 1.0, [VB1_b])
    for g in range(KVB):
        B.dma(VB1[:, :, g, 0:64], VB_d[:, g * 64:(g + 1) * 64].rearrange("(n p) d -> p n d", p=128), [db["vb"]], [VB1_b])
    HG = min(4, G)
    pbi = [0]
    B.ld(QBT[0][0][0:64], QB_d[:, 0, :, :], [db["qb"]], [QBT[0][1]])
    for i in range(NQT):
        qb_, qbb = QBT[i % 2]
        if i + 1 < NQT:
            B.ld(QBT[(i + 1) % 2][0][0:64], QB_d[:, i + 1, :, :], [db["qb"]], [QBT[(i + 1) % 2][1]])
        cur = T // 128 + i
        prv = cur - 1
        y_, yb_ = ybt[i % 2]
        for g in range(KVB):
            for hg in range(G // HG):
                h0 = g * G + hg * HG
                rhs = qb_[0:64, h0:h0 + HG, :]
                ps_ = []
                for (ktile, msk) in ((prv, MASKP0 if i == 0 else MASKP), (cur, TRI)):
                    sp_, spb = B.ps()
                    B.mm(sp_[:, 0:HG * 128], KBT[0:64, g, ktile * 128:(ktile + 1) * 128], rhs, True, True, [KBT_b, qbb], [spb])
                    pp_, ppb = Pb[pbi[0] % 4]; pbi[0] += 1
                    B.act(pp_[:, 0:HG * 128], sp_[:, 0:HG * 128], AF.Exp, [spb], [ppb], scale=scaleB)
                    for hh in range(HG):
                        B.tt('pool', pp_[:, hh * 128:(hh + 1) * 128], pp_[:, hh * 128:(hh + 1) * 128], msk, ALU.mult,
                             [ppb, consts_b], [ppb])
                    ps_.append((pp_, ppb, ktile))
                oo, oob = B.ps()
                for hh in range(HG):
                    for kk in range(2):
                        pp_, ppb, ktile = ps_[kk]
                        B.mm(oo[:, hh * 65:hh * 65 + 65], pp_[:, hh * 128:(hh + 1) * 128], VB1[:, ktile, g, 0:65],
                             kk == 0, kk == 1, [ppb, VB1_b], [oob])
                d_, db_ = den[(g * (G // HG) + hg) % 2]
                ov = oo[:, 0:HG * 65].rearrange("p (h d) -> p h d", h=HG)
                B.tt('dve', d_[:, 0:HG], ov[:, :, 64], ESK[:, h0:h0 + HG], ALU.add, [oob, ESK_b], [db_])
                B.R.add('dve', lambda e, d_=d_: e.reciprocal(d_[:, 0:HG], d_[:, 0:HG]), [db_], [db_])
                for hh in range(HG):
                    B.ts('dve', y_[:, (h0 + hh) * 64:(h0 + hh + 1) * 64], ov[:, hh, 0:64], d_[:, hh:hh + 1], None,
                         ALU.mult, None, [oob, db_], [yb_])
        yT, yTb = ybT[i % 2]
        for ch in range(HB * 64 // 128):
            pt_, ptb = B.ps()
            B.tr(pt_[:, 0:128], y_[:, ch * 128:(ch + 1) * 128], IDENT, [yb_, consts_b], [ptb])
            B.cp('act', yT[:, ch, :], pt_[:, 0:128], [ptb], [yTb])
        B.dma(YB_d[:, i * 128:(i + 1) * 128].rearrange("(c p) n -> p c n", p=128), yT, [yTb], [db["yb"]])

    B.phase()
    NCA, NCB = HA, HB * 64 // 128
    yaT, yaT_b = B.at([NCA, TT], BF16)
    ybT2, ybT2_b = B.at([NCB, TT], BF16)
    mT, _ = B.at([ND, TT], BF16)
    mT_b = [Buf() for _ in range(ND)]
    sg = [B.at([2, 512], BF16) for _ in range(3)]
    tm = [B.at([2, 512], F32) for _ in range(3)]
    xo = [B.at([512], F32) for _ in range(4)]
    Gcol = AB[:, 1, 2, :]
    k_i = [0]
    for ti in range(T // TT):
        t0 = ti * TT
        B.ld(yaT, YA_d[:, t0:t0 + TT].rearrange("(c p) n -> p c n", p=128), [db["ya"]], [yaT_b])
        B.ld(ybT2, YB_d[:, t0:t0 + TT].rearrange("(c p) n -> p c n", p=128), [db["yb"]], [ybT2_b])
        specs = []
        for d2 in range(ND // 2):
            specs.append((w_bm, 0, NCA, d2 * 256, 256))
            specs.append((w_bs, 0, NCB, d2 * 256, 256))
        wp = WPipe(B, specs)
        for d2 in range(ND // 2):
            um = wp.get(2 * d2)
            us = wp.get(2 * d2 + 1)
            for cc in range(2):
                dc = d2 * 2 + cc
                for su in range(NSUB):
                    sl = slice(su * 512, (su + 1) * 512)
                    tok = t0 + su * 512
                    s_, sb_ = sg[k_i[0] % 3]
                    t_, tb_ = tm[k_i[0] % 3]
                    k_i[0] += 1
                    B.ld(s_[:, 0, :], SGA_d[dc * 128:(dc + 1) * 128, tok:tok + 512], [db["sga"]], [sb_])
                    B.ld(s_[:, 1, :], SGB_d[dc * 128:(dc + 1) * 128, tok:tok + 512], [db["sgb"]], [sb_])
                    pm, pmb = B.ps()
                    pq, pqb = B.ps()
                    for kc in range(NCA):
                        l, lb = B.wl(um, kc, cc * 128)
                        B.mm(pm[:, :], l, yaT[:, kc, sl], kc == 0, kc == NCA - 1, [lb, yaT_b], [pmb])
                    for kc in range(NCB):
                        l, lb = B.wl(us, kc, cc * 128)
                        B.mm(pq[:, :], l, ybT2[:, kc, sl], kc == 0, kc == NCB - 1, [lb, ybT2_b], [pqb])
                    B.tt('dve', t_[:, 0, :], pm[:, :], s_[:, 0, :], ALU.mult, [pmb, sb_], [tb_])
                    B.tt('dve', t_[:, 1, :], pq[:, :], s_[:, 1, :], ALU.mult, [pqb, sb_], [tb_])
                    B.tt('pool', mT[:, dc, sl], t_[:, 0, :], t_[:, 1, :], ALU.add, [tb_], [mT_b[dc]])
            wp.done(2 * d2)
            wp.done(2 * d2 + 1)
        specs = [(w_out, 0, ND, d2 * 256, 256) for d2 in range(ND // 2)]
        wp = WPipe(B, specs)
        for d2 in range(ND // 2):
            u = wp.get(d2)
            for cc in range(2):
                dc = d2 * 2 + cc
                for su in range(NSUB):
                    sl = slice(t0 + su * 512, t0 + (su + 1) * 512)
                    xt, xtb = xo[(dc * NSUB + su) % len(xo)]
                    B.ld(xt, S_x1[dc * 128:(dc + 1) * 128, sl], [db["x1"]], [xtb])
                    pd, pdb = B.ps()
                    for kc in range(ND):
                        l, lb = B.wl(u, kc, cc * 128)
                        B.mm(pd[:, :], l, mT[:, kc, su * 512:(su + 1) * 512], kc == 0, kc == ND - 1, [lb, mT_b[kc]], [pdb])
                    B.stt(xt, pd[:, :], Gcol[:, dc:dc + 1], xt, ALU.mult, ALU.add, [pdb, xtb, AB_b], [xtb])
                    B.dma(S_x2[dc * 128:(dc + 1) * 128, sl], xt, [xtb], [db["x2"]])
            wp.done(d2)

    ffn_phase(S_x2, db["x2"], S_x3, db["x3"], 2, f2g, f2u, f2d, T // TT)
    B.phase()
    xr = [B.at([TT], F32) for _ in range(4)]
    sq = [B.at([TT], BF16) for _ in range(2)]
    rstd, rstd_b = B.at([TT], F32)
    tmpf = [B.at([512], F32) for _ in range(4)]
    for ti in range(T // TT):
        norm_apply(S_x3, db["x3"], ti * TT, None, None, GN[:, 3 * ND:4 * ND], None, xr, sq, rstd, rstd_b, tmpf,
                   out_dram=outT, out_b=db["out"])
    B.R.finish(nc)
    return nc


def rope_tables(cfg, half):
    T = cfg.T
    pos_ctx = np.arange(0, T, dtype=np.float64)
    pos_own = np.arange(half * T, (half + 1) * T, dtype=np.float64)
    pos = np.concatenate([pos_ctx, pos_own])
    out = []
    for hs in (64, 32):
        inv = 10000.0 ** (-np.arange(hs, dtype=np.float64) / hs)
        ang = (pos.astype(np.float32)[None, :] * inv.astype(np.float32)[:, None]).astype(np.float32)
        cos = np.cos(ang).astype(np.float32)
        sin = np.sin(ang).astype(np.float32)
        reps = 128 // (2 * hs)
        cosT = np.concatenate([cos, cos] * reps, axis=0)
        sinT = np.concatenate([sin, -sin] * reps, axis=0)
        out.append(np.stack([cosT, sinT]).astype(np.float32))
    return out


def const_masks(cfg, half):
    k = np.arange(128)[:, None]
    q = np.arange(128)[None, :]
    tri = (k <= q).astype(np.float32)
    mp = (k > q).astype(np.float32)
    mp0 = mp * float(half)
    ident = np.eye(128, dtype=np.float32)
    cm = np.concatenate([tri, mp, mp0, ident], axis=1)
    NQT, NKB = cfg.NQT, cfg.NKB
    valid = np.zeros((NQT, NKB), np.float32)
    for qt in range(NQT):
        j = qt // 2
        for n in range(NKB):
            if n < NKB // 2:
                valid[qt, n] = float(half)
            elif n < NKB // 2 + j:
                valid[qt, n] = 1.0
    bias = np.where(valid > 0, 0.0, NEG).astype(np.float32)
    vb = np.concatenate([bias.reshape(-1), valid.reshape(-1)])[None, :].repeat(128, 0).astype(np.float32)
    return cm, vb


def colT(v, n):
    return np.ascontiguousarray(np.asarray(v, np.float32).reshape(n, 128).T)


def make_in_maps(cfg, inp):
    D, ND, T = cfg.D, cfg.ND, cfg.T
    f = lambda a: np.ascontiguousarray(np.asarray(a, np.float32))
    x = np.asarray(inp["x"], np.float32)
    shared = {
        "w_ada": f(inp["w_ada"][0]), "b_adaT": colT(inp["b_ada"][0], 9 * ND),
        "gains": np.concatenate([colT(inp["norm_ffn1"][0], ND), colT(inp["norm_mix"][0], ND),
                                 colT(inp["norm_ffn2"][0], ND), colT(inp["norm_final"], ND)], axis=1),
        "f1g": f(inp["ffn1_gate"][0]), "f1u": f(inp["ffn1_up"][0]), "f1d": f(inp["ffn1_down"][0]),
        "w_in": f(inp["w_in"][0]),
        "sinks": np.ascontiguousarray(np.broadcast_to(np.asarray(inp["swa_sinks"][0], np.float32)[None, :], (128, cfg.HB))),
        "w_bm": f(inp["w_branch_moba"][0]), "w_bs": f(inp["w_branch_swa"][0]), "w_out": f(inp["w_out"][0]),
        "f2g": f(inp["ffn2_gate"][0]), "f2u": f(inp["ffn2_up"][0]), "f2d": f(inp["ffn2_down"][0]),
    }
    maps = []
    for core in range(2 * cfg.BATCH):
        b, half = core // 2, core % 2
        rA, rB = rope_tables(cfg, half)
        cm, vb = const_masks(cfg, half)
        m = dict(shared)
        m["xT_own"] = np.ascontiguousarray(x[b, half * T:(half + 1) * T, :].T)
        m["xT_ctx"] = np.ascontiguousarray(x[b, 0:T, :].T)
        m["cT"] = colT(inp["c"][b], ND)
        m["ropeA"], m["ropeB"], m["cmask"], m["vbias"] = rA, rB, cm, vb
        maps.append(m)
    return maps


def assemble(cfg, results):
    out = np.empty((cfg.BATCH, cfg.SEQ, cfg.D), np.float32)
    for core, r in enumerate(results):
        b, half = core // 2, core % 2
        out[b, half * cfg.T:(half + 1) * cfg.T, :] = np.asarray(r["outT"], np.float32).T
    return out


def kernel(**inputs):
    cfg = Cfg()
    nc = build(cfg)
    maps = make_in_maps(cfg, inputs)
    res = run_bass_kernel_spmd(nc, maps, core_ids=list(range(8)))
    return assemble(cfg, res.results)
```

```python
import numpy as np
import ml_dtypes
import concourse.bass as bass
import concourse.mybir as mybir
from concourse.bass_utils import run_bass_kernel_spmd

F32 = mybir.dt.float32
BF16 = mybir.dt.bfloat16
ALU = mybir.AluOpType
AF = mybir.ActivationFunctionType
AX = mybir.AxisListType
NEG = -1.0e30


class Cfg:
    def __init__(s, D=2048, FF=5632, HA=8, HB=16, KVB=2, SEQ=4096, BATCH=4, TT=1024, FSPLIT=2):
        s.D, s.FF, s.HA, s.HB, s.KVB, s.SEQ, s.BATCH, s.TT, s.FSPLIT = D, FF, HA, HB, KVB, SEQ, BATCH, TT, FSPLIT
        s.T = SEQ // 2
        s.ND = D // 128
        s.NF = FF // 128
        s.BLK, s.W, s.TOPK = 256, 128, 3
        s.G = HB // KVB
        s.oqa = 0
        s.oka = HA * 128
        s.ova = 2 * HA * 128
        s.oqb = 3 * HA * 128
        s.okb = s.oqb + HB * 64
        s.ovb = s.okb + KVB * 64
        s.oga = s.ovb + KVB * 64
        s.ogb = s.oga + D
        s.INC = s.ogb + D
        s.NSUB = TT // 512
        s.NQT = s.T // 128
        s.NKB = 2 * s.T // 256
        s.NKT = 2 * s.T // 128
        assert KVB == 2 and s.T % TT == 0 and TT % 512 == 0 and s.NF % FSPLIT == 0


class Buf:
    __slots__ = ("n", "w", "r")

    def __init__(s, n=""):
        s.n = n
        s.w = {}
        s.r = {}


class Op:
    __slots__ = ("eng", "fn", "deps", "sig", "val", "dsem", "dval", "waits", "isdma")


class Rec:
    NDS = 24
    QPOOL = {'sp': (0, 16), 'act': (16, 8)}

    def __init__(s):
        s.ops = []
        s.last = {}
        s.bar = None
        s.bar_done = set()
        s.ndma = 0
        s.dma_last = {}
        s.qcnt = {}

    def add(s, eng, fn, reads=(), writes=(), dma=False):
        op = Op()
        op.eng, op.fn, op.isdma, op.sig, op.deps = eng, fn, dma, False, set()
        op.val = op.dsem = op.dval = None
        for b in reads:
            for k, v in b.w.items():
                if k == 'dma':
                    op.deps.update(v)
                elif k == eng and not dma and eng == 'pe':
                    pass
                else:
                    op.deps.add(v)
        for b in writes:
            for k, v in b.r.items():
                if k == 'dma':
                    op.deps.update(v)
                elif k == eng and not dma:
                    pass
                else:
                    op.deps.add(v)
            for k, v in b.w.items():
                if k == 'dma':
                    op.deps.update(v)
                elif k == eng and not dma:
                    pass
                else:
                    op.deps.add(v)
        if s.bar is not None and eng not in s.bar_done:
            op.deps.update(s.bar)
            s.bar_done.add(eng)
        if dma:
            base, cnt_ = s.QPOOL[eng]
            nq = s.qcnt.get(eng, 0)
            s.qcnt[eng] = nq + 1
            op.dsem = base + nq % cnt_
            op.dval = 16 * (nq // cnt_ + 1)
            s.ndma += 1
            prev = s.dma_last.get(op.dsem)
            if prev is not None:
                op.deps.add(prev)
            s.dma_last[op.dsem] = op
            op.sig = True
        for b in reads:
            if dma:
                b.r.setdefault('dma', []).append(op)
            else:
                b.r[eng] = op
        for b in writes:
            if b.r:
                b.w = {}
                b.r = {}
            if dma:
                b.w.setdefault('dma', []).append(op)
            else:
                b.w[eng] = op
        if not dma:
            s.last[eng] = op
        s.ops.append(op)
        return op

    def barrier(s):
        s.bar = set(s.last.values()) | set(s.dma_last.values())
        s.bar_done = set()

    def finish(s, nc):
        for op in s.ops:
            for d in op.deps:
                d.sig = True
        cnt = {}
        for op in s.ops:
            if op.isdma:
                continue
            if op.sig:
                cnt[op.eng] = cnt.get(op.eng, 0) + 1
                op.val = cnt[op.eng]
        engs = ['pe', 'act', 'dve', 'pool', 'sp']
        esem = {e: nc.alloc_semaphore("sem_" + e) for e in engs if e != 'sp'}
        dsem = [nc.alloc_semaphore("dsem%d" % i) for i in range(s.NDS)]
        per = {e: [] for e in engs}
        for op in s.ops:
            per[op.eng].append(op)
        for e in engs:
            waited = {}
            for op in per[e]:
                need = {}
                for d in op.deps:
                    if d.isdma:
                        k, v = ('d', d.dsem), d.dval
                    else:
                        k, v = ('e', d.eng), d.val
                    if v > need.get(k, 0):
                        need[k] = v
                op.waits = []
                for k, v in need.items():
                    if v > waited.get(k, 0):
                        waited[k] = v
                        op.waits.append((k, v))
        dfinal = {}
        for op in s.ops:
            if op.isdma:
                dfinal[op.dsem] = op.dval

        def body_for(e):
            def body(eng):
                for op in per[e]:
                    for (k, v) in op.waits:
                        eng.wait_ge(dsem[k[1]] if k[0] == 'd' else esem[k[1]], v)
                    ins = op.fn(eng)
                    if op.isdma:
                        ins.then_inc(dsem[op.dsem], 16)
                    elif op.sig:
                        ins.then_inc(esem[e], 1)
                if e == 'sp':
                    for k, v in dfinal.items():
                        eng.wait_ge(dsem[k], v)
                    for e2 in esem:
                        if cnt.get(e2, 0) > 0:
                            eng.wait_ge(esem[e2], cnt[e2])
            return body

        with nc.Block() as blk:
            blk.tensor(body_for('pe'))
            blk.scalar(body_for('act'))
            blk.vector(body_for('dve'))
            blk.gpsimd(body_for('pool'))
            blk.sync(body_for('sp'))


class Builder:
    def __init__(s, cfg):
        s.c = cfg
        s.nc = bass.Bass("TRN2", target_bir_lowering=False)
        s.R = Rec()
        s.dram = {}
        s.nbuf = 0
        s.cast_rr = 0

    def din(s, name, shape, dt=F32):
        t = s.nc.dram_tensor(name, list(shape), dt, kind="ExternalInput").ap()
        s.dram[name] = t
        return t

    def dscr(s, name, shape, dt):
        return s.nc.dram_tensor(name, list(shape), dt, kind="Internal").ap()

    def buf(s, n=""):
        return Buf(n)

    def mm(s, out, lhsT, rhs, start, stop, reads, writes):
        s.R.add('pe', lambda e: e.matmul(out, lhsT, rhs, start=start, stop=stop), reads, writes)

    def tr(s, out, in_, ident, reads, writes):
        s.R.add('pe', lambda e: e.transpose(out, in_, ident), reads, writes)

    def dma(s, out, in_, reads, writes, q='sp'):
        s.R.add(q, lambda e: e.dma_start(out=out, in_=in_), reads, writes, dma=True)

    def ld(s, out, in_, reads, writes):
        s.dma(out, in_, reads, writes, q='act')

    def act(s, out, in_, func, reads, writes, bias=None, scale=None, accum_out=None):
        kw = {}
        if bias is not None:
            kw['bias'] = bias
        if scale is not None:
            kw['scale'] = scale
        if accum_out is not None:
            kw['accum_out'] = accum_out
        s.R.add('act', lambda e: e.activation(out, in_, func, **kw), reads, writes)

    def tt(s, eng, out, in0, in1, op, reads, writes):
        s.R.add(eng, lambda e: e.tensor_tensor(out, in0, in1, op), reads, writes)

    def ts(s, eng, out, in0, s1, s2, op0, op1, reads, writes):
        if op1 is None:
            s.R.add(eng, lambda e: e.tensor_scalar(out, in0, s1, None, op0), reads, writes)
        else:
            s.R.add(eng, lambda e: e.tensor_scalar(out, in0, s1, s2, op0, op1), reads, writes)

    def stt(s, out, in0, scalar, in1, op0, op1, reads, writes):
        s.R.add('dve', lambda e: e.scalar_tensor_tensor(out, in0, scalar, in1, op0, op1), reads, writes)

    def cp(s, eng, out, in_, reads, writes):
        if eng == 'act':
            s.R.add('act', lambda e: e.activation(out, in_, AF.Copy), reads, writes)
        else:
            s.R.add(eng, lambda e: e.tensor_copy(out, in_), reads, writes)

    def memset(s, eng, ap, val, writes):
        s.R.add(eng, lambda e: e.memset(ap, val), (), writes)

    def init_psum(s):
        s.psb = [(s.nc.alloc_psum_tensor("psb%d" % i, [128, 512], F32), Buf("ps%d" % i)) for i in range(8)]
        s.psi = 0

    def ps(s):
        t = s.psb[s.psi % 8]
        s.psi += 1
        return t

    def init_arena(s, nbytes):
        s.arena = s.nc.alloc_sbuf_tensor("arena", [128, nbytes // 4], F32)
        s.arena_n = nbytes // 4
        s.aoff = 0

    def phase(s):
        s.R.barrier()
        s.aoff = 0

    def at(s, shape, dt):
        n = int(np.prod(shape))
        nw = (n * (2 if dt == BF16 else 4) + 3) // 4
        nw = (nw + 7) // 8 * 8
        assert s.aoff + nw <= s.arena_n, ("arena overflow", s.aoff, nw, s.arena_n)
        ap = s.arena[:, s.aoff:s.aoff + nw]
        s.aoff += nw
        if dt == BF16:
            ap = ap.bitcast(BF16)
        ap = ap[:, 0:n]
        if len(shape) == 2:
            ap = ap.rearrange("p (a b) -> p a b", a=shape[0])
        elif len(shape) == 3:
            ap = ap.rearrange("p (a b c) -> p a b c", a=shape[0], b=shape[1])
        return ap, Buf()

    def init_wstream(s, nslot=12, nstage=4):
        nc = s.nc
        s.wring = [(nc.alloc_sbuf_tensor("wr%d" % i, [128, 8, 256], BF16), Buf()) for i in range(nslot)]
        s.wstage = [(nc.alloc_sbuf_tensor("ws%d" % i, [128, 8, 256], F32), Buf()) for i in range(nstage)]
        s.wri = 0
        s.wsi = 0
        s.wring_owner = [None] * nslot
        s.wblk_id = 0

    def wl(s, units, kc, c0, n=128):
        u = units[kc // 8]
        return u[0][:, kc % 8, c0:c0 + n], u[1]


class WPipe:
    AHEAD = 2

    def __init__(s, B, specs):
        s.B, s.specs = B, specs
        s.units, s.bunits = [], []
        for bi, (W, r0, nk, col0, ncols) in enumerate(specs):
            k, us = 0, []
            while k < nk:
                nku = min(8, nk - k)
                us.append(len(s.units))
                s.units.append((bi, W, r0 + k, nku, col0, ncols))
                k += nku
            s.bunits.append(us)
        s.nd = s.ncs = 0
        s.st, s.rg = {}, {}

    def pump(s, i):
        B = s.B
        NST = len(B.wstage)
        while True:
            if s.nd < len(s.units) and s.nd < s.ncs + NST:
                bi, W, r, nku, col0, ncols = s.units[s.nd]
                st, stb = B.wstage[B.wsi % NST]
                B.wsi += 1
                src = W[r * 128:(r + nku) * 128, col0:col0 + ncols].rearrange("(c p) n -> p c n", p=128)
                B.dma(st[:, 0:nku, 0:ncols], src, [], [stb])
                s.st[s.nd] = (st, stb)
                s.nd += 1
                continue
            if s.ncs < s.nd and s.units[s.ncs][0] <= i + s.AHEAD:
                si = B.wri % len(B.wring)
                if B.wring_owner[si] is None:
                    bi, W, r, nku, col0, ncols = s.units[s.ncs]
                    B.wri += 1
                    B.wring_owner[si] = 1
                    sl, slb = B.wring[si]
                    st, stb = s.st.pop(s.ncs)
                    B.cp('act', sl[:, 0:nku, 0:ncols], st[:, 0:nku, 0:ncols], [stb], [slb])
                    s.rg[s.ncs] = (sl, slb, nku, si)
                    s.ncs += 1
                    continue
            break

    def get(s, i):
        s.pump(i)
        us = s.bunits[i]
        assert all(u in s.rg for u in us), "weight ring stalled"
        return [s.rg[u] for u in us]

    def done(s, i):
        for u in s.bunits[i]:
            s.B.wring_owner[s.rg.pop(u)[3]] = None


def build(cfg):
    B = Builder(cfg)
    nc = B.nc
    c = cfg
    D, FF, ND, NF, T, TT, NSUB = c.D, c.FF, c.ND, c.NF, c.T, c.TT, c.NSUB
    HA, HB, KVB, G = c.HA, c.HB, c.KVB, c.G
    NQT, NKB, NKT = c.NQT, c.NKB, c.NKT
    T2 = 2 * T

    xT_own = B.din("xT_own", [D, T])
    xT_ctx = B.din("xT_ctx", [D, T])
    cT = B.din("cT", [128, ND])
    w_ada = B.din("w_ada", [D, 9 * D])
    b_adaT = B.din("b_adaT", [128, 9 * ND])
    gains = B.din("gains", [128, 4 * ND])
    f1g = B.din("f1g", [D, FF]); f1u = B.din("f1u", [D, FF]); f1d = B.din("f1d", [FF, D])
    w_in = B.din("w_in", [D, c.INC])
    sinks = B.din("sinks", [128, HB])
    w_bm = B.din("w_bm", [HA * 128, D]); w_bs = B.din("w_bs", [HB * 64, D]); w_out = B.din("w_out", [D, D])
    f2g = B.din("f2g", [D, FF]); f2u = B.din("f2u", [D, FF]); f2d = B.din("f2d", [FF, D])
    ropeA = B.din("ropeA", [2, 128, T2])
    ropeB = B.din("ropeB", [2, 128, T2])
    cmask = B.din("cmask", [128, 4 * 128])
    vbias = B.din("vbias", [128, 2 * NQT * NKB])
    outT = nc.dram_tensor("outT", [D, T], F32, kind="ExternalOutput").ap()

    S_x1 = B.dscr("S_x1", [D, T], F32); S_x1c = B.dscr("S_x1c", [D, T], F32)
    S_x2 = B.dscr("S_x2", [D, T], F32); S_x3 = B.dscr("S_x3", [D, T], F32)
    S_tmp = B.dscr("S_tmp", [D, T], F32)
    QA_d = B.dscr("QA_d", [HA * 128, T], BF16)
    KA_d = B.dscr("KA_d", [HA * 128, T2], BF16)
    VA_d = B.dscr("VA_d", [T2, HA * 128], BF16)
    QB_d = B.dscr("QB_d", [64, NQT, HB, 128], BF16)
    KB_d = B.dscr("KB_d", [64, KVB, T2], BF16)
    VB_d = B.dscr("VB_d", [T2, KVB * 64], BF16)
    SGA_d = B.dscr("SGA_d", [D, T], BF16); SGB_d = B.dscr("SGB_d", [D, T], BF16)
    YA_d = B.dscr("YA_d", [HA * 128, T], BF16); YB_d = B.dscr("YB_d", [HB * 64, T], BF16)
    db = {k: Buf(k) for k in ["x1", "x1c", "x2", "x3", "tmp", "qa", "ka", "va", "qb", "kb", "vb", "sga", "sgb", "ya", "yb", "out"]}

    B.init_psum()
    B.init_wstream(12, 4)
    consts = nc.alloc_sbuf_tensor("consts", [128, 4 * 128], F32); consts_b = Buf()
    ones_bf = nc.alloc_sbuf_tensor("ones_bf", [128, 128], BF16); ones_b = Buf()
    MOD = nc.alloc_sbuf_tensor("MOD", [128, 9 * ND], F32); MOD_b = Buf()
    GN = nc.alloc_sbuf_tensor("GN", [128, 4 * ND], F32); GN_b = Buf()
    AB = nc.alloc_sbuf_tensor("AB", [128, 3, 3, ND], F32); AB_b = Buf()
    VBI = nc.alloc_sbuf_tensor("VBI", [128, 2 * NQT * NKB], F32); VBI_b = Buf()
    ESK = nc.alloc_sbuf_tensor("ESK", [128, HB], F32); ESK_b = Buf()
    EPS = nc.alloc_sbuf_tensor("EPS", [128, 1], F32); EPS_b = Buf()
    B.init_arena(118 * 1024)

    TRI = consts[:, 0:128]; MASKP = consts[:, 128:256]; MASKP0 = consts[:, 256:384]; IDENT = consts[:, 384:512]

    B.dma(consts[:], cmask[:, :], [], [consts_b])
    B.dma(GN[:], gains[:, :], [], [GN_b])
    B.dma(VBI[:], vbias[:, :], [], [VBI_b])
    B.dma(ESK[:], sinks[:, :], [], [ESK_b])
    B.memset('dve', ones_bf[:], 1.0, [ones_b])
    B.memset('dve', EPS[:], 1e-6, [EPS_b])
    B.act(ESK[:], ESK[:], AF.Exp, [ESK_b], [ESK_b])

    B.phase()
    csb, csb_b = B.at([ND], F32)
    cbf, cbf_b = B.at([ND], BF16)
    bad, bad_b = B.at([9 * ND], F32)
    B.dma(csb, cT[:, :], [], [csb_b])
    B.dma(bad, b_adaT[:, :], [], [bad_b])
    B.act(cbf, csb, AF.Silu, [csb_b], [cbf_b])
    psm, psm_b = B.ps()
    specs = [(w_ada, 0, ND, cb * 256, 256) for cb in range(9 * D // 256)]
    wp = WPipe(B, specs)
    for cb in range(len(specs)):
        u = wp.get(cb)
        for cc in range(2):
            j = cb * 2 + cc
            for kc in range(ND):
                l, lb = B.wl(u, kc, cc * 128)
                B.mm(psm[:, j:j + 1], l, cbf[:, kc:kc + 1], kc == 0, kc == ND - 1, [lb, cbf_b], [psm_b])
        wp.done(cb)
    B.tt('dve', MOD[:], psm[:, 0:9 * ND], bad, ALU.add, [psm_b, bad_b], [MOD_b])
    for i in range(3):
        sh = MOD[:, (3 * i) * ND:(3 * i + 1) * ND]
        sc = MOD[:, (3 * i + 1) * ND:(3 * i + 2) * ND]
        gg = MOD[:, (3 * i + 2) * ND:(3 * i + 3) * ND]
        B.stt(AB[:, i, 0, :], sc, 1.0, GN[:, i * ND:(i + 1) * ND], ALU.add, ALU.mult, [MOD_b, GN_b], [AB_b])
        B.cp('dve', AB[:, i, 1, :], sh, [MOD_b], [AB_b])
        B.ts('dve', AB[:, i, 2, :], gg, (1.0 if i == 1 else 0.5), None, ALU.mult, None, [MOD_b], [AB_b])

    def norm_apply(src, src_b, t0, hT, hT_b, Acol, Bcol, xr, sq, rstd, rstd_b, tmpf, out_dram=None, out_b=None):
        ssb = [B.ps() for _ in range(NSUB)]
        NX = len(xr)

        def ldx(dc):
            xt, xb = xr[dc % NX]
            B.ld(xt, src[dc * 128:(dc + 1) * 128, t0:t0 + TT], [src_b], [xb])

        for dc in range(min(NX - 1, ND)):
            ldx(dc)
        for dc in range(ND):
            if dc + NX - 1 < ND:
                ldx(dc + NX - 1)
            xt, xb = xr[dc % NX]
            st, sb_ = sq[dc % len(sq)]
            B.act(st, xt, AF.Square, [xb], [sb_])
            for su in range(NSUB):
                B.mm(ssb[su][0][:, :], ones_bf[:], st[:, su * 512:(su + 1) * 512], dc == 0, dc == ND - 1,
                     [ones_b, sb_], [ssb[su][1]])
        for dc in range(min(NX - 1, ND)):
            ldx(dc)
        for su in range(NSUB):
            r = rstd[:, su * 512:(su + 1) * 512]
            B.act(r, ssb[su][0][:, :], AF.Sqrt, [ssb[su][1], EPS_b], [rstd_b], bias=EPS[:, 0:1], scale=1.0 / D)
            B.R.add('dve', lambda e, r=r: e.reciprocal(r, r), [rstd_b], [rstd_b])
        for dc in range(ND):
            if dc + NX - 1 < ND:
                ldx(dc + NX - 1)
            xt, xb = xr[dc % NX]
            for su in range(NSUB):
                tf, tfb = tmpf[(dc * NSUB + su) % len(tmpf)]
                sl = slice(su * 512, (su + 1) * 512)
                B.tt('dve', tf, xt[:, sl], rstd[:, sl], ALU.mult, [xb, rstd_b], [tfb])
                if out_dram is None:
                    B.act(hT[:, dc, sl], tf, AF.Identity, [tfb, AB_b], [hT_b[dc]],
                          bias=Bcol[:, dc:dc + 1], scale=Acol[:, dc:dc + 1])
                else:
                    B.act(tf, tf, AF.Identity, [tfb, GN_b], [tfb], scale=Acol[:, dc:dc + 1])
                    B.dma(out_dram[dc * 128:(dc + 1) * 128, t0 + su * 512:t0 + (su + 1) * 512], tf, [tfb], [out_b])

    def ffn_phase(src, src_b, dst, dst_b, ph, Wg, Wu, Wd, ntiles):
        B.phase()
        hT, _ = B.at([ND, TT], BF16)
        hT_b = [Buf() for _ in range(ND)]
        NFH = NF // c.FSPLIT
        aT, _ = B.at([NFH, TT], BF16)
        aT_b = [Buf() for _ in range(NFH)]
        xr = [B.at([TT], F32) for _ in range(4)]
        sq = [B.at([TT], BF16) for _ in range(2)]
        rstd, rstd_b = B.at([TT], F32)
        tmpf = [B.at([512], F32) for _ in range(4)]
        xo = [B.at([512], F32) for _ in range(4)]
        Acol, Bcol, Gcol = AB[:, ph, 0, :], AB[:, ph, 1, :], AB[:, ph, 2, :]
        specs, gu_i, dn_i = [], {}, {}
        for ti in range(ntiles):
            for fs in range(c.FSPLIT):
                for fb in range(NFH // 2):
                    col0 = (fs * NFH + fb * 2) * 128
                    gu_i[(ti, fs, fb)] = len(specs)
                    specs.append((Wg, 0, ND, col0, 256))
                    specs.append((Wu, 0, ND, col0, 256))
                for db_ in range(ND // 2):
                    dn_i[(ti, fs, db_)] = len(specs)
                    specs.append((Wd, fs * NFH, NFH, db_ * 256, 256))
        wp = WPipe(B, specs)
        wp.pump(0)
        for ti in range(ntiles):
            t0 = ti * TT
            norm_apply(src, src_b, t0, hT, hT_b, Acol, Bcol, xr, sq, rstd, rstd_b, tmpf)
            for fs in range(c.FSPLIT):
                for fb in range(NFH // 2):
                    bi = gu_i[(ti, fs, fb)]
                    ug = wp.get(bi)
                    uu = wp.get(bi + 1)
                    for cc in range(2):
                        fl = fb * 2 + cc
                        for su in range(NSUB):
                            sl = slice(su * 512, (su + 1) * 512)
                            pg, pgb = B.ps()
                            pu, pub = B.ps()
                            for kc in range(ND):
                                l, lb = B.wl(ug, kc, cc * 128)
                                B.mm(pg[:, :], l, hT[:, kc, sl], kc == 0, kc == ND - 1, [lb, hT_b[kc]], [pgb])
                            for kc in range(ND):
                                l, lb = B.wl(uu, kc, cc * 128)
                                B.mm(pu[:, :], l, hT[:, kc, sl], kc == 0, kc == ND - 1, [lb, hT_b[kc]], [pub])
                            tf, tfb = tmpf[(fl * NSUB + su) % len(tmpf)]
                            B.act(tf, pg[:, :], AF.Silu, [pgb], [tfb])
                            B.tt('dve', aT[:, fl, sl], tf, pu[:, :], ALU.mult, [tfb, pub], [aT_b[fl]])
                    wp.done(bi)
                    wp.done(bi + 1)
                xin, xin_b = (src, src_b) if fs == 0 else (S_tmp, db["tmp"])
                xout, xout_b = (dst, dst_b) if fs == c.FSPLIT - 1 else (S_tmp, db["tmp"])
                for db_ in range(ND // 2):
                    bi = dn_i[(ti, fs, db_)]
                    u = wp.get(bi)
                    for cc in range(2):
                        dc = db_ * 2 + cc
                        for su in range(NSUB):
                            sl = slice(t0 + su * 512, t0 + (su + 1) * 512)
                            xt, xtb = xo[(dc * NSUB + su) % len(xo)]
                            B.ld(xt, xin[dc * 128:(dc + 1) * 128, sl], [xin_b], [xtb])
                            pd, pdb = B.ps()
                            for kc in range(NFH):
                                l, lb = B.wl(u, kc, cc * 128)
                                B.mm(pd[:, :], l, aT[:, kc, su * 512:(su + 1) * 512], kc == 0, kc == NFH - 1,
                                     [lb, aT_b[kc]], [pdb])
                            B.stt(xt, pd[:, :], Gcol[:, dc:dc + 1], xt, ALU.mult, ALU.add, [pdb, xtb, AB_b], [xtb])
                            B.dma(xout[dc * 128:(dc + 1) * 128, sl], xt, [xtb], [xout_b])
                    wp.done(bi)

    ffn_phase(xT_ctx, Buf(), S_x1c, db["x1c"], 0, f1g, f1u, f1d, T // TT)
    ffn_phase(xT_own, Buf(), S_x1, db["x1"], 0, f1g, f1u, f1d, T // TT)

    def proj_phase(src, src_b, is_ctx):
        B.phase()
        hT, _ = B.at([ND, TT], BF16)
        hT_b = [Buf() for _ in range(ND)]
        xr = [B.at([TT], F32) for _ in range(4)]
        sq = [B.at([TT], BF16) for _ in range(2)]
        rstd, rstd_b = B.at([TT], F32)
        tmpf = [B.at([512], F32) for _ in range(4)]
        rp = [B.at([2, 512], F32) for _ in range(4)]
        xs = [B.at([512], F32) for _ in range(3)]
        ra = [B.at([512], F32) for _ in range(3)]
        rb = [B.at([512], F32) for _ in range(3)]
        ev = [B.at([512], BF16) for _ in range(4)]
        evi = [0]
        Acol, Bcol = AB[:, 1, 0, :], AB[:, 1, 1, :]
        loc0 = 0 if is_ctx else T
        for ti in range(T // TT):
            t0 = ti * TT
            blocks = []
            if not is_ctx:
                for i in range(HA // 2):
                    blocks.append(("qa", c.oqa + i * 256, 256, i * 2))
            for i in range(HA // 2):
                blocks.append(("ka", c.oka + i * 256, 256, i * 2))
            for i in range(HA // 2):
                blocks.append(("va", c.ova + i * 256, 256, i * 2))
            if not is_ctx:
                for i in range(HB * 64 // 256):
                    blocks.append(("qb", c.oqb + i * 256, 256, i * 2))
            blocks.append(("kb", c.okb, 128, 0))
            blocks.append(("vb", c.ovb, 128, 0))
            if not is_ctx:
                for i in range(ND // 2):
                    blocks.append(("ga", c.oga + i * 256, 256, i * 2))
                for i in range(ND // 2):
                    blocks.append(("gb", c.ogb + i * 256, 256, i * 2))
            wp = WPipe(B, [(w_in, 0, ND, b[1], b[2]) for b in blocks])
            wp.pump(0)
            norm_apply(src, src_b, t0, hT, hT_b, Acol, Bcol, xr, sq, rstd, rstd_b, tmpf)
            ropeslices = {}
            for su in range(NSUB):
                for which, tab in ((0, ropeA), (1, ropeB)):
                    r_, rb_ = rp[su * 2 + which] if NSUB * 2 <= len(rp) else rp[which]
                    lo = loc0 + t0 + su * 512
                    B.ld(r_, tab[:, :, lo:lo + 512].rearrange("a p n -> p a n"), [], [rb_])
                    ropeslices[(su, which)] = (r_, rb_)
            for bi, (kind, col0, ncols, ch0) in enumerate(blocks):
                u = wp.get(bi)
                if kind in ("va", "vb"):
                    for tk in range(TT // 128):
                        pv, pvb = B.ps()
                        for kc in range(ND):
                            r_, rb_ = B.wl(u, kc, 0, ncols)
                            B.mm(pv[:, 0:ncols], hT[:, kc, tk * 128:(tk + 1) * 128], r_, kc == 0, kc == ND - 1,
                                 [rb_, hT_b[kc]], [pvb])
                        e, eb = ev[evi[0] % len(ev)]
                        evi[0] += 1
                        B.cp('act', e[:, 0:ncols], pv[:, 0:ncols], [pvb], [eb])
                        tok = loc0 + t0 + tk * 128
                        if kind == "va":
                            B.dma(VA_d[tok:tok + 128, col0 - c.ova:col0 - c.ova + ncols], e[:, 0:ncols], [eb], [db["va"]])
                        else:
                            B.dma(VB_d[tok:tok + 128, 0:ncols], e[:, 0:ncols], [eb], [db["vb"]])
                    wp.done(bi)
                    continue
                for cc in range(ncols // 128):
                    ch = ch0 + cc
                    for su in range(NSUB):
                        sl = slice(su * 512, (su + 1) * 512)
                        pp, ppb = B.ps()
                        for kc in range(ND):
                            l, lb = B.wl(u, kc, cc * 128)
                            B.mm(pp[:, :], l, hT[:, kc, sl], kc == 0, kc == ND - 1, [lb, hT_b[kc]], [ppb])
                        tok = t0 + su * 512
                        if kind in ("ga", "gb"):
                            e, eb = ev[evi[0] % len(ev)]
                            evi[0] += 1
                            B.act(e, pp[:, :], AF.Sigmoid, [ppb], [eb])
                            dd, ddb = (SGA_d, db["sga"]) if kind == "ga" else (SGB_d, db["sgb"])
                            B.dma(dd[ch * 128:(ch + 1) * 128, tok:tok + 512], e, [eb], [ddb])
                            continue
                        isA = kind in ("qa", "ka")
                        rt, rtb = ropeslices[(su, 0 if isA else 1)]
                        x_, xb_ = xs[evi[0] % 3]
                        a_, ab_ = ra[evi[0] % 3]
                        b_, bb_ = rb[evi[0] % 3]
                        B.cp('act', x_, pp[:, :], [ppb], [xb_])
                        B.tt('pool', a_, x_, rt[:, 0, :], ALU.mult, [xb_, rtb], [ab_])
                        hs = 64 if isA else 32
                        for g0 in range(0, 128, 2 * hs):
                            B.tt('pool', b_[g0:g0 + hs, :], x_[g0 + hs:g0 + 2 * hs, :], rt[g0 + hs:g0 + 2 * hs, 1, :],
                                 ALU.mult, [xb_, rtb], [bb_])
                            B.tt('pool', b_[g0 + hs:g0 + 2 * hs, :], x_[g0:g0 + hs, :], rt[g0:g0 + hs, 1, :],
                                 ALU.mult, [xb_, rtb], [bb_])
                        e, eb = ev[evi[0] % len(ev)]
                        evi[0] += 1
                        if isA:
                            B.tt('dve', e, a_, b_, ALU.add, [ab_, bb_], [eb])
                            if kind == "qa":
                                B.dma(QA_d[ch * 128:(ch + 1) * 128, tok:tok + 512], e, [eb], [db["qa"]])
                            else:
                                B.dma(KA_d[ch * 128:(ch + 1) * 128, loc0 + tok:loc0 + tok + 512], e, [eb], [db["ka"]])
                        else:
                            B.tt('dve', e[0:64, 0:512], a_[0:64, :], b_[0:64, :], ALU.add, [ab_, bb_], [eb])
                            e2, eb2 = ev[evi[0] % len(ev)]
                            evi[0] += 1
                            B.tt('dve', e2[0:64, 0:512], a_[64:128, :], b_[64:128, :], ALU.add, [ab_, bb_], [eb2])
                            for hh, (ee, eeb) in enumerate(((e, eb), (e2, eb2))):
                                if kind == "qb":
                                    head = ch * 2 + hh
                                    B.dma(QB_d[:, tok // 128:tok // 128 + 4, head, :],
                                          ee[0:64, 0:512].rearrange("p (a b) -> p a b", a=4), [eeb], [db["qb"]])
                                else:
                                    B.dma(KB_d[:, hh, loc0 + tok:loc0 + tok + 512], ee[0:64, 0:512], [eeb], [db["kb"]])
                wp.done(bi)

    proj_phase(S_x1c, db["x1c"], True)
    proj_phase(S_x1, db["x1"], False)

    B.phase()
    scaleA = 128.0 ** -0.5
    KT = [B.at([T2], BF16) for _ in range(2)]
    V1 = [B.at([NKT, 132], BF16) for _ in range(2)]
    QT = [B.at([T], BF16) for _ in range(2)]
    kmf = [B.at([NKB], F32) for _ in range(2)]
    kmT = [B.at([NKB], BF16) for _ in range(2)]
    gsm = [B.at([NKB], F32) for _ in range(2)]
    top8 = [B.at([8], F32) for _ in range(2)]
    SELs = [B.at([NQT, NKB], F32) for _ in range(2)]
    Pt = [B.at([256], BF16) for _ in range(8)]
    acc = [B.at([132], F32) for _ in range(4)]
    rec = [B.at([1], F32) for _ in range(2)]
    yt = [B.at([128], F32) for _ in range(2)]
    ytT = [B.at([256], BF16) for _ in range(2)]
    for vv, vb_ in V1:
        B.memset('pool', vv[:, :, 128:129], 1.0, [vb_])
    pti = [0]
    VBb = VBI[:, 0:NQT * NKB].rearrange("p (a b) -> p a b", a=NQT)
    VBv = VBI[:, NQT * NKB:2 * NQT * NKB].rearrange("p (a b) -> p a b", a=NQT)

    def load_head(h):
        kt, ktb = KT[h % 2]
        v1, v1b = V1[h % 2]
        qt, qtb = QT[h % 2]
        B.ld(kt, KA_d[h * 128:(h + 1) * 128, :], [db["ka"]], [ktb])
        B.ld(v1[:, :, 0:128], VA_d[:, h * 128:(h + 1) * 128].rearrange("(n p) d -> p n d", p=128), [db["va"]], [v1b])
        B.ld(qt, QA_d[h * 128:(h + 1) * 128, :], [db["qa"]], [qtb])

    def gate_steps(h):
        kt, ktb = KT[h % 2]
        qt, qtb = QT[h % 2]
        kf, kfb = kmf[h % 2]
        km, kmb = kmT[h % 2]
        SEL, SEL_b = SELs[h % 2]
        steps = []

        def s0():
            B.R.add('dve', lambda e: e.tensor_reduce(kf, kt.rearrange("p (n k) -> p n k", k=256), AX.X, ALU.add),
                    [ktb], [kfb])
            B.ts('dve', km, kf, 1.0 / 256, None, ALU.mult, None, [kfb], [kmb])
        steps.append(s0)
        for q in range(NQT):
            def sq_(q=q):
                pg, pgb = B.ps()
                B.mm(pg[:, 0:NKB], qt[:, q * 128:(q + 1) * 128], km, True, True, [qtb, kmb], [pgb])
                g_, gb_ = gsm[q % 2]
                t8, t8b = top8[q % 2]
                B.tt('dve', g_, pg[:, 0:NKB], VBb[:, q, :], ALU.add, [pgb, VBI_b], [gb_])
                B.R.add('dve', lambda e: e.max(t8, g_), [gb_], [t8b])
                B.ts('dve', g_, g_, t8[:, c.TOPK - 1:c.TOPK], None, ALU.is_ge, None, [gb_, t8b], [gb_])
                B.tt('dve', SEL[:, q, :], g_, VBv[:, q, :], ALU.mult, [gb_, VBI_b], [SEL_b])
            steps.append(sq_)
        return steps

    def moba_items(h):
        kt, ktb = KT[h % 2]
        v1, v1b = V1[h % 2]
        qt, qtb = QT[h % 2]
        SEL, SEL_b = SELs[h % 2]
        items = []
        for j in range(T // 256):
            nb = T // 256 + j
            q0 = j * 256
            accs = (acc[(2 * j) % 4], acc[(2 * j + 1) % 4])
            k0 = 2 * nb

            def A_own(q0=q0, k0=k0):
                s0, s0b = B.ps()
                B.mm(s0[:, 0:256], kt[:, k0 * 128:(k0 + 1) * 128], qt[:, q0:q0 + 256], True, True, [ktb, qtb], [s0b])
                B.mm(s0[:, 256:384], kt[:, (k0 + 1) * 128:(k0 + 2) * 128], qt[:, q0 + 128:q0 + 256], True, True, [ktb, qtb], [s0b])
                p0, p0b = Pt[pti[0] % 8]; pti[0] += 1
                p1, p1b = Pt[pti[0] % 8]; pti[0] += 1
                B.act(p0, s0[:, 0:256], AF.Exp, [s0b], [p0b], scale=scaleA)
                B.act(p1[:, 0:128], s0[:, 256:384], AF.Exp, [s0b], [p1b], scale=scaleA)
                B.tt('pool', p0[:, 0:128], p0[:, 0:128], TRI, ALU.mult, [p0b, consts_b], [p0b])
                B.tt('pool', p1[:, 0:128], p1[:, 0:128], TRI, ALU.mult, [p1b, consts_b], [p1b])
                return (p0, p0b, p1, p1b)

            def B_own(st, k0=k0, accs=accs):
                p0, p0b, p1, p1b = st
                oo, oob = B.ps()
                B.mm(oo[:, 0:129], p0[:, 0:128], v1[:, k0, 0:129], True, True, [p0b, v1b], [oob])
                B.mm(oo[:, 256:385], p0[:, 128:256], v1[:, k0, 0:129], True, False, [p0b, v1b], [oob])
                B.mm(oo[:, 256:385], p1[:, 0:128], v1[:, k0 + 1, 0:129], False, True, [p1b, v1b], [oob])
                B.cp('dve', accs[0][0][:, 0:129], oo[:, 0:129], [oob], [accs[0][1]])
                B.cp('dve', accs[1][0][:, 0:129], oo[:, 256:385], [oob], [accs[1][1]])
            items.append((A_own, B_own))
            for n in range(nb):
                def A_p(n=n, q0=q0):
                    sp_, spb = B.ps()
                    pk = []
                    for kk in range(2):
                        ktile = 2 * n + kk
                        B.mm(sp_[:, kk * 256:(kk + 1) * 256], kt[:, ktile * 128:(ktile + 1) * 128], qt[:, q0:q0 + 256], True, True,
                             [ktb, qtb], [spb])
                    for kk in range(2):
                        pp_, ppb = Pt[pti[0] % 8]; pti[0] += 1
                        B.act(pp_, sp_[:, kk * 256:(kk + 1) * 256], AF.Exp, [spb], [ppb], scale=scaleA)
                        pk.append((pp_, ppb, 2 * n + kk))
                    return pk

                def B_p(pk, n=n, j=j, q0=q0, accs=accs, last=(n == nb - 1)):
                    oo, oob = B.ps()
                    for qs in range(2):
                        for kk in range(2):
                            pp_, ppb, ktile = pk[kk]
                            B.mm(oo[:, qs * 256:qs * 256 + 129], pp_[:, qs * 128:(qs + 1) * 128], v1[:, ktile, 0:129],
                                 kk == 0, kk == 1, [ppb, v1b], [oob])
                    for qs in range(2):
                        aa, aab = accs[qs]
                        B.stt(aa[:, 0:129], oo[:, qs * 256:qs * 256 + 129], SEL[:, 2 * j + qs, n:n + 1], aa[:, 0:129],
                              ALU.mult, ALU.add, [oob, SEL_b, aab], [aab])
                    if last:
                        yT, yTb = ytT[j % 2]
                        for qs in range(2):
                            aa, aab = accs[qs]
                            r_, rb_ = rec[qs]
                            y_, yb_ = yt[qs]
                            B.R.add('dve', lambda e, r_=r_, aa=aa: e.reciprocal(r_, aa[:, 128:129]), [aab], [rb_])
                            B.ts('dve', y_, aa[:, 0:128], r_[:, 0:1], None, ALU.mult, None, [aab, rb_], [yb_])
                            pt_, ptb = B.ps()
                            B.tr(pt_[:, 0:128], y_, IDENT, [yb_, consts_b], [ptb])
                            B.cp('act', yT[:, qs * 128:(qs + 1) * 128], pt_[:, 0:128], [ptb], [yTb])
                        B.dma(YA_d[h * 128:(h + 1) * 128, q0:q0 + 256], yT, [yTb], [db["ya"]])
                items.append((A_p, B_p))
        return items

    load_head(0)
    for st_ in gate_steps(0):
        st_()
    for h in range(HA):
        gs_next = []
        if h + 1 < HA:
            load_head(h + 1)
            gs_next = gate_steps(h + 1)
        items = moba_items(h)
        every = max(1, len(items) // (len(gs_next) + 1)) if gs_next else 0
        st = items[0][0]()
        for k in range(len(items)):
            nst = items[k + 1][0]() if k + 1 < len(items) else None
            items[k][1](st)
            st = nst
            if gs_next and k % every == every - 1 and k > len(items) // 8:
                gs_next.pop(0)()
        while gs_next:
            gs_next.pop(0)()

    B.phase()
    scaleB = 64.0 ** -0.5
    KBT, KBT_b = B.at([KVB, T2], BF16)
    VB1, VB1_b = B.at([NKT, KVB, 66], BF16)
    QBT = [B.at([HB, 128], BF16) for _ in range(2)]
    Pb = [B.at([512], BF16) for _ in range(6)]
    den = [B.at([4], F32) for _ in range(2)]
    ybt = [B.at([HB * 64], F32) for _ in range(2)]
    ybT = [B.at([HB * 64 // 128, 128], BF16) for _ in range(2)]
    B.ld(KBT[0:64], KB_d[:, :, :], [db["kb"]], [KBT_b])
    B.memset('pool', VB1[:, :, :, 64:65], 1.0, [VB1_b])
    for g in range(KVB):
        B.ld(VB1[:, :, g, 0:64], VB_d[:, g * 64:(g + 1) * 64].rearrange("(n p) d -> p n d", p=128), [db["vb"]], [VB1_b])
    HG = min(4, G)
    pbi = [0]
    B.ld(QBT[0][0][0:64], QB_d[:, 0, :, :], [db["qb"]], [QBT[0][1]])
    sitems = []
    for i in range(NQT):
        cur = T // 128 + i
        prv = cur - 1
        for g in range(KVB):
            for hg in range(G // HG):
                first = (g == 0 and hg == 0)
                lastit = (g == KVB - 1 and hg == G // HG - 1)

                def A_s(i=i, g=g, hg=hg, cur=cur, prv=prv, first=first):
                    qb_, qbb = QBT[i % 2]
                    if first and i + 1 < NQT:
                        B.ld(QBT[(i + 1) % 2][0][0:64], QB_d[:, i + 1, :, :], [db["qb"]], [QBT[(i + 1) % 2][1]])
                    h0 = g * G + hg * HG
                    rhs = qb_[0:64, h0:h0 + HG, :]
                    ps_ = []
                    for (ktile, msk) in ((prv, MASKP0 if i == 0 else MASKP), (cur, TRI)):
                        sp_, spb = B.ps()
                        B.mm(sp_[:, 0:HG * 128], KBT[0:64, g, ktile * 128:(ktile + 1) * 128], rhs, True, True, [KBT_b, qbb], [spb])
                        pp_, ppb = Pb[pbi[0] % 6]; pbi[0] += 1
                        B.act(pp_[:, 0:HG * 128], sp_[:, 0:HG * 128], AF.Exp, [spb], [ppb], scale=scaleB)
                        pv = pp_[:, 0:HG * 128].rearrange("p (h q) -> p h q", h=HG)
                        B.tt('pool', pv, pv, msk.unsqueeze(1).to_broadcast([128, HG, 128]), ALU.mult, [ppb, consts_b], [ppb])
                        ps_.append((pp_, ppb, ktile))
                    return ps_

                def B_s(ps_, i=i, g=g, hg=hg, lastit=lastit):
                    y_, yb_ = ybt[i % 2]
                    h0 = g * G + hg * HG
                    oo, oob = B.ps()
                    for hh in range(HG):
                        for kk in range(2):
                            pp_, ppb, ktile = ps_[kk]
                            B.mm(oo[:, hh * 65:hh * 65 + 65], pp_[:, hh * 128:(hh + 1) * 128], VB1[:, ktile, g, 0:65],
                                 kk == 0, kk == 1, [ppb, VB1_b], [oob])
                    d_, db_ = den[(g * (G // HG) + hg) % 2]
                    ov = oo[:, 0:HG * 65].rearrange("p (h d) -> p h d", h=HG)
                    B.tt('dve', d_[:, 0:HG], ov[:, :, 64], ESK[:, h0:h0 + HG], ALU.add, [oob, ESK_b], [db_])
                    B.R.add('dve', lambda e, d_=d_: e.reciprocal(d_[:, 0:HG], d_[:, 0:HG]), [db_], [db_])
                    yv = y_[:, h0 * 64:(h0 + HG) * 64].rearrange("p (h d) -> p h d", h=HG)
                    B.tt('dve', yv, ov[:, :, 0:64], d_[:, 0:HG].unsqueeze(2).to_broadcast([128, HG, 64]), ALU.mult,
                         [oob, db_], [yb_])
                    if lastit:
                        yT, yTb = ybT[i % 2]
                        for ch in range(HB * 64 // 128):
                            pt_, ptb = B.ps()
                            B.tr(pt_[:, 0:128], y_[:, ch * 128:(ch + 1) * 128], IDENT, [yb_, consts_b], [ptb])
                            B.cp('act', yT[:, ch, :], pt_[:, 0:128], [ptb], [yTb])
                        B.dma(YB_d[:, i * 128:(i + 1) * 128].rearrange("(c p) n -> p c n", p=128), yT, [yTb], [db["yb"]])
                sitems.append((A_s, B_s))
    st = sitems[0][0]()
    for k in range(len(sitems)):
        nst = sitems[k + 1][0]() if k + 1 < len(sitems) else None
        sitems[k][1](st)
        st = nst

    B.phase()
    NCA, NCB = HA, HB * 64 // 128
    yaT, yaT_b = B.at([NCA, TT], BF16)
    ybT2, ybT2_b = B.at([NCB, TT], BF16)
    mT, _ = B.at([ND, TT], BF16)
    mT_b = [Buf() for _ in range(ND)]
    sg = [B.at([2, 512], BF16) for _ in range(3)]
    tm = [B.at([2, 512], F32) for _ in range(3)]
    xo = [B.at([512], F32) for _ in range(4)]
    Gcol = AB[:, 1, 2, :]
    k_i = [0]
    for ti in range(T // TT):
        t0 = ti * TT
        B.ld(yaT, YA_d[:, t0:t0 + TT].rearrange("(c p) n -> p c n", p=128), [db["ya"]], [yaT_b])
        B.ld(ybT2, YB_d[:, t0:t0 + TT].rearrange("(c p) n -> p c n", p=128), [db["yb"]], [ybT2_b])
        specs = []
        for d2 in range(ND // 2):
            specs.append((w_bm, 0, NCA, d2 * 256, 256))
            specs.append((w_bs, 0, NCB, d2 * 256, 256))
        wp = WPipe(B, specs)
        for d2 in range(ND // 2):
            um = wp.get(2 * d2)
            us = wp.get(2 * d2 + 1)
            for cc in range(2):
                dc = d2 * 2 + cc
                for su in range(NSUB):
                    sl = slice(su * 512, (su + 1) * 512)
                    tok = t0 + su * 512
                    s_, sb_ = sg[k_i[0] % 3]
                    t_, tb_ = tm[k_i[0] % 3]
                    k_i[0] += 1
                    B.ld(s_[:, 0, :], SGA_d[dc * 128:(dc + 1) * 128, tok:tok + 512], [db["sga"]], [sb_])
                    B.ld(s_[:, 1, :], SGB_d[dc * 128:(dc + 1) * 128, tok:tok + 512], [db["sgb"]], [sb_])
                    pm, pmb = B.ps()
                    pq, pqb = B.ps()
                    for kc in range(NCA):
                        l, lb = B.wl(um, kc, cc * 128)
                        B.mm(pm[:, :], l, yaT[:, kc, sl], kc == 0, kc == NCA - 1, [lb, yaT_b], [pmb])
                    for kc in range(NCB):
                        l, lb = B.wl(us, kc, cc * 128)
                        B.mm(pq[:, :], l, ybT2[:, kc, sl], kc == 0, kc == NCB - 1, [lb, ybT2_b], [pqb])
                    B.tt('dve', t_[:, 0, :], pm[:, :], s_[:, 0, :], ALU.mult, [pmb, sb_], [tb_])
                    B.tt('dve', t_[:, 1, :], pq[:, :], s_[:, 1, :], ALU.mult, [pqb, sb_], [tb_])
                    B.tt('pool', mT[:, dc, sl], t_[:, 0, :], t_[:, 1, :], ALU.add, [tb_], [mT_b[dc]])
            wp.done(2 * d2)
            wp.done(2 * d2 + 1)
        specs = [(w_out, 0, ND, d2 * 256, 256) for d2 in range(ND // 2)]
        wp = WPipe(B, specs)
        for d2 in range(ND // 2):
            u = wp.get(d2)
            for cc in range(2):
                dc = d2 * 2 + cc
                for su in range(NSUB):
                    sl = slice(t0 + su * 512, t0 + (su + 1) * 512)
                    xt, xtb = xo[(dc * NSUB + su) % len(xo)]
                    B.ld(xt, S_x1[dc * 128:(dc + 1) * 128, sl], [db["x1"]], [xtb])
                    pd, pdb = B.ps()
                    for kc in range(ND):
                        l, lb = B.wl(u, kc, cc * 128)
                        B.mm(pd[:, :], l, mT[:, kc, su * 512:(su + 1) * 512], kc == 0, kc == ND - 1, [lb, mT_b[kc]], [pdb])
                    B.stt(xt, pd[:, :], Gcol[:, dc:dc + 1], xt, ALU.mult, ALU.add, [pdb, xtb, AB_b], [xtb])
                    B.dma(S_x2[dc * 128:(dc + 1) * 128, sl], xt, [xtb], [db["x2"]])
            wp.done(d2)

    ffn_phase(S_x2, db["x2"], S_x3, db["x3"], 2, f2g, f2u, f2d, T // TT)
    B.phase()
    xr = [B.at([TT], F32) for _ in range(4)]
    sq = [B.at([TT], BF16) for _ in range(2)]
    rstd, rstd_b = B.at([TT], F32)
    tmpf = [B.at([512], F32) for _ in range(4)]
    for ti in range(T // TT):
        norm_apply(S_x3, db["x3"], ti * TT, None, None, GN[:, 3 * ND:4 * ND], None, xr, sq, rstd, rstd_b, tmpf,
                   out_dram=outT, out_b=db["out"])
    B.R.finish(nc)
    return nc


def rope_tables(cfg, half):
    T = cfg.T
    pos_ctx = np.arange(0, T, dtype=np.float64)
    pos_own = np.arange(half * T, (half + 1) * T, dtype=np.float64)
    pos = np.concatenate([pos_ctx, pos_own])
    out = []
    for hs in (64, 32):
        inv = 10000.0 ** (-np.arange(hs, dtype=np.float64) / hs)
        ang = (pos.astype(np.float32)[None, :] * inv.astype(np.float32)[:, None]).astype(np.float32)
        cos = np.cos(ang).astype(np.float32)
        sin = np.sin(ang).astype(np.float32)
        reps = 128 // (2 * hs)
        cosT = np.concatenate([cos, cos] * reps, axis=0)
        sinT = np.concatenate([sin, -sin] * reps, axis=0)
        out.append(np.stack([cosT, sinT]).astype(np.float32))
    return out


def const_masks(cfg, half):
    k = np.arange(128)[:, None]
    q = np.arange(128)[None, :]
    tri = (k <= q).astype(np.float32)
    mp = (k > q).astype(np.float32)
    mp0 = mp * float(half)
    ident = np.eye(128, dtype=np.float32)
    cm = np.concatenate([tri, mp, mp0, ident], axis=1)
    NQT, NKB = cfg.NQT, cfg.NKB
    valid = np.zeros((NQT, NKB), np.float32)
    for qt in range(NQT):
        j = qt // 2
        for n in range(NKB):
            if n < NKB // 2:
                valid[qt, n] = float(half)
            elif n < NKB // 2 + j:
                valid[qt, n] = 1.0
    bias = np.where(valid > 0, 0.0, NEG).astype(np.float32)
    vb = np.concatenate([bias.reshape(-1), valid.reshape(-1)])[None, :].repeat(128, 0).astype(np.float32)
    return cm, vb


def colT(v, n):
    return np.ascontiguousarray(np.asarray(v, np.float32).reshape(n, 128).T)


def make_in_maps(cfg, inp):
    D, ND, T = cfg.D, cfg.ND, cfg.T
    f = lambda a: np.ascontiguousarray(np.asarray(a, np.float32))
    x = np.asarray(inp["x"], np.float32)
    shared = {
        "w_ada": f(inp["w_ada"][0]), "b_adaT": colT(inp["b_ada"][0], 9 * ND),
        "gains": np.concatenate([colT(inp["norm_ffn1"][0], ND), colT(inp["norm_mix"][0], ND),
                                 colT(inp["norm_ffn2"][0], ND), colT(inp["norm_final"], ND)], axis=1),
        "f1g": f(inp["ffn1_gate"][0]), "f1u": f(inp["ffn1_up"][0]), "f1d": f(inp["ffn1_down"][0]),
        "w_in": f(inp["w_in"][0]),
        "sinks": np.ascontiguousarray(np.broadcast_to(np.asarray(inp["swa_sinks"][0], np.float32)[None, :], (128, cfg.HB))),
        "w_bm": f(inp["w_branch_moba"][0]), "w_bs": f(inp["w_branch_swa"][0]), "w_out": f(inp["w_out"][0]),
        "f2g": f(inp["ffn2_gate"][0]), "f2u": f(inp["ffn2_up"][0]), "f2d": f(inp["ffn2_down"][0]),
    }
    maps = []
    for core in range(2 * cfg.BATCH):
        b, half = core // 2, core % 2
        rA, rB = rope_tables(cfg, half)
        cm, vb = const_masks(cfg, half)
        m = dict(shared)
        m["xT_own"] = np.ascontiguousarray(x[b, half * T:(half + 1) * T, :].T)
        m["xT_ctx"] = np.ascontiguousarray(x[b, 0:T, :].T)
        m["cT"] = colT(inp["c"][b], ND)
        m["ropeA"], m["ropeB"], m["cmask"], m["vbias"] = rA, rB, cm, vb
        maps.append(m)
    return maps


def assemble(cfg, results):
    out = np.empty((cfg.BATCH, cfg.SEQ, cfg.D), np.float32)
    for core, r in enumerate(results):
        b, half = core // 2, core % 2
        out[b, half * cfg.T:(half + 1) * cfg.T, :] = np.asarray(r["outT"], np.float32).T
    return out


def kernel(**inputs):
    cfg = Cfg()
    nc = build(cfg)
    maps = make_in_maps(cfg, inputs)
    res = run_bass_kernel_spmd(nc, maps, core_ids=list(range(8)))
    return assemble(cfg, res.results)
```

```python
import numpy as np
import ml_dtypes
import concourse.bass as bass
import concourse.mybir as mybir
from concourse.bass_utils import run_bass_kernel_spmd

F32 = mybir.dt.float32
BF16 = mybir.dt.bfloat16
ALU = mybir.AluOpType
AF = mybir.ActivationFunctionType
AX = mybir.AxisListType
NEG = -1.0e30


class Cfg:
    def __init__(s, D=2048, FF=5632, HA=8, HB=16, KVB=2, SEQ=4096, BATCH=4, TT=1024, FSPLIT=2):
        s.D, s.FF, s.HA, s.HB, s.KVB, s.SEQ, s.BATCH, s.TT, s.FSPLIT = D, FF, HA, HB, KVB, SEQ, BATCH, TT, FSPLIT
        s.T = SEQ // 2
        s.ND = D // 128
        s.NF = FF // 128
        s.BLK, s.W, s.TOPK = 256, 128, 3
        s.G = HB // KVB
        s.oqa = 0
        s.oka = HA * 128
        s.ova = 2 * HA * 128
        s.oqb = 3 * HA * 128
        s.okb = s.oqb + HB * 64
        s.ovb = s.okb + KVB * 64
        s.oga = s.ovb + KVB * 64
        s.ogb = s.oga + D
        s.INC = s.ogb + D
        s.NSUB = TT // 512
        s.NQT = s.T // 128
        s.NKB = 2 * s.T // 256
        s.NKT = 2 * s.T // 128
        assert KVB == 2 and s.T % TT == 0 and TT % 512 == 0 and s.NF % FSPLIT == 0


class Buf:
    __slots__ = ("n", "w", "r")

    def __init__(s, n=""):
        s.n = n
        s.w = {}
        s.r = {}


class Op:
    __slots__ = ("eng", "fn", "deps", "sig", "val", "dsem", "dval", "waits", "isdma")


class Rec:
    NDS = 40
    QPOOL = {'sp': (0, 16), 'act': (16, 24)}

    def __init__(s):
        s.ops = []
        s.last = {}
        s.bar = None
        s.bar_done = set()
        s.ndma = 0
        s.dma_last = {}
        s.qcnt = {}

    def add(s, eng, fn, reads=(), writes=(), dma=False):
        op = Op()
        op.eng, op.fn, op.isdma, op.sig, op.deps = eng, fn, dma, False, set()
        op.val = op.dsem = op.dval = None
        for b in reads:
            for k, v in b.w.items():
                if k == 'dma':
                    op.deps.update(v)
                elif k == eng and not dma and eng == 'pe':
                    pass
                else:
                    op.deps.add(v)
        for b in writes:
            for k, v in b.r.items():
                if k == 'dma':
                    op.deps.update(v)
                elif k == eng and not dma:
                    pass
                else:
                    op.deps.add(v)
            for k, v in b.w.items():
                if k == 'dma':
                    op.deps.update(v)
                elif k == eng and not dma:
                    pass
                else:
                    op.deps.add(v)
        if s.bar is not None and eng not in s.bar_done:
            op.deps.update(s.bar)
            s.bar_done.add(eng)
        if dma:
            base, cnt_ = s.QPOOL[eng]
            nq = s.qcnt.get(eng, 0)
            s.qcnt[eng] = nq + 1
            op.dsem = base + nq % cnt_
            op.dval = 16 * (nq // cnt_ + 1)
            s.ndma += 1
            prev = s.dma_last.get(op.dsem)
            if prev is not None:
                op.deps.add(prev)
            s.dma_last[op.dsem] = op
            op.sig = True
        for b in reads:
            if dma:
                b.r.setdefault('dma', []).append(op)
            else:
                b.r[eng] = op
        for b in writes:
            if b.r:
                b.w = {}
                b.r = {}
            if dma:
                b.w.setdefault('dma', []).append(op)
            else:
                b.w[eng] = op
        if not dma:
            s.last[eng] = op
        s.ops.append(op)
        return op

    def barrier(s):
        s.bar = set(s.last.values()) | set(s.dma_last.values())
        s.bar_done = set()

    def finish(s, nc):
        for op in s.ops:
            for d in op.deps:
                d.sig = True
        cnt = {}
        for op in s.ops:
            if op.isdma:
                continue
            if op.sig:
                cnt[op.eng] = cnt.get(op.eng, 0) + 1
                op.val = cnt[op.eng]
        engs = ['pe', 'act', 'dve', 'pool', 'sp']
        esem = {e: nc.alloc_semaphore("sem_" + e) for e in engs if e != 'sp'}
        dsem = [nc.alloc_semaphore("dsem%d" % i) for i in range(s.NDS)]
        per = {e: [] for e in engs}
        for op in s.ops:
            per[op.eng].append(op)
        for e in engs:
            waited = {}
            for op in per[e]:
                need = {}
                for d in op.deps:
                    if d.isdma:
                        k, v = ('d', d.dsem), d.dval
                    else:
                        k, v = ('e', d.eng), d.val
                    if v > need.get(k, 0):
                        need[k] = v
                op.waits = []
                for k, v in need.items():
                    if v > waited.get(k, 0):
                        waited[k] = v
                        op.waits.append((k, v))
        dfinal = {}
        for op in s.ops:
            if op.isdma:
                dfinal[op.dsem] = op.dval

        def body_for(e):
            def body(eng):
                for op in per[e]:
                    for (k, v) in op.waits:
                        eng.wait_ge(dsem[k[1]] if k[0] == 'd' else esem[k[1]], v)
                    ins = op.fn(eng)
                    if op.isdma:
                        ins.then_inc(dsem[op.dsem], 16)
                    elif op.sig:
                        ins.then_inc(esem[e], 1)
                if e == 'sp':
                    for k, v in dfinal.items():
                        eng.wait_ge(dsem[k], v)
                    for e2 in esem:
                        if cnt.get(e2, 0) > 0:
                            eng.wait_ge(esem[e2], cnt[e2])
            return body

        with nc.Block() as blk:
            blk.tensor(body_for('pe'))
            blk.scalar(body_for('act'))
            blk.vector(body_for('dve'))
            blk.gpsimd(body_for('pool'))
            blk.sync(body_for('sp'))


class Builder:
    def __init__(s, cfg):
        s.c = cfg
        s.nc = bass.Bass("TRN2", target_bir_lowering=False)
        s.R = Rec()
        s.dram = {}
        s.nbuf = 0
        s.cast_rr = 0

    def din(s, name, shape, dt=F32):
        t = s.nc.dram_tensor(name, list(shape), dt, kind="ExternalInput").ap()
        s.dram[name] = t
        return t

    def dscr(s, name, shape, dt):
        return s.nc.dram_tensor(name, list(shape), dt, kind="Internal").ap()

    def buf(s, n=""):
        return Buf(n)

    def mm(s, out, lhsT, rhs, start, stop, reads, writes):
        s.R.add('pe', lambda e: e.matmul(out, lhsT, rhs, start=start, stop=stop), reads, writes)

    def tr(s, out, in_, ident, reads, writes):
        s.R.add('pe', lambda e: e.transpose(out, in_, ident), reads, writes)

    def dma(s, out, in_, reads, writes, q='sp'):
        s.R.add(q, lambda e: e.dma_start(out=out, in_=in_), reads, writes, dma=True)

    def sta(s, out, in_, reads, writes):
        s.dma(out, in_, reads, writes, q='act')

    def ld(s, out, in_, reads, writes):
        s.dma(out, in_, reads, writes, q='act')

    def act(s, out, in_, func, reads, writes, bias=None, scale=None, accum_out=None):
        kw = {}
        if bias is not None:
            kw['bias'] = bias
        if scale is not None:
            kw['scale'] = scale
        if accum_out is not None:
            kw['accum_out'] = accum_out
        s.R.add('act', lambda e: e.activation(out, in_, func, **kw), reads, writes)

    def tt(s, eng, out, in0, in1, op, reads, writes):
        s.R.add(eng, lambda e: e.tensor_tensor(out, in0, in1, op), reads, writes)

    def ts(s, eng, out, in0, s1, s2, op0, op1, reads, writes):
        if op1 is None:
            s.R.add(eng, lambda e: e.tensor_scalar(out, in0, s1, None, op0), reads, writes)
        else:
            s.R.add(eng, lambda e: e.tensor_scalar(out, in0, s1, s2, op0, op1), reads, writes)

    def stt(s, out, in0, scalar, in1, op0, op1, reads, writes):
        s.R.add('dve', lambda e: e.scalar_tensor_tensor(out, in0, scalar, in1, op0, op1), reads, writes)

    def cp(s, eng, out, in_, reads, writes):
        if eng == 'act':
            s.R.add('act', lambda e: e.activation(out, in_, AF.Copy), reads, writes)
        else:
            s.R.add(eng, lambda e: e.tensor_copy(out, in_), reads, writes)

    def memset(s, eng, ap, val, writes):
        s.R.add(eng, lambda e: e.memset(ap, val), (), writes)

    def init_psum(s):
        s.psb = [(s.nc.alloc_psum_tensor("psb%d" % i, [128, 512], F32), Buf("ps%d" % i)) for i in range(8)]
        s.psres = s.psb.pop()
        s.psres2 = s.psb.pop()
        s.psi = 0

    def ps(s):
        t = s.psb[s.psi % len(s.psb)]
        s.psi += 1
        return t

    def init_arena(s, nbytes):
        s.arena = s.nc.alloc_sbuf_tensor("arena", [128, nbytes // 4], F32)
        s.arena_n = nbytes // 4
        s.aoff = 0

    def phase(s):
        s.R.barrier()
        s.aoff = 0

    def at(s, shape, dt):
        n = int(np.prod(shape))
        nw = (n * (2 if dt == BF16 else 4) + 3) // 4
        nw = (nw + 7) // 8 * 8
        assert s.aoff + nw <= s.arena_n, ("arena overflow", s.aoff, nw, s.arena_n)
        ap = s.arena[:, s.aoff:s.aoff + nw]
        s.aoff += nw
        if dt == BF16:
            ap = ap.bitcast(BF16)
        ap = ap[:, 0:n]
        if len(shape) == 2:
            ap = ap.rearrange("p (a b) -> p a b", a=shape[0])
        elif len(shape) == 3:
            ap = ap.rearrange("p (a b c) -> p a b c", a=shape[0], b=shape[1])
        return ap, Buf()

    def init_wstream(s, nslot=12, nstage=4):
        nc = s.nc
        s.wring = [(nc.alloc_sbuf_tensor("wr%d" % i, [128, 8, 256], BF16), Buf()) for i in range(nslot)]
        s.wstage = [(nc.alloc_sbuf_tensor("ws%d" % i, [128, 8, 256], F32), Buf()) for i in range(nstage)]
        s.wri = 0
        s.wsi = 0
        s.wring_owner = [None] * nslot
        s.wblk_id = 0

    def wl(s, units, kc, c0, n=128):
        u = units[kc // 8]
        return u[0][:, kc % 8, c0:c0 + n], u[1]


class WPipe:
    AHEAD = 2

    def __init__(s, B, specs):
        s.B, s.specs = B, specs
        s.units, s.bunits = [], []
        for bi, (W, r0, nk, col0, ncols) in enumerate(specs):
            k, us = 0, []
            while k < nk:
                nku = min(8, nk - k)
                us.append(len(s.units))
                s.units.append((bi, W, r0 + k, nku, col0, ncols))
                k += nku
            s.bunits.append(us)
        s.nd = s.ncs = 0
        s.st, s.rg = {}, {}

    def pump(s, i):
        B = s.B
        NST = len(B.wstage)
        while True:
            if s.nd < len(s.units) and s.nd < s.ncs + NST:
                bi, W, r, nku, col0, ncols = s.units[s.nd]
                st, stb = B.wstage[B.wsi % NST]
                B.wsi += 1
                src = W[r * 128:(r + nku) * 128, col0:col0 + ncols].rearrange("(c p) n -> p c n", p=128)
                B.dma(st[:, 0:nku, 0:ncols], src, [], [stb])
                s.st[s.nd] = (st, stb)
                s.nd += 1
                continue
            if s.ncs < s.nd and s.units[s.ncs][0] <= i + s.AHEAD:
                si = B.wri % len(B.wring)
                if B.wring_owner[si] is None:
                    bi, W, r, nku, col0, ncols = s.units[s.ncs]
                    B.wri += 1
                    B.wring_owner[si] = 1
                    sl, slb = B.wring[si]
                    st, stb = s.st.pop(s.ncs)
                    B.cp('act', sl[:, 0:nku, 0:ncols], st[:, 0:nku, 0:ncols], [stb], [slb])
                    s.rg[s.ncs] = (sl, slb, nku, si)
                    s.ncs += 1
                    continue
            break

    def get(s, i):
        s.pump(i)
        us = s.bunits[i]
        assert all(u in s.rg for u in us), "weight ring stalled"
        return [s.rg[u] for u in us]

    def done(s, i):
        for u in s.bunits[i]:
            s.B.wring_owner[s.rg.pop(u)[3]] = None


def build(cfg):
    B = Builder(cfg)
    nc = B.nc
    c = cfg
    D, FF, ND, NF, T, TT, NSUB = c.D, c.FF, c.ND, c.NF, c.T, c.TT, c.NSUB
    HA, HB, KVB, G = c.HA, c.HB, c.KVB, c.G
    NQT, NKB, NKT = c.NQT, c.NKB, c.NKT
    T2 = 2 * T

    xT_own = B.din("xT_own", [D, T])
    xT_ctx = B.din("xT_ctx", [D, T])
    cT = B.din("cT", [128, ND])
    w_ada = B.din("w_ada", [D, 9 * D])
    b_adaT = B.din("b_adaT", [128, 9 * ND])
    gains = B.din("gains", [128, 4 * ND])
    f1g = B.din("f1g", [D, FF]); f1u = B.din("f1u", [D, FF]); f1d = B.din("f1d", [FF, D])
    w_in = B.din("w_in", [D, c.INC])
    sinks = B.din("sinks", [128, HB])
    w_bm = B.din("w_bm", [HA * 128, D]); w_bs = B.din("w_bs", [HB * 64, D]); w_out = B.din("w_out", [D, D])
    f2g = B.din("f2g", [D, FF]); f2u = B.din("f2u", [D, FF]); f2d = B.din("f2d", [FF, D])
    ropeA = B.din("ropeA", [2, 128, T2])
    ropeB = B.din("ropeB", [2, 128, T2])
    cmask = B.din("cmask", [128, 4 * 128])
    vbias = B.din("vbias", [128, 2 * NQT * NKB])
    outT = nc.dram_tensor("outT", [D, T], F32, kind="ExternalOutput").ap()

    S_x1 = B.dscr("S_x1", [D, T], F32); S_x1c = B.dscr("S_x1c", [D, T], F32)
    S_x2 = B.dscr("S_x2", [D, T], F32); S_x3 = B.dscr("S_x3", [D, T], F32)
    S_tmp = B.dscr("S_tmp", [D, T], F32)
    QA_d = B.dscr("QA_d", [HA * 128, T], BF16)
    KA_d = B.dscr("KA_d", [HA * 128, T2], BF16)
    VA_d = B.dscr("VA_d", [T2, HA * 128], BF16)
    QB_d = B.dscr("QB_d", [64, NQT, HB, 128], BF16)
    KB_d = B.dscr("KB_d", [64, KVB, T2], BF16)
    VB_d = B.dscr("VB_d", [T2, KVB * 64], BF16)
    SGA_d = B.dscr("SGA_d", [D, T], BF16); SGB_d = B.dscr("SGB_d", [D, T], BF16)
    YA_d = B.dscr("YA_d", [HA * 128, T], BF16); YB_d = B.dscr("YB_d", [HB * 64, T], BF16)
    db = {k: Buf(k) for k in ["x1", "x1c", "x2", "x3", "tmp", "qa", "ka", "va", "qb", "kb", "vb", "sga", "sgb", "ya", "yb", "out"]}

    B.init_psum()
    B.init_wstream(12, 4)
    consts = nc.alloc_sbuf_tensor("consts", [128, 4 * 128], F32); consts_b = Buf()
    ones_bf = nc.alloc_sbuf_tensor("ones_bf", [128, 128], BF16); ones_b = Buf()
    MOD = nc.alloc_sbuf_tensor("MOD", [128, 9 * ND], F32); MOD_b = Buf()
    GN = nc.alloc_sbuf_tensor("GN", [128, 4 * ND], F32); GN_b = Buf()
    AB = nc.alloc_sbuf_tensor("AB", [128, 3, 3, ND], F32); AB_b = Buf()
    VBI = nc.alloc_sbuf_tensor("VBI", [128, 2 * NQT * NKB], F32); VBI_b = Buf()
    ESK = nc.alloc_sbuf_tensor("ESK", [128, HB], F32); ESK_b = Buf()
    EPS = nc.alloc_sbuf_tensor("EPS", [128, 1], F32); EPS_b = Buf()
    B.init_arena(118 * 1024)

    TRI = consts[:, 0:128]; MASKP = consts[:, 128:256]; MASKP0 = consts[:, 256:384]; IDENT = consts[:, 384:512]

    B.dma(consts[:], cmask[:, :], [], [consts_b])
    B.dma(GN[:], gains[:, :], [], [GN_b])
    B.dma(VBI[:], vbias[:, :], [], [VBI_b])
    B.dma(ESK[:], sinks[:, :], [], [ESK_b])
    B.memset('dve', ones_bf[:], 1.0, [ones_b])
    B.memset('dve', EPS[:], 1e-6, [EPS_b])
    B.act(ESK[:], ESK[:], AF.Exp, [ESK_b], [ESK_b])

    B.phase()
    csb, csb_b = B.at([ND], F32)
    cbf = nc.alloc_sbuf_tensor("cbf", [128, ND], BF16); cbf_b = Buf()
    bad = nc.alloc_sbuf_tensor("bad", [128, 9 * ND], F32); bad_b = Buf()
    B.dma(csb, cT[:, :], [], [csb_b])
    B.dma(bad[:], b_adaT[:, :], [], [bad_b])
    B.act(cbf[:], csb, AF.Silu, [csb_b], [cbf_b])
    psm, psm_b = B.psres

    def ada_spec(cb):
        return (w_ada, 0, ND, cb * 256, 256)

    def ada_block(cb, u):
        for cc in range(2):
            j = cb * 2 + cc
            for kc in range(ND):
                l, lb = B.wl(u, kc, cc * 128)
                B.mm(psm[:, j:j + 1], l, cbf[:, kc:kc + 1], kc == 0, kc == ND - 1, [lb, cbf_b], [psm_b])

    def ada_finish(ilist):
        lo, hi = 3 * ilist[0] * ND, 3 * (ilist[-1] + 1) * ND
        B.tt('dve', MOD[:, lo:hi], psm[:, lo:hi], bad[:, lo:hi], ALU.add, [psm_b, bad_b], [MOD_b])
        for i in ilist:
            sh = MOD[:, (3 * i) * ND:(3 * i + 1) * ND]
            sc = MOD[:, (3 * i + 1) * ND:(3 * i + 2) * ND]
            gg = MOD[:, (3 * i + 2) * ND:(3 * i + 3) * ND]
            B.stt(AB[:, i, 0, :], sc, 1.0, GN[:, i * ND:(i + 1) * ND], ALU.add, ALU.mult, [MOD_b, GN_b], [AB_b])
            B.cp('dve', AB[:, i, 1, :], sh, [MOD_b], [AB_b])
            B.ts('dve', AB[:, i, 2, :], gg, (1.0 if i == 1 else 0.5), None, ALU.mult, None, [MOD_b], [AB_b])

    NA = 3 * D // 256
    wp = WPipe(B, [ada_spec(cb) for cb in range(NA)])
    for cb in range(NA):
        u = wp.get(cb)
        ada_block(cb, u)
        wp.done(cb)
    ada_finish([0])

    def norm_apply(*a, **k):
        for _ in norm_gen(*a, **k):
            pass

    def norm_gen(src, src_b, t0, hT, hT_b, Acol, Bcol, xr, sq, rstd, rstd_b, tmpf, out_dram=None, out_b=None, ssb=None):
        if ssb is None:
            ssb = [B.ps() for _ in range(NSUB)]
        NX = len(xr)

        def ldx(dc):
            xt, xb = xr[dc % NX]
            B.ld(xt, src[dc * 128:(dc + 1) * 128, t0:t0 + TT], [src_b], [xb])

        for dc in range(min(NX - 1, ND)):
            ldx(dc)
        for dc in range(ND):
            if dc + NX - 1 < ND:
                ldx(dc + NX - 1)
            xt, xb = xr[dc % NX]
            st, sb_ = sq[dc % len(sq)]
            B.act(st, xt, AF.Square, [xb], [sb_])
            for su in range(NSUB):
                B.mm(ssb[su][0][:, :], ones_bf[:], st[:, su * 512:(su + 1) * 512], dc == 0, dc == ND - 1,
                     [ones_b, sb_], [ssb[su][1]])
            yield
        for dc in range(min(NX - 1, ND)):
            ldx(dc)
        for su in range(NSUB):
            r = rstd[:, su * 512:(su + 1) * 512]
            B.act(r, ssb[su][0][:, :], AF.Sqrt, [ssb[su][1], EPS_b], [rstd_b], bias=EPS[:, 0:1], scale=1.0 / D)
            B.R.add('dve', lambda e, r=r: e.reciprocal(r, r), [rstd_b], [rstd_b])
        for dc in range(ND):
            if dc + NX - 1 < ND:
                ldx(dc + NX - 1)
            xt, xb = xr[dc % NX]
            for su in range(NSUB):
                tf, tfb = tmpf[(dc * NSUB + su) % len(tmpf)]
                sl = slice(su * 512, (su + 1) * 512)
                B.tt('dve', tf, xt[:, sl], rstd[:, sl], ALU.mult, [xb, rstd_b], [tfb])
                if out_dram is None:
                    B.act(hT[:, dc, sl], tf, AF.Identity, [tfb, AB_b], [hT_b[dc]],
                          bias=Bcol[:, dc:dc + 1], scale=Acol[:, dc:dc + 1])
                else:
                    B.act(tf, tf, AF.Identity, [tfb, GN_b], [tfb], scale=Acol[:, dc:dc + 1])
                    B.sta(out_dram[dc * 128:(dc + 1) * 128, t0 + su * 512:t0 + (su + 1) * 512], tf, [tfb], [out_b])
            yield

    def ffn_phase(src, src_b, dst, dst_b, ph, Wg, Wu, Wd, ntiles, extra=()):
        B.phase()
        hT, _ = B.at([ND, TT], BF16)
        hT_b = [Buf() for _ in range(ND)]
        NFH = NF // c.FSPLIT
        aT, _ = B.at([NFH, TT], BF16)
        aT_b = [Buf() for _ in range(NFH)]
        xr = [B.at([TT], F32) for _ in range(4)]
        sq = [B.at([TT], BF16) for _ in range(2)]
        rstd, rstd_b = B.at([TT], F32)
        tmpf = [B.at([512], F32) for _ in range(4)]
        xo = [B.at([512], F32) for _ in range(4)]
        Acol, Bcol, Gcol = AB[:, ph, 0, :], AB[:, ph, 1, :], AB[:, ph, 2, :]
        specs, gu_i, dn_i, ex_i = [], {}, {}, {}
        extra = list(extra)
        npairs = ntiles * c.FSPLIT * (NFH // 2)
        pk_ = 0
        for ti in range(ntiles):
            for fs in range(c.FSPLIT):
                for fb in range(NFH // 2):
                    col0 = (fs * NFH + fb * 2) * 128
                    gu_i[(ti, fs, fb)] = len(specs)
                    specs.append((Wg, 0, ND, col0, 256))
                    specs.append((Wu, 0, ND, col0, 256))
                    ex_i[(ti, fs, fb)] = []
                    for e_ in extra[(len(extra) * pk_) // npairs:(len(extra) * (pk_ + 1)) // npairs]:
                        ex_i[(ti, fs, fb)].append((len(specs), e_))
                        specs.append(ada_spec(e_))
                    pk_ += 1
                for db_ in range(ND // 2):
                    dn_i[(ti, fs, db_)] = len(specs)
                    specs.append((Wd, fs * NFH, NFH, db_ * 256, 256))
        wp = WPipe(B, specs)
        wp.pump(0)
        hide = (len(extra) == 0) and NSUB <= 2
        res_ssb = [B.psres, B.psres2][0:NSUB] if hide else None
        gen = None
        for ti in range(ntiles):
            t0 = ti * TT
            if gen is None:
                norm_apply(src, src_b, t0, hT, hT_b, Acol, Bcol, xr, sq, rstd, rstd_b, tmpf, ssb=res_ssb)
            else:
                for _ in gen:
                    pass
                gen = None
            for fs in range(c.FSPLIT):
                for fb in range(NFH // 2):
                    bi = gu_i[(ti, fs, fb)]
                    ug = wp.get(bi)
                    uu = wp.get(bi + 1)
                    for cc in range(2):
                        fl = fb * 2 + cc
                        for su in range(NSUB):
                            sl = slice(su * 512, (su + 1) * 512)
                            pg, pgb = B.ps()
                            pu, pub = B.ps()
                            for kc in range(ND):
                                l, lb = B.wl(ug, kc, cc * 128)
                                B.mm(pg[:, :], l, hT[:, kc, sl], kc == 0, kc == ND - 1, [lb, hT_b[kc]], [pgb])
                            for kc in range(ND):
                                l, lb = B.wl(uu, kc, cc * 128)
                                B.mm(pu[:, :], l, hT[:, kc, sl], kc == 0, kc == ND - 1, [lb, hT_b[kc]], [pub])
                            tf, tfb = tmpf[(fl * NSUB + su) % len(tmpf)]
                            B.act(tf, pg[:, :], AF.Silu, [pgb], [tfb])
                            B.tt('dve', aT[:, fl, sl], tf, pu[:, :], ALU.mult, [tfb, pub], [aT_b[fl]])
                    wp.done(bi)
                    wp.done(bi + 1)
                    for (xi, e_) in ex_i[(ti, fs, fb)]:
                        ada_block(e_, wp.get(xi))
                        wp.done(xi)
                xin, xin_b = (src, src_b) if fs == 0 else (S_tmp, db["tmp"])
                xout, xout_b = (dst, dst_b) if fs == c.FSPLIT - 1 else (S_tmp, db["tmp"])
                if hide and fs == c.FSPLIT - 1 and ti + 1 < ntiles:
                    gen = norm_gen(src, src_b, t0 + TT, hT, hT_b, Acol, Bcol, xr, sq, rstd, rstd_b, tmpf, ssb=res_ssb)
                nstep = -(-(2 * ND) // (ND // 2))
                for db_ in range(ND // 2):
                    bi = dn_i[(ti, fs, db_)]
                    u = wp.get(bi)
                    for cc in range(2):
                        dc = db_ * 2 + cc
                        for su in range(NSUB):
                            sl = slice(t0 + su * 512, t0 + (su + 1) * 512)
                            xt, xtb = xo[(dc * NSUB + su) % len(xo)]
                            B.ld(xt, xin[dc * 128:(dc + 1) * 128, sl], [xin_b], [xtb])
                            pd, pdb = B.ps()
                            for kc in range(NFH):
                                l, lb = B.wl(u, kc, cc * 128)
                                B.mm(pd[:, :], l, aT[:, kc, su * 512:(su + 1) * 512], kc == 0, kc == NFH - 1,
                                     [lb, aT_b[kc]], [pdb])
                            B.stt(xt, pd[:, :], Gcol[:, dc:dc + 1], xt, ALU.mult, ALU.add, [pdb, xtb, AB_b], [xtb])
                            B.dma(xout[dc * 128:(dc + 1) * 128, sl], xt, [xtb], [xout_b])
                    wp.done(bi)
                    if gen is not None and fs == c.FSPLIT - 1:
                        for _ in range(nstep):
                            if next(gen, 'end') == 'end':
                                break

    ffn_phase(xT_ctx, Buf(), S_x1c, db["x1c"], 0, f1g, f1u, f1d, T // TT, extra=range(NA, 9 * D // 256))
    ada_finish([1, 2])
    ffn_phase(xT_own, Buf(), S_x1, db["x1"], 0, f1g, f1u, f1d, T // TT)

    def proj_phase(src, src_b, is_ctx):
        B.phase()
        hT, _ = B.at([ND, TT], BF16)
        hT_b = [Buf() for _ in range(ND)]
        xr = [B.at([TT], F32) for _ in range(4)]
        sq = [B.at([TT], BF16) for _ in range(2)]
        rstd, rstd_b = B.at([TT], F32)
        tmpf = [B.at([512], F32) for _ in range(4)]
        rp = [B.at([2, 512], F32) for _ in range(4)]
        xs = [B.at([512], F32) for _ in range(3)]
        ra = [B.at([512], F32) for _ in range(3)]
        rb = [B.at([512], F32) for _ in range(3)]
        ev = [B.at([512], BF16) for _ in range(8)]
        evi = [0]
        Acol, Bcol = AB[:, 1, 0, :], AB[:, 1, 1, :]
        loc0 = 0 if is_ctx else T
        for ti in range(T // TT):
            t0 = ti * TT
            blocks = []
            if not is_ctx:
                for i in range(HA // 2):
                    blocks.append(("qa", c.oqa + i * 256, 256, i * 2))
            for i in range(HA // 2):
                blocks.append(("ka", c.oka + i * 256, 256, i * 2))
            for i in range(HA // 2):
                blocks.append(("va", c.ova + i * 256, 256, i * 2))
            if not is_ctx:
                for i in range(HB * 64 // 256):
                    blocks.append(("qb", c.oqb + i * 256, 256, i * 2))
            blocks.append(("kb", c.okb, 128, 0))
            blocks.append(("vb", c.ovb, 128, 0))
            if not is_ctx:
                for i in range(ND // 2):
                    blocks.append(("ga", c.oga + i * 256, 256, i * 2))
                for i in range(ND // 2):
                    blocks.append(("gb", c.ogb + i * 256, 256, i * 2))
            wp = WPipe(B, [(w_in, 0, ND, b[1], b[2]) for b in blocks])
            wp.pump(0)
            norm_apply(src, src_b, t0, hT, hT_b, Acol, Bcol, xr, sq, rstd, rstd_b, tmpf)
            ropeslices = {}
            for su in range(NSUB):
                for which, tab in ((0, ropeA), (1, ropeB)):
                    r_, rb_ = rp[su * 2 + which] if NSUB * 2 <= len(rp) else rp[which]
                    lo = loc0 + t0 + su * 512
                    B.ld(r_, tab[:, :, lo:lo + 512].rearrange("a p n -> p a n"), [], [rb_])
                    ropeslices[(su, which)] = (r_, rb_)
            for bi, (kind, col0, ncols, ch0) in enumerate(blocks):
                u = wp.get(bi)
                if kind in ("va", "vb"):
                    for tk in range(TT // 128):
                        pv, pvb = B.ps()
                        for kc in range(ND):
                            r_, rb_ = B.wl(u, kc, 0, ncols)
                            B.mm(pv[:, 0:ncols], hT[:, kc, tk * 128:(tk + 1) * 128], r_, kc == 0, kc == ND - 1,
                                 [rb_, hT_b[kc]], [pvb])
                        e, eb = ev[evi[0] % len(ev)]
                        evi[0] += 1
                        B.cp('act', e[:, 0:ncols], pv[:, 0:ncols], [pvb], [eb])
                        tok = loc0 + t0 + tk * 128
                        if kind == "va":
                            B.sta(VA_d[tok:tok + 128, col0 - c.ova:col0 - c.ova + ncols], e[:, 0:ncols], [eb], [db["va"]])
                        else:
                            B.sta(VB_d[tok:tok + 128, 0:ncols], e[:, 0:ncols], [eb], [db["vb"]])
                    wp.done(bi)
                    continue
                for cc in range(ncols // 128):
                    ch = ch0 + cc
                    for su in range(NSUB):
                        sl = slice(su * 512, (su + 1) * 512)
                        pp, ppb = B.ps()
                        for kc in range(ND):
                            l, lb = B.wl(u, kc, cc * 128)
                            B.mm(pp[:, :], l, hT[:, kc, sl], kc == 0, kc == ND - 1, [lb, hT_b[kc]], [ppb])
                        tok = t0 + su * 512
                        if kind in ("ga", "gb"):
                            e, eb = ev[evi[0] % len(ev)]
                            evi[0] += 1
                            B.act(e, pp[:, :], AF.Sigmoid, [ppb], [eb])
                            dd, ddb = (SGA_d, db["sga"]) if kind == "ga" else (SGB_d, db["sgb"])
                            B.sta(dd[ch * 128:(ch + 1) * 128, tok:tok + 512], e, [eb], [ddb])
                            continue
                        isA = kind in ("qa", "ka")
                        rt, rtb = ropeslices[(su, 0 if isA else 1)]
                        x_, xb_ = xs[evi[0] % 3]
                        a_, ab_ = ra[evi[0] % 3]
                        b_, bb_ = rb[evi[0] % 3]
                        B.cp('act', x_, pp[:, :], [ppb], [xb_])
                        B.tt('pool', a_, x_, rt[:, 0, :], ALU.mult, [xb_, rtb], [ab_])
                        hs = 64 if isA else 32
                        for g0 in range(0, 128, 2 * hs):
                            B.tt('pool', b_[g0:g0 + hs, :], x_[g0 + hs:g0 + 2 * hs, :], rt[g0 + hs:g0 + 2 * hs, 1, :],
                                 ALU.mult, [xb_, rtb], [bb_])
                            B.tt('pool', b_[g0 + hs:g0 + 2 * hs, :], x_[g0:g0 + hs, :], rt[g0:g0 + hs, 1, :],
                                 ALU.mult, [xb_, rtb], [bb_])
                        e, eb = ev[evi[0] % len(ev)]
                        evi[0] += 1
                        if isA:
                            B.tt('dve', e, a_, b_, ALU.add, [ab_, bb_], [eb])
                            if kind == "qa":
                                B.dma(QA_d[ch * 128:(ch + 1) * 128, tok:tok + 512], e, [eb], [db["qa"]])
                            else:
                                B.dma(KA_d[ch * 128:(ch + 1) * 128, loc0 + tok:loc0 + tok + 512], e, [eb], [db["ka"]])
                        else:
                            B.tt('dve', e[0:64, 0:512], a_[0:64, :], b_[0:64, :], ALU.add, [ab_, bb_], [eb])
                            e2, eb2 = ev[evi[0] % len(ev)]
                            evi[0] += 1
                            B.tt('dve', e2[0:64, 0:512], a_[64:128, :], b_[64:128, :], ALU.add, [ab_, bb_], [eb2])
                            for hh, (ee, eeb) in enumerate(((e, eb), (e2, eb2))):
                                if kind == "qb":
                                    head = ch * 2 + hh
                                    B.dma(QB_d[:, tok // 128:tok // 128 + 4, head, :],
                                          ee[0:64, 0:512].rearrange("p (a b) -> p a b", a=4), [eeb], [db["qb"]])
                                else:
                                    B.dma(KB_d[:, hh, loc0 + tok:loc0 + tok + 512], ee[0:64, 0:512], [eeb], [db["kb"]])
                wp.done(bi)

    proj_phase(S_x1c, db["x1c"], True)
    proj_phase(S_x1, db["x1"], False)

    B.phase()
    scaleA = 128.0 ** -0.5
    KT = [B.at([T2], BF16) for _ in range(2)]
    V1 = [B.at([NKT, 132], BF16) for _ in range(2)]
    QT = [B.at([T], BF16) for _ in range(2)]
    kmf = [B.at([NKB], F32) for _ in range(2)]
    kmT = [B.at([NKB], BF16) for _ in range(2)]
    gsm = [B.at([NKB], F32) for _ in range(2)]
    top8 = [B.at([8], F32) for _ in range(2)]
    SELs = [B.at([NQT, NKB], F32) for _ in range(2)]
    Pt = [B.at([256], BF16) for _ in range(8)]
    acc = [B.at([132], F32) for _ in range(4)]
    rec = [B.at([1], F32) for _ in range(2)]
    yt = [B.at([128], F32) for _ in range(2)]
    ytT = [B.at([256], BF16) for _ in range(2)]
    for vv, vb_ in V1:
        B.memset('pool', vv[:, :, 128:129], 1.0, [vb_])
    pti = [0]
    VBb = VBI[:, 0:NQT * NKB].rearrange("p (a b) -> p a b", a=NQT)
    VBv = VBI[:, NQT * NKB:2 * NQT * NKB].rearrange("p (a b) -> p a b", a=NQT)

    def load_head(h):
        kt, ktb = KT[h % 2]
        v1, v1b = V1[h % 2]
        qt, qtb = QT[h % 2]
        B.ld(kt, KA_d[h * 128:(h + 1) * 128, :], [db["ka"]], [ktb])
        B.ld(v1[:, :, 0:128], VA_d[:, h * 128:(h + 1) * 128].rearrange("(n p) d -> p n d", p=128), [db["va"]], [v1b])
        B.ld(qt, QA_d[h * 128:(h + 1) * 128, :], [db["qa"]], [qtb])

    def gate_steps(h):
        kt, ktb = KT[h % 2]
        qt, qtb = QT[h % 2]
        kf, kfb = kmf[h % 2]
        km, kmb = kmT[h % 2]
        SEL, SEL_b = SELs[h % 2]
        steps = []

        def s0():
            B.R.add('dve', lambda e: e.tensor_reduce(kf, kt.rearrange("p (n k) -> p n k", k=256), AX.X, ALU.add),
                    [ktb], [kfb])
            B.ts('dve', km, kf, 1.0 / 256, None, ALU.mult, None, [kfb], [kmb])
        steps.append(s0)
        for q in range(NQT):
            def sq_(q=q):
                pg, pgb = B.ps()
                B.mm(pg[:, 0:NKB], qt[:, q * 128:(q + 1) * 128], km, True, True, [qtb, kmb], [pgb])
                g_, gb_ = gsm[q % 2]
                t8, t8b = top8[q % 2]
                B.tt('dve', g_, pg[:, 0:NKB], VBb[:, q, :], ALU.add, [pgb, VBI_b], [gb_])
                B.R.add('dve', lambda e: e.max(t8, g_), [gb_], [t8b])
                B.ts('dve', g_, g_, t8[:, c.TOPK - 1:c.TOPK], None, ALU.is_ge, None, [gb_, t8b], [gb_])
                B.tt('dve', SEL[:, q, :], g_, VBv[:, q, :], ALU.mult, [gb_, VBI_b], [SEL_b])
            steps.append(sq_)
        return steps

    def moba_items(h):
        kt, ktb = KT[h % 2]
        v1, v1b = V1[h % 2]
        qt, qtb = QT[h % 2]
        SEL, SEL_b = SELs[h % 2]
        items = []
        for j in range(T // 256):
            nb = T // 256 + j
            q0 = j * 256
            accs = (acc[(2 * j) % 4], acc[(2 * j + 1) % 4])
            k0 = 2 * nb

            def A_own(q0=q0, k0=k0):
                s0, s0b = B.ps()
                B.mm(s0[:, 0:256], kt[:, k0 * 128:(k0 + 1) * 128], qt[:, q0:q0 + 256], True, True, [ktb, qtb], [s0b])
                B.mm(s0[:, 256:384], kt[:, (k0 + 1) * 128:(k0 + 2) * 128], qt[:, q0 + 128:q0 + 256], True, True, [ktb, qtb], [s0b])
                p0, p0b = Pt[pti[0] % 8]; pti[0] += 1
                p1, p1b = Pt[pti[0] % 8]; pti[0] += 1
                B.act(p0, s0[:, 0:256], AF.Exp, [s0b], [p0b], scale=scaleA)
                B.act(p1[:, 0:128], s0[:, 256:384], AF.Exp, [s0b], [p1b], scale=scaleA)
                B.tt('pool', p0[:, 0:128], p0[:, 0:128], TRI, ALU.mult, [p0b, consts_b], [p0b])
                B.tt('pool', p1[:, 0:128], p1[:, 0:128], TRI, ALU.mult, [p1b, consts_b], [p1b])
                return (p0, p0b, p1, p1b)

            def B_own(st, k0=k0, accs=accs):
                p0, p0b, p1, p1b = st
                oo, oob = B.ps()
                B.mm(oo[:, 0:129], p0[:, 0:128], v1[:, k0, 0:129], True, True, [p0b, v1b], [oob])
                B.mm(oo[:, 256:385], p0[:, 128:256], v1[:, k0, 0:129], True, False, [p0b, v1b], [oob])
                B.mm(oo[:, 256:385], p1[:, 0:128], v1[:, k0 + 1, 0:129], False, True, [p1b, v1b], [oob])
                B.cp('dve', accs[0][0][:, 0:129], oo[:, 0:129], [oob], [accs[0][1]])
                B.cp('dve', accs[1][0][:, 0:129], oo[:, 256:385], [oob], [accs[1][1]])
            items.append((A_own, B_own))
            for n in range(nb):
                def A_p(n=n, q0=q0):
                    sp_, spb = B.ps()
                    pk = []
                    for kk in range(2):
                        ktile = 2 * n + kk
                        B.mm(sp_[:, kk * 256:(kk + 1) * 256], kt[:, ktile * 128:(ktile + 1) * 128], qt[:, q0:q0 + 256], True, True,
                             [ktb, qtb], [spb])
                    for kk in range(2):
                        pp_, ppb = Pt[pti[0] % 8]; pti[0] += 1
                        B.act(pp_, sp_[:, kk * 256:(kk + 1) * 256], AF.Exp, [spb], [ppb], scale=scaleA)
                        pk.append((pp_, ppb, 2 * n + kk))
                    return pk

                def B_p(pk, n=n, j=j, q0=q0, accs=accs, last=(n == nb - 1)):
                    oo, oob = B.ps()
                    for qs in range(2):
                        for kk in range(2):
                            pp_, ppb, ktile = pk[kk]
                            B.mm(oo[:, qs * 256:qs * 256 + 129], pp_[:, qs * 128:(qs + 1) * 128], v1[:, ktile, 0:129],
                                 kk == 0, kk == 1, [ppb, v1b], [oob])
                    for qs in range(2):
                        aa, aab = accs[qs]
                        B.stt(aa[:, 0:129], oo[:, qs * 256:qs * 256 + 129], SEL[:, 2 * j + qs, n:n + 1], aa[:, 0:129],
                              ALU.mult, ALU.add, [oob, SEL_b, aab], [aab])
                    if last:
                        yT, yTb = ytT[j % 2]
                        for qs in range(2):
                            aa, aab = accs[qs]
                            r_, rb_ = rec[qs]
                            y_, yb_ = yt[qs]
                            B.R.add('dve', lambda e, r_=r_, aa=aa: e.reciprocal(r_, aa[:, 128:129]), [aab], [rb_])
                            B.ts('dve', y_, aa[:, 0:128], r_[:, 0:1], None, ALU.mult, None, [aab, rb_], [yb_])
                            pt_, ptb = B.ps()
                            B.tr(pt_[:, 0:128], y_, IDENT, [yb_, consts_b], [ptb])
                            B.cp('act', yT[:, qs * 128:(qs + 1) * 128], pt_[:, 0:128], [ptb], [yTb])
                        B.sta(YA_d[h * 128:(h + 1) * 128, q0:q0 + 256], yT, [yTb], [db["ya"]])
                items.append((A_p, B_p))
        return items

    load_head(0)
    for st_ in gate_steps(0):
        st_()
    for h in range(HA):
        gs_next = []
        if h + 1 < HA:
            load_head(h + 1)
            gs_next = gate_steps(h + 1)
        items = moba_items(h)
        every = max(1, len(items) // (len(gs_next) + 1)) if gs_next else 0
        st = items[0][0]()
        for k in range(len(items)):
            nst = items[k + 1][0]() if k + 1 < len(items) else None
            items[k][1](st)
            st = nst
            if gs_next and k % every == every - 1 and k > len(items) // 8:
                gs_next.pop(0)()
        while gs_next:
            gs_next.pop(0)()

    B.phase()
    scaleB = 64.0 ** -0.5
    KBT, KBT_b = B.at([KVB, T2], BF16)
    VB1, VB1_b = B.at([NKT, KVB, 66], BF16)
    QBT = [B.at([HB, 128], BF16) for _ in range(2)]
    Pb = [B.at([512], BF16) for _ in range(6)]
    den = [B.at([4], F32) for _ in range(2)]
    ybt = [B.at([HB * 64], F32) for _ in range(2)]
    ybT = [B.at([HB * 64 // 128, 128], BF16) for _ in range(2)]
    B.ld(KBT[0:64], KB_d[:, :, :], [db["kb"]], [KBT_b])
    B.memset('pool', VB1[:, :, :, 64:65], 1.0, [VB1_b])
    for g in range(KVB):
        B.ld(VB1[:, :, g, 0:64], VB_d[:, g * 64:(g + 1) * 64].rearrange("(n p) d -> p n d", p=128), [db["vb"]], [VB1_b])
    HG = min(4, G)
    pbi = [0]
    B.ld(QBT[0][0][0:64], QB_d[:, 0, :, :], [db["qb"]], [QBT[0][1]])
    sitems = []
    for i in range(NQT):
        cur = T // 128 + i
        prv = cur - 1
        for g in range(KVB):
            for hg in range(G // HG):
                first = (g == 0 and hg == 0)
                lastit = (g == KVB - 1 and hg == G // HG - 1)

                def A_s(i=i, g=g, hg=hg, cur=cur, prv=prv, first=first):
                    qb_, qbb = QBT[i % 2]
                    if first and i + 1 < NQT:
                        B.ld(QBT[(i + 1) % 2][0][0:64], QB_d[:, i + 1, :, :], [db["qb"]], [QBT[(i + 1) % 2][1]])
                    h0 = g * G + hg * HG
                    rhs = qb_[0:64, h0:h0 + HG, :]
                    ps_ = []
                    for (ktile, msk) in ((prv, MASKP0 if i == 0 else MASKP), (cur, TRI)):
                        sp_, spb = B.ps()
                        B.mm(sp_[:, 0:HG * 128], KBT[0:64, g, ktile * 128:(ktile + 1) * 128], rhs, True, True, [KBT_b, qbb], [spb])
                        pp_, ppb = Pb[pbi[0] % 6]; pbi[0] += 1
                        B.act(pp_[:, 0:HG * 128], sp_[:, 0:HG * 128], AF.Exp, [spb], [ppb], scale=scaleB)
                        pv = pp_[:, 0:HG * 128].rearrange("p (h q) -> p h q", h=HG)
                        B.tt('pool', pv, pv, msk.unsqueeze(1).to_broadcast([128, HG, 128]), ALU.mult, [ppb, consts_b], [ppb])
                        ps_.append((pp_, ppb, ktile))
                    return ps_

                def B_s(ps_, i=i, g=g, hg=hg, lastit=lastit):
                    y_, yb_ = ybt[i % 2]
                    h0 = g * G + hg * HG
                    oo, oob = B.ps()
                    for hh in range(HG):
                        for kk in range(2):
                            pp_, ppb, ktile = ps_[kk]
                            B.mm(oo[:, hh * 65:hh * 65 + 65], pp_[:, hh * 128:(hh + 1) * 128], VB1[:, ktile, g, 0:65],
                                 kk == 0, kk == 1, [ppb, VB1_b], [oob])
                    d_, db_ = den[(g * (G // HG) + hg) % 2]
                    ov = oo[:, 0:HG * 65].rearrange("p (h d) -> p h d", h=HG)
                    B.tt('dve', d_[:, 0:HG], ov[:, :, 64], ESK[:, h0:h0 + HG], ALU.add, [oob, ESK_b], [db_])
                    B.R.add('dve', lambda e, d_=d_: e.reciprocal(d_[:, 0:HG], d_[:, 0:HG]), [db_], [db_])
                    yv = y_[:, h0 * 64:(h0 + HG) * 64].rearrange("p (h d) -> p h d", h=HG)
                    B.tt('dve', yv, ov[:, :, 0:64], d_[:, 0:HG].unsqueeze(2).to_broadcast([128, HG, 64]), ALU.mult,
                         [oob, db_], [yb_])
                    if lastit:
                        yT, yTb = ybT[i % 2]
                        for ch in range(HB * 64 // 128):
                            pt_, ptb = B.ps()
                            B.tr(pt_[:, 0:128], y_[:, ch * 128:(ch + 1) * 128], IDENT, [yb_, consts_b], [ptb])
                            B.cp('act', yT[:, ch, :], pt_[:, 0:128], [ptb], [yTb])
                        B.sta(YB_d[:, i * 128:(i + 1) * 128].rearrange("(c p) n -> p c n", p=128), yT, [yTb], [db["yb"]])
                sitems.append((A_s, B_s))
    st = sitems[0][0]()
    for k in range(len(sitems)):
        nst = sitems[k + 1][0]() if k + 1 < len(sitems) else None
        sitems[k][1](st)
        st = nst

    B.phase()
    NCA, NCB = HA, HB * 64 // 128
    yaT, yaT_b = B.at([NCA, TT], BF16)
    ybT2, ybT2_b = B.at([NCB, TT], BF16)
    mT, _ = B.at([ND, TT], BF16)
    mT_b = [Buf() for _ in range(ND)]
    sg = [B.at([2, 512], BF16) for _ in range(3)]
    tm = [B.at([2, 512], F32) for _ in range(3)]
    xo = [B.at([512], F32) for _ in range(4)]
    Gcol = AB[:, 1, 2, :]
    k_i = [0]
    for ti in range(T // TT):
        t0 = ti * TT
        B.ld(yaT, YA_d[:, t0:t0 + TT].rearrange("(c p) n -> p c n", p=128), [db["ya"]], [yaT_b])
        B.ld(ybT2, YB_d[:, t0:t0 + TT].rearrange("(c p) n -> p c n", p=128), [db["yb"]], [ybT2_b])
        specs = []
        for d2 in range(ND // 2):
            specs.append((w_bm, 0, NCA, d2 * 256, 256))
            specs.append((w_bs, 0, NCB, d2 * 256, 256))
        wp = WPipe(B, specs)
        for d2 in range(ND // 2):
            um = wp.get(2 * d2)
            us = wp.get(2 * d2 + 1)
            for cc in range(2):
                dc = d2 * 2 + cc
                for su in range(NSUB):
                    sl = slice(su * 512, (su + 1) * 512)
                    tok = t0 + su * 512
                    s_, sb_ = sg[k_i[0] % 3]
                    t_, tb_ = tm[k_i[0] % 3]
                    k_i[0] += 1
                    B.ld(s_[:, 0, :], SGA_d[dc * 128:(dc + 1) * 128, tok:tok + 512], [db["sga"]], [sb_])
                    B.ld(s_[:, 1, :], SGB_d[dc * 128:(dc + 1) * 128, tok:tok + 512], [db["sgb"]], [sb_])
                    pm, pmb = B.ps()
                    pq, pqb = B.ps()
                    for kc in range(NCA):
                        l, lb = B.wl(um, kc, cc * 128)
                        B.mm(pm[:, :], l, yaT[:, kc, sl], kc == 0, kc == NCA - 1, [lb, yaT_b], [pmb])
                    for kc in range(NCB):
                        l, lb = B.wl(us, kc, cc * 128)
                        B.mm(pq[:, :], l, ybT2[:, kc, sl], kc == 0, kc == NCB - 1, [lb, ybT2_b], [pqb])
                    B.tt('dve', t_[:, 0, :], pm[:, :], s_[:, 0, :], ALU.mult, [pmb, sb_], [tb_])
                    B.tt('dve', t_[:, 1, :], pq[:, :], s_[:, 1, :], ALU.mult, [pqb, sb_], [tb_])
                    B.tt('pool', mT[:, dc, sl], t_[:, 0, :], t_[:, 1, :], ALU.add, [tb_], [mT_b[dc]])
            wp.done(2 * d2)
            wp.done(2 * d2 + 1)
        specs = [(w_out, 0, ND, d2 * 256, 256) for d2 in range(ND // 2)]
        wp = WPipe(B, specs)
        for d2 in range(ND // 2):
            u = wp.get(d2)
            for cc in range(2):
                dc = d2 * 2 + cc
                for su in range(NSUB):
                    sl = slice(t0 + su * 512, t0 + (su + 1) * 512)
                    xt, xtb = xo[(dc * NSUB + su) % len(xo)]
                    B.ld(xt, S_x1[dc * 128:(dc + 1) * 128, sl], [db["x1"]], [xtb])
                    pd, pdb = B.ps()
                    for kc in range(ND):
                        l, lb = B.wl(u, kc, cc * 128)
                        B.mm(pd[:, :], l, mT[:, kc, su * 512:(su + 1) * 512], kc == 0, kc == ND - 1, [lb, mT_b[kc]], [pdb])
                    B.stt(xt, pd[:, :], Gcol[:, dc:dc + 1], xt, ALU.mult, ALU.add, [pdb, xtb, AB_b], [xtb])
                    B.dma(S_x2[dc * 128:(dc + 1) * 128, sl], xt, [xtb], [db["x2"]])
            wp.done(d2)

    ffn_phase(S_x2, db["x2"], S_x3, db["x3"], 2, f2g, f2u, f2d, T // TT)
    B.phase()
    xr = [B.at([TT], F32) for _ in range(4)]
    sq = [B.at([TT], BF16) for _ in range(2)]
    rstd, rstd_b = B.at([TT], F32)
    tmpf = [B.at([512], F32) for _ in range(4)]
    for ti in range(T // TT):
        norm_apply(S_x3, db["x3"], ti * TT, None, None, GN[:, 3 * ND:4 * ND], None, xr, sq, rstd, rstd_b, tmpf,
                   out_dram=outT, out_b=db["out"])
    B.R.finish(nc)
    return nc


def rope_tables(cfg, half):
    T = cfg.T
    pos_ctx = np.arange(0, T, dtype=np.float64)
    pos_own = np.arange(half * T, (half + 1) * T, dtype=np.float64)
    pos = np.concatenate([pos_ctx, pos_own])
    out = []
    for hs in (64, 32):
        inv = 10000.0 ** (-np.arange(hs, dtype=np.float64) / hs)
        ang = (pos.astype(np.float32)[None, :] * inv.astype(np.float32)[:, None]).astype(np.float32)
        cos = np.cos(ang).astype(np.float32)
        sin = np.sin(ang).astype(np.float32)
        reps = 128 // (2 * hs)
        cosT = np.concatenate([cos, cos] * reps, axis=0)
        sinT = np.concatenate([sin, -sin] * reps, axis=0)
        out.append(np.stack([cosT, sinT]).astype(np.float32))
    return out


def const_masks(cfg, half):
    k = np.arange(128)[:, None]
    q = np.arange(128)[None, :]
    tri = (k <= q).astype(np.float32)
    mp = (k > q).astype(np.float32)
    mp0 = mp * float(half)
    ident = np.eye(128, dtype=np.float32)
    cm = np.concatenate([tri, mp, mp0, ident], axis=1)
    NQT, NKB = cfg.NQT, cfg.NKB
    valid = np.zeros((NQT, NKB), np.float32)
    for qt in range(NQT):
        j = qt // 2
        for n in range(NKB):
            if n < NKB // 2:
                valid[qt, n] = float(half)
            elif n < NKB // 2 + j:
                valid[qt, n] = 1.0
    bias = np.where(valid > 0, 0.0, NEG).astype(np.float32)
    vb = np.concatenate([bias.reshape(-1), valid.reshape(-1)])[None, :].repeat(128, 0).astype(np.float32)
    return cm, vb


def colT(v, n):
    return np.ascontiguousarray(np.asarray(v, np.float32).reshape(n, 128).T)


def make_in_maps(cfg, inp):
    D, ND, T = cfg.D, cfg.ND, cfg.T
    f = lambda a: np.ascontiguousarray(np.asarray(a, np.float32))
    x = np.asarray(inp["x"], np.float32)
    shared = {
        "w_ada": f(inp["w_ada"][0]), "b_adaT": colT(inp["b_ada"][0], 9 * ND),
        "gains": np.concatenate([colT(inp["norm_ffn1"][0], ND), colT(inp["norm_mix"][0], ND),
                                 colT(inp["norm_ffn2"][0], ND), colT(inp["norm_final"], ND)], axis=1),
        "f1g": f(inp["ffn1_gate"][0]), "f1u": f(inp["ffn1_up"][0]), "f1d": f(inp["ffn1_down"][0]),
        "w_in": f(inp["w_in"][0]),
        "sinks": np.ascontiguousarray(np.broadcast_to(np.asarray(inp["swa_sinks"][0], np.float32)[None, :], (128, cfg.HB))),
        "w_bm": f(inp["w_branch_moba"][0]), "w_bs": f(inp["w_branch_swa"][0]), "w_out": f(inp["w_out"][0]),
        "f2g": f(inp["ffn2_gate"][0]), "f2u": f(inp["ffn2_up"][0]), "f2d": f(inp["ffn2_down"][0]),
    }
    maps = []
    for core in range(2 * cfg.BATCH):
        b, half = core // 2, core % 2
        rA, rB = rope_tables(cfg, half)
        cm, vb = const_masks(cfg, half)
        m = dict(shared)
        m["xT_own"] = np.ascontiguousarray(x[b, half * T:(half + 1) * T, :].T)
        m["xT_ctx"] = np.ascontiguousarray(x[b, 0:T, :].T)
        m["cT"] = colT(inp["c"][b], ND)
        m["ropeA"], m["ropeB"], m["cmask"], m["vbias"] = rA, rB, cm, vb
        maps.append(m)
    return maps


def assemble(cfg, results):
    out = np.empty((cfg.BATCH, cfg.SEQ, cfg.D), np.float32)
    for core, r in enumerate(results):
        b, half = core // 2, core % 2
        out[b, half * cfg.T:(half + 1) * cfg.T, :] = np.asarray(r["outT"], np.float32).T
    return out


def kernel(**inputs):
    cfg = Cfg()
    nc = build(cfg)
    maps = make_in_maps(cfg, inputs)
    res = run_bass_kernel_spmd(nc, maps, core_ids=list(range(8)))
    return assemble(cfg, res.results)
```

```python
import numpy as np
import ml_dtypes
import concourse.bass as bass
import concourse.mybir as mybir
from concourse.bass_utils import run_bass_kernel_spmd

F32 = mybir.dt.float32
BF16 = mybir.dt.bfloat16
ALU = mybir.AluOpType
AF = mybir.ActivationFunctionType
AX = mybir.AxisListType
NEG = -1.0e30


class Cfg:
    def __init__(s, D=2048, FF=5632, HA=8, HB=16, KVB=2, SEQ=4096, BATCH=4, TT=1024, FSPLIT=2):
        s.D, s.FF, s.HA, s.HB, s.KVB, s.SEQ, s.BATCH, s.TT, s.FSPLIT = D, FF, HA, HB, KVB, SEQ, BATCH, TT, FSPLIT
        s.T = SEQ // 2
        s.ND = D // 128
        s.NF = FF // 128
        s.BLK, s.W, s.TOPK = 256, 128, 3
        s.G = HB // KVB
        s.oqa = 0
        s.oka = HA * 128
        s.ova = 2 * HA * 128
        s.oqb = 3 * HA * 128
        s.okb = s.oqb + HB * 64
        s.ovb = s.okb + KVB * 64
        s.oga = s.ovb + KVB * 64
        s.ogb = s.oga + D
        s.INC = s.ogb + D
        s.NSUB = TT // 512
        s.NQT = s.T // 128
        s.NKB = 2 * s.T // 256
        s.NKT = 2 * s.T // 128
        assert KVB == 2 and s.T % TT == 0 and TT % 512 == 0 and s.NF % FSPLIT == 0


class Buf:
    __slots__ = ("n", "w", "r")

    def __init__(s, n=""):
        s.n = n
        s.w = {}
        s.r = {}


class Op:
    __slots__ = ("eng", "fn", "deps", "sig", "val", "dsem", "dval", "waits", "isdma")


class Rec:
    NDS = 40
    QPOOL = {'sp': (0, 16), 'act': (16, 24)}

    def __init__(s):
        s.ops = []
        s.last = {}
        s.bar = None
        s.bar_done = set()
        s.ndma = 0
        s.dma_last = {}
        s.qcnt = {}

    def add(s, eng, fn, reads=(), writes=(), dma=False):
        op = Op()
        op.eng, op.fn, op.isdma, op.sig, op.deps = eng, fn, dma, False, set()
        op.val = op.dsem = op.dval = None
        for b in reads:
            for k, v in b.w.items():
                if k == 'dma':
                    op.deps.update(v)
                elif k == eng and not dma and eng == 'pe':
                    pass
                else:
                    op.deps.add(v)
        for b in writes:
            for k, v in b.r.items():
                if k == 'dma':
                    op.deps.update(v)
                elif k == eng and not dma:
                    pass
                else:
                    op.deps.add(v)
            for k, v in b.w.items():
                if k == 'dma':
                    op.deps.update(v)
                elif k == eng and not dma:
                    pass
                else:
                    op.deps.add(v)
        if s.bar is not None and eng not in s.bar_done:
            op.deps.update(s.bar)
            s.bar_done.add(eng)
        if dma:
            base, cnt_ = s.QPOOL[eng]
            nq = s.qcnt.get(eng, 0)
            s.qcnt[eng] = nq + 1
            op.dsem = base + nq % cnt_
            op.dval = 16 * (nq // cnt_ + 1)
            s.ndma += 1
            prev = s.dma_last.get(op.dsem)
            if prev is not None:
                op.deps.add(prev)
            s.dma_last[op.dsem] = op
            op.sig = True
        for b in reads:
            if dma:
                b.r.setdefault('dma', []).append(op)
            else:
                b.r[eng] = op
        for b in writes:
            if b.r:
                b.w = {}
                b.r = {}
            if dma:
                b.w.setdefault('dma', []).append(op)
            else:
                b.w[eng] = op
        if not dma:
            s.last[eng] = op
        s.ops.append(op)
        return op

    def barrier(s):
        s.bar = set(s.last.values()) | set(s.dma_last.values())
        s.bar_done = set()

    def finish(s, nc):
        for op in s.ops:
            for d in op.deps:
                d.sig = True
        cnt = {}
        for op in s.ops:
            if op.isdma:
                continue
            if op.sig:
                cnt[op.eng] = cnt.get(op.eng, 0) + 1
                op.val = cnt[op.eng]
        engs = ['pe', 'act', 'dve', 'pool', 'sp']
        esem = {e: nc.alloc_semaphore("sem_" + e) for e in engs if e != 'sp'}
        dsem = [nc.alloc_semaphore("dsem%d" % i) for i in range(s.NDS)]
        per = {e: [] for e in engs}
        for op in s.ops:
            per[op.eng].append(op)
        for e in engs:
            waited = {}
            for op in per[e]:
                need = {}
                for d in op.deps:
                    if d.isdma:
                        k, v = ('d', d.dsem), d.dval
                    else:
                        k, v = ('e', d.eng), d.val
                    if v > need.get(k, 0):
                        need[k] = v
                op.waits = []
                for k, v in need.items():
                    if v > waited.get(k, 0):
                        waited[k] = v
                        op.waits.append((k, v))
        dfinal = {}
        for op in s.ops:
            if op.isdma:
                dfinal[op.dsem] = op.dval

        def body_for(e):
            def body(eng):
                for op in per[e]:
                    for (k, v) in op.waits:
                        eng.wait_ge(dsem[k[1]] if k[0] == 'd' else esem[k[1]], v)
                    ins = op.fn(eng)
                    if op.isdma:
                        ins.then_inc(dsem[op.dsem], 16)
                    elif op.sig:
                        ins.then_inc(esem[e], 1)
                if e == 'sp':
                    for k, v in dfinal.items():
                        eng.wait_ge(dsem[k], v)
                    for e2 in esem:
                        if cnt.get(e2, 0) > 0:
                            eng.wait_ge(esem[e2], cnt[e2])
            return body

        with nc.Block() as blk:
            blk.tensor(body_for('pe'))
            blk.scalar(body_for('act'))
            blk.vector(body_for('dve'))
            blk.gpsimd(body_for('pool'))
            blk.sync(body_for('sp'))


class Builder:
    def __init__(s, cfg):
        s.c = cfg
        s.nc = bass.Bass("TRN2", target_bir_lowering=False)
        s.R = Rec()
        s.dram = {}
        s.nbuf = 0
        s.cast_rr = 0

    def din(s, name, shape, dt=F32):
        t = s.nc.dram_tensor(name, list(shape), dt, kind="ExternalInput").ap()
        s.dram[name] = t
        return t

    def dscr(s, name, shape, dt):
        return s.nc.dram_tensor(name, list(shape), dt, kind="Internal").ap()

    def buf(s, n=""):
        return Buf(n)

    def mm(s, out, lhsT, rhs, start, stop, reads, writes):
        s.R.add('pe', lambda e: e.matmul(out, lhsT, rhs, start=start, stop=stop), reads, writes)

    def tr(s, out, in_, ident, reads, writes):
        s.R.add('pe', lambda e: e.transpose(out, in_, ident), reads, writes)

    def dma(s, out, in_, reads, writes, q='sp'):
        s.R.add(q, lambda e: e.dma_start(out=out, in_=in_), reads, writes, dma=True)

    def sta(s, out, in_, reads, writes):
        s.dma(out, in_, reads, writes, q='act')

    def ld(s, out, in_, reads, writes):
        s.dma(out, in_, reads, writes, q='act')

    def act(s, out, in_, func, reads, writes, bias=None, scale=None, accum_out=None):
        kw = {}
        if bias is not None:
            kw['bias'] = bias
        if scale is not None:
            kw['scale'] = scale
        if accum_out is not None:
            kw['accum_out'] = accum_out
        s.R.add('act', lambda e: e.activation(out, in_, func, **kw), reads, writes)

    def tt(s, eng, out, in0, in1, op, reads, writes):
        s.R.add(eng, lambda e: e.tensor_tensor(out, in0, in1, op), reads, writes)

    def ts(s, eng, out, in0, s1, s2, op0, op1, reads, writes):
        if op1 is None:
            s.R.add(eng, lambda e: e.tensor_scalar(out, in0, s1, None, op0), reads, writes)
        else:
            s.R.add(eng, lambda e: e.tensor_scalar(out, in0, s1, s2, op0, op1), reads, writes)

    def stt(s, out, in0, scalar, in1, op0, op1, reads, writes):
        s.R.add('dve', lambda e: e.scalar_tensor_tensor(out, in0, scalar, in1, op0, op1), reads, writes)

    def cp(s, eng, out, in_, reads, writes):
        if eng == 'act':
            s.R.add('act', lambda e: e.activation(out, in_, AF.Copy), reads, writes)
        else:
            s.R.add(eng, lambda e: e.tensor_copy(out, in_), reads, writes)

    def memset(s, eng, ap, val, writes):
        s.R.add(eng, lambda e: e.memset(ap, val), (), writes)

    def init_psum(s):
        s.psb = [(s.nc.alloc_psum_tensor("psb%d" % i, [128, 512], F32), Buf("ps%d" % i)) for i in range(8)]
        s.psres = s.psb.pop()
        s.psres2 = s.psb.pop()
        s.psi = 0

    def ps(s):
        t = s.psb[s.psi % len(s.psb)]
        s.psi += 1
        return t

    def init_arena(s, nbytes):
        s.arena = s.nc.alloc_sbuf_tensor("arena", [128, nbytes // 4], F32)
        s.arena_n = nbytes // 4
        s.aoff = 0

    def phase(s):
        s.R.barrier()
        s.aoff = 0

    def at(s, shape, dt):
        n = int(np.prod(shape))
        nw = (n * (2 if dt == BF16 else 4) + 3) // 4
        nw = (nw + 7) // 8 * 8
        assert s.aoff + nw <= s.arena_n, ("arena overflow", s.aoff, nw, s.arena_n)
        ap = s.arena[:, s.aoff:s.aoff + nw]
        s.aoff += nw
        if dt == BF16:
            ap = ap.bitcast(BF16)
        ap = ap[:, 0:n]
        if len(shape) == 2:
            ap = ap.rearrange("p (a b) -> p a b", a=shape[0])
        elif len(shape) == 3:
            ap = ap.rearrange("p (a b c) -> p a b c", a=shape[0], b=shape[1])
        return ap, Buf()

    def init_wstream(s, nslot=12, nstage=4):
        nc = s.nc
        s.wring = [(nc.alloc_sbuf_tensor("wr%d" % i, [128, 8, 256], BF16), Buf()) for i in range(nslot)]
        s.wstage = [(nc.alloc_sbuf_tensor("ws%d" % i, [128, 8, 256], F32), Buf()) for i in range(nstage)]
        s.wri = 0
        s.wsi = 0
        s.wring_owner = [None] * nslot
        s.wblk_id = 0

    def wl(s, units, kc, c0, n=128):
        u = units[kc // 8]
        return u[0][:, kc % 8, c0:c0 + n], u[1]


class WPipe:
    AHEAD = 2

    def __init__(s, B, specs):
        s.B, s.specs = B, specs
        s.units, s.bunits = [], []
        for bi, (W, r0, nk, col0, ncols) in enumerate(specs):
            k, us = 0, []
            while k < nk:
                nku = min(8, nk - k)
                us.append(len(s.units))
                s.units.append((bi, W, r0 + k, nku, col0, ncols))
                k += nku
            s.bunits.append(us)
        s.nd = s.ncs = 0
        s.st, s.rg = {}, {}

    def pump(s, i):
        B = s.B
        NST = len(B.wstage)
        while True:
            if s.nd < len(s.units) and s.nd < s.ncs + NST:
                bi, W, r, nku, col0, ncols = s.units[s.nd]
                st, stb = B.wstage[B.wsi % NST]
                B.wsi += 1
                src = W[r * 128:(r + nku) * 128, col0:col0 + ncols].rearrange("(c p) n -> p c n", p=128)
                B.dma(st[:, 0:nku, 0:ncols], src, [], [stb])
                s.st[s.nd] = (st, stb)
                s.nd += 1
                continue
            if s.ncs < s.nd and s.units[s.ncs][0] <= i + s.AHEAD:
                si = B.wri % len(B.wring)
                if B.wring_owner[si] is None:
                    bi, W, r, nku, col0, ncols = s.units[s.ncs]
                    B.wri += 1
                    B.wring_owner[si] = 1
                    sl, slb = B.wring[si]
                    st, stb = s.st.pop(s.ncs)
                    B.cp('act', sl[:, 0:nku, 0:ncols], st[:, 0:nku, 0:ncols], [stb], [slb])
                    s.rg[s.ncs] = (sl, slb, nku, si)
                    s.ncs += 1
                    continue
            break

    def get(s, i):
        s.pump(i)
        us = s.bunits[i]
        assert all(u in s.rg for u in us), "weight ring stalled"
        return [s.rg[u] for u in us]

    def done(s, i):
        for u in s.bunits[i]:
            s.B.wring_owner[s.rg.pop(u)[3]] = None


def build(cfg):
    B = Builder(cfg)
    nc = B.nc
    c = cfg
    D, FF, ND, NF, T, TT, NSUB = c.D, c.FF, c.ND, c.NF, c.T, c.TT, c.NSUB
    HA, HB, KVB, G = c.HA, c.HB, c.KVB, c.G
    NQT, NKB, NKT = c.NQT, c.NKB, c.NKT
    T2 = 2 * T

    xT_own = B.din("xT_own", [D, T])
    xT_ctx = B.din("xT_ctx", [D, T])
    cT = B.din("cT", [128, ND])
    w_ada = B.din("w_ada", [D, 9 * D])
    b_adaT = B.din("b_adaT", [128, 9 * ND])
    gains = B.din("gains", [128, 4 * ND])
    f1g = B.din("f1g", [D, FF]); f1u = B.din("f1u", [D, FF]); f1d = B.din("f1d", [FF, D])
    w_in = B.din("w_in", [D, c.INC])
    sinks = B.din("sinks", [128, HB])
    w_bm = B.din("w_bm", [HA * 128, D]); w_bs = B.din("w_bs", [HB * 64, D]); w_out = B.din("w_out", [D, D])
    f2g = B.din("f2g", [D, FF]); f2u = B.din("f2u", [D, FF]); f2d = B.din("f2d", [FF, D])
    ropeA = B.din("ropeA", [2, 128, T2])
    ropeB = B.din("ropeB", [2, 128, T2])
    cmask = B.din("cmask", [128, 4 * 128])
    vbias = B.din("vbias", [128, 2 * NQT * NKB])
    outT = nc.dram_tensor("outT", [D, T], F32, kind="ExternalOutput").ap()

    S_x1 = B.dscr("S_x1", [D, T], F32); S_x1c = B.dscr("S_x1c", [D, T], F32)
    S_x2 = B.dscr("S_x2", [D, T], F32); S_x3 = B.dscr("S_x3", [D, T], F32)
    S_tmp = B.dscr("S_tmp", [D, T], F32)
    QA_d = B.dscr("QA_d", [HA * 128, T], BF16)
    KA_d = B.dscr("KA_d", [HA * 128, T2], BF16)
    VA_d = B.dscr("VA_d", [T2, HA * 128], BF16)
    QB_d = B.dscr("QB_d", [64, NQT, HB, 128], BF16)
    KB_d = B.dscr("KB_d", [64, KVB, T2], BF16)
    VB_d = B.dscr("VB_d", [T2, KVB * 64], BF16)
    SGA_d = B.dscr("SGA_d", [D, T], BF16); SGB_d = B.dscr("SGB_d", [D, T], BF16)
    YA_d = B.dscr("YA_d", [HA * 128, T], BF16); YB_d = B.dscr("YB_d", [HB * 64, T], BF16)
    db = {k: Buf(k) for k in ["x1", "x1c", "x2", "x3", "tmp", "qa", "ka", "va", "qb", "kb", "vb", "sga", "sgb", "ya", "yb", "out"]}

    B.init_psum()
    B.init_wstream(12, 4)
    consts = nc.alloc_sbuf_tensor("consts", [128, 4 * 128], F32); consts_b = Buf()
    ones_bf = nc.alloc_sbuf_tensor("ones_bf", [128, 128], BF16); ones_b = Buf()
    MOD = nc.alloc_sbuf_tensor("MOD", [128, 9 * ND], F32); MOD_b = Buf()
    GN = nc.alloc_sbuf_tensor("GN", [128, 4 * ND], F32); GN_b = Buf()
    AB = nc.alloc_sbuf_tensor("AB", [128, 3, 3, ND], F32); AB_b = Buf()
    VBI = nc.alloc_sbuf_tensor("VBI", [128, 2 * NQT * NKB], F32); VBI_b = Buf()
    ESK = nc.alloc_sbuf_tensor("ESK", [128, HB], F32); ESK_b = Buf()
    EPS = nc.alloc_sbuf_tensor("EPS", [128, 1], F32); EPS_b = Buf()
    B.init_arena(118 * 1024)

    TRI = consts[:, 0:128]; MASKP = consts[:, 128:256]; MASKP0 = consts[:, 256:384]; IDENT = consts[:, 384:512]

    B.dma(consts[:], cmask[:, :], [], [consts_b])
    B.dma(GN[:], gains[:, :], [], [GN_b])
    B.dma(VBI[:], vbias[:, :], [], [VBI_b])
    B.dma(ESK[:], sinks[:, :], [], [ESK_b])
    B.memset('dve', ones_bf[:], 1.0, [ones_b])
    B.memset('dve', EPS[:], 1e-6, [EPS_b])
    B.act(ESK[:], ESK[:], AF.Exp, [ESK_b], [ESK_b])

    B.phase()
    csb, csb_b = B.at([ND], F32)
    cbf = nc.alloc_sbuf_tensor("cbf", [128, ND], BF16); cbf_b = Buf()
    bad = nc.alloc_sbuf_tensor("bad", [128, 9 * ND], F32); bad_b = Buf()
    B.dma(csb, cT[:, :], [], [csb_b])
    B.dma(bad[:], b_adaT[:, :], [], [bad_b])
    B.act(cbf[:], csb, AF.Silu, [csb_b], [cbf_b])
    psm, psm_b = B.psres

    def ada_spec(cb):
        return (w_ada, 0, ND, cb * 256, 256)

    def ada_block(cb, u):
        for cc in range(2):
            j = cb * 2 + cc
            for kc in range(ND):
                l, lb = B.wl(u, kc, cc * 128)
                B.mm(psm[:, j:j + 1], l, cbf[:, kc:kc + 1], kc == 0, kc == ND - 1, [lb, cbf_b], [psm_b])

    def ada_finish(ilist):
        lo, hi = 3 * ilist[0] * ND, 3 * (ilist[-1] + 1) * ND
        B.tt('dve', MOD[:, lo:hi], psm[:, lo:hi], bad[:, lo:hi], ALU.add, [psm_b, bad_b], [MOD_b])
        for i in ilist:
            sh = MOD[:, (3 * i) * ND:(3 * i + 1) * ND]
            sc = MOD[:, (3 * i + 1) * ND:(3 * i + 2) * ND]
            gg = MOD[:, (3 * i + 2) * ND:(3 * i + 3) * ND]
            B.stt(AB[:, i, 0, :], sc, 1.0, GN[:, i * ND:(i + 1) * ND], ALU.add, ALU.mult, [MOD_b, GN_b], [AB_b])
            B.cp('dve', AB[:, i, 1, :], sh, [MOD_b], [AB_b])
            B.ts('dve', AB[:, i, 2, :], gg, (1.0 if i == 1 else 0.5), None, ALU.mult, None, [MOD_b], [AB_b])

    NA = 3 * D // 256
    wp = WPipe(B, [ada_spec(cb) for cb in range(NA)])
    for cb in range(NA):
        u = wp.get(cb)
        ada_block(cb, u)
        wp.done(cb)
    ada_finish([0])

    def norm_apply(*a, **k):
        for _ in norm_gen(*a, **k):
            pass

    def norm_gen(src, src_b, t0, hT, hT_b, Acol, Bcol, xr, sq, rstd, rstd_b, tmpf, out_dram=None, out_b=None, ssb=None):
        if ssb is None:
            ssb = [B.ps() for _ in range(NSUB)]
        NX = len(xr)

        def ldx(dc):
            xt, xb = xr[dc % NX]
            B.ld(xt, src[dc * 128:(dc + 1) * 128, t0:t0 + TT], [src_b], [xb])

        for dc in range(min(NX - 1, ND)):
            ldx(dc)
        for dc in range(ND):
            if dc + NX - 1 < ND:
                ldx(dc + NX - 1)
            xt, xb = xr[dc % NX]
            st, sb_ = sq[dc % len(sq)]
            B.act(st, xt, AF.Square, [xb], [sb_])
            for su in range(NSUB):
                B.mm(ssb[su][0][:, :], ones_bf[:], st[:, su * 512:(su + 1) * 512], dc == 0, dc == ND - 1,
                     [ones_b, sb_], [ssb[su][1]])
            yield
        for dc in range(min(NX - 1, ND)):
            ldx(dc)
        for su in range(NSUB):
            r = rstd[:, su * 512:(su + 1) * 512]
            B.act(r, ssb[su][0][:, :], AF.Sqrt, [ssb[su][1], EPS_b], [rstd_b], bias=EPS[:, 0:1], scale=1.0 / D)
            B.R.add('dve', lambda e, r=r: e.reciprocal(r, r), [rstd_b], [rstd_b])
        for dc in range(ND):
            if dc + NX - 1 < ND:
                ldx(dc + NX - 1)
            xt, xb = xr[dc % NX]
            for su in range(NSUB):
                tf, tfb = tmpf[(dc * NSUB + su) % len(tmpf)]
                sl = slice(su * 512, (su + 1) * 512)
                B.tt('dve', tf, xt[:, sl], rstd[:, sl], ALU.mult, [xb, rstd_b], [tfb])
                if out_dram is None:
                    B.act(hT[:, dc, sl], tf, AF.Identity, [tfb, AB_b], [hT_b[dc]],
                          bias=Bcol[:, dc:dc + 1], scale=Acol[:, dc:dc + 1])
                else:
                    B.act(tf, tf, AF.Identity, [tfb, GN_b], [tfb], scale=Acol[:, dc:dc + 1])
                    B.sta(out_dram[dc * 128:(dc + 1) * 128, t0 + su * 512:t0 + (su + 1) * 512], tf, [tfb], [out_b])
            yield

    def ffn_phase(tiles, ph, Wg, Wu, Wd, extra=()):
        B.phase()
        ntiles = len(tiles)
        hT, _ = B.at([ND, TT], BF16)
        hT_b = [Buf() for _ in range(ND)]
        NFH = NF // c.FSPLIT
        aT, _ = B.at([NFH, TT], BF16)
        aT_b = [Buf() for _ in range(NFH)]
        xr = [B.at([TT], F32) for _ in range(4)]
        sq = [B.at([TT], BF16) for _ in range(2)]
        rstd, rstd_b = B.at([TT], F32)
        tmpf = [B.at([512], F32) for _ in range(4)]
        xo = [B.at([512], F32) for _ in range(4)]
        Acol, Bcol, Gcol = AB[:, ph, 0, :], AB[:, ph, 1, :], AB[:, ph, 2, :]
        specs, gu_i, dn_i, ex_i = [], {}, {}, {}
        extra = list(extra)
        npairs = ntiles * c.FSPLIT * (NFH // 2)
        pk_ = 0
        for ti in range(ntiles):
            for fs in range(c.FSPLIT):
                for fb in range(NFH // 2):
                    col0 = (fs * NFH + fb * 2) * 128
                    gu_i[(ti, fs, fb)] = len(specs)
                    specs.append((Wg, 0, ND, col0, 256))
                    specs.append((Wu, 0, ND, col0, 256))
                    ex_i[(ti, fs, fb)] = []
                    for e_ in extra[(len(extra) * pk_) // npairs:(len(extra) * (pk_ + 1)) // npairs]:
                        ex_i[(ti, fs, fb)].append((len(specs), e_))
                        specs.append(ada_spec(e_))
                    pk_ += 1
                for db_ in range(ND // 2):
                    dn_i[(ti, fs, db_)] = len(specs)
                    specs.append((Wd, fs * NFH, NFH, db_ * 256, 256))
        wp = WPipe(B, specs)
        wp.pump(0)
        hide = NSUB <= 2
        third = None
        if extra:
            third = B.psb.pop()
            res_ssb = [B.psres2, third][0:NSUB]
        else:
            res_ssb = [B.psres, B.psres2][0:NSUB]
        gen = None
        for ti in range(ntiles):
            src, src_b, dst, dst_b, t0 = tiles[ti]
            if gen is None:
                norm_apply(src, src_b, t0, hT, hT_b, Acol, Bcol, xr, sq, rstd, rstd_b, tmpf, ssb=res_ssb)
            else:
                for _ in gen:
                    pass
                gen = None
            for fs in range(c.FSPLIT):
                for fb in range(NFH // 2):
                    bi = gu_i[(ti, fs, fb)]
                    ug = wp.get(bi)
                    uu = wp.get(bi + 1)
                    for cc in range(2):
                        fl = fb * 2 + cc
                        for su in range(NSUB):
                            sl = slice(su * 512, (su + 1) * 512)
                            pg, pgb = B.ps()
                            pu, pub = B.ps()
                            for kc in range(ND):
                                l, lb = B.wl(ug, kc, cc * 128)
                                B.mm(pg[:, :], l, hT[:, kc, sl], kc == 0, kc == ND - 1, [lb, hT_b[kc]], [pgb])
                            for kc in range(ND):
                                l, lb = B.wl(uu, kc, cc * 128)
                                B.mm(pu[:, :], l, hT[:, kc, sl], kc == 0, kc == ND - 1, [lb, hT_b[kc]], [pub])
                            tf, tfb = tmpf[(fl * NSUB + su) % len(tmpf)]
                            B.act(tf, pg[:, :], AF.Silu, [pgb], [tfb])
                            B.tt('dve', aT[:, fl, sl], tf, pu[:, :], ALU.mult, [tfb, pub], [aT_b[fl]])
                    wp.done(bi)
                    wp.done(bi + 1)
                    for (xi, e_) in ex_i[(ti, fs, fb)]:
                        ada_block(e_, wp.get(xi))
                        wp.done(xi)
                xin, xin_b = (src, src_b) if fs == 0 else (S_tmp, db["tmp"])
                xout, xout_b = (dst, dst_b) if fs == c.FSPLIT - 1 else (S_tmp, db["tmp"])
                if hide and fs == c.FSPLIT - 1 and ti + 1 < ntiles:
                    nsrc, nsrc_b, _, _, nt0 = tiles[ti + 1]
                    gen = norm_gen(nsrc, nsrc_b, nt0, hT, hT_b, Acol, Bcol, xr, sq, rstd, rstd_b, tmpf, ssb=res_ssb)
                nstep = -(-(2 * ND) // (ND // 2))
                for db_ in range(ND // 2):
                    bi = dn_i[(ti, fs, db_)]
                    u = wp.get(bi)
                    for cc in range(2):
                        dc = db_ * 2 + cc
                        for su in range(NSUB):
                            sl = slice(t0 + su * 512, t0 + (su + 1) * 512)
                            xt, xtb = xo[(dc * NSUB + su) % len(xo)]
                            B.ld(xt, xin[dc * 128:(dc + 1) * 128, sl], [xin_b], [xtb])
                            pd, pdb = B.ps()
                            for kc in range(NFH):
                                l, lb = B.wl(u, kc, cc * 128)
                                B.mm(pd[:, :], l, aT[:, kc, su * 512:(su + 1) * 512], kc == 0, kc == NFH - 1,
                                     [lb, aT_b[kc]], [pdb])
                            B.stt(xt, pd[:, :], Gcol[:, dc:dc + 1], xt, ALU.mult, ALU.add, [pdb, xtb, AB_b], [xtb])
                            B.dma(xout[dc * 128:(dc + 1) * 128, sl], xt, [xtb], [xout_b])
                    wp.done(bi)
                    if gen is not None and fs == c.FSPLIT - 1:
                        for _ in range(nstep):
                            if next(gen, 'end') == 'end':
                                break
        if third is not None:
            B.psb.append(third)

    xcb, xob = Buf(), Buf()
    ffn_phase([(xT_ctx, xcb, S_x1c, db["x1c"], t * TT) for t in range(T // TT)]
              + [(xT_own, xob, S_x1, db["x1"], t * TT) for t in range(T // TT)],
              0, f1g, f1u, f1d, extra=range(NA, 9 * D // 256))
    ada_finish([1, 2])

    def proj_phase(src, src_b, is_ctx):
        B.phase()
        hT, _ = B.at([ND, TT], BF16)
        hT_b = [Buf() for _ in range(ND)]
        xr = [B.at([TT], F32) for _ in range(4)]
        sq = [B.at([TT], BF16) for _ in range(2)]
        rstd, rstd_b = B.at([TT], F32)
        tmpf = [B.at([512], F32) for _ in range(4)]
        rp = [B.at([2, 512], F32) for _ in range(4)]
        xs = [B.at([512], F32) for _ in range(3)]
        ra = [B.at([512], F32) for _ in range(3)]
        rb = [B.at([512], F32) for _ in range(3)]
        ev = [B.at([512], BF16) for _ in range(8)]
        evi = [0]
        Acol, Bcol = AB[:, 1, 0, :], AB[:, 1, 1, :]
        loc0 = 0 if is_ctx else T
        for ti in range(T // TT):
            t0 = ti * TT
            blocks = []
            if not is_ctx:
                for i in range(HA // 2):
                    blocks.append(("qa", c.oqa + i * 256, 256, i * 2))
            for i in range(HA // 2):
                blocks.append(("ka", c.oka + i * 256, 256, i * 2))
            for i in range(HA // 2):
                blocks.append(("va", c.ova + i * 256, 256, i * 2))
            if not is_ctx:
                for i in range(HB * 64 // 256):
                    blocks.append(("qb", c.oqb + i * 256, 256, i * 2))
            blocks.append(("kb", c.okb, 128, 0))
            blocks.append(("vb", c.ovb, 128, 0))
            if not is_ctx:
                for i in range(ND // 2):
                    blocks.append(("ga", c.oga + i * 256, 256, i * 2))
                for i in range(ND // 2):
                    blocks.append(("gb", c.ogb + i * 256, 256, i * 2))
            wp = WPipe(B, [(w_in, 0, ND, b[1], b[2]) for b in blocks])
            wp.pump(0)
            norm_apply(src, src_b, t0, hT, hT_b, Acol, Bcol, xr, sq, rstd, rstd_b, tmpf)
            ropeslices = {}
            for su in range(NSUB):
                for which, tab in ((0, ropeA), (1, ropeB)):
                    r_, rb_ = rp[su * 2 + which] if NSUB * 2 <= len(rp) else rp[which]
                    lo = loc0 + t0 + su * 512
                    B.ld(r_, tab[:, :, lo:lo + 512].rearrange("a p n -> p a n"), [], [rb_])
                    ropeslices[(su, which)] = (r_, rb_)
            for bi, (kind, col0, ncols, ch0) in enumerate(blocks):
                u = wp.get(bi)
                if kind in ("va", "vb"):
                    for tk in range(TT // 128):
                        pv, pvb = B.ps()
                        for kc in range(ND):
                            r_, rb_ = B.wl(u, kc, 0, ncols)
                            B.mm(pv[:, 0:ncols], hT[:, kc, tk * 128:(tk + 1) * 128], r_, kc == 0, kc == ND - 1,
                                 [rb_, hT_b[kc]], [pvb])
                        e, eb = ev[evi[0] % len(ev)]
                        evi[0] += 1
                        B.cp('act', e[:, 0:ncols], pv[:, 0:ncols], [pvb], [eb])
                        tok = loc0 + t0 + tk * 128
                        if kind == "va":
                            B.sta(VA_d[tok:tok + 128, col0 - c.ova:col0 - c.ova + ncols], e[:, 0:ncols], [eb], [db["va"]])
                        else:
                            B.sta(VB_d[tok:tok + 128, 0:ncols], e[:, 0:ncols], [eb], [db["vb"]])
                    wp.done(bi)
                    continue
                for cc in range(ncols // 128):
                    ch = ch0 + cc
                    for su in range(NSUB):
                        sl = slice(su * 512, (su + 1) * 512)
                        pp, ppb = B.ps()
                        for kc in range(ND):
                            l, lb = B.wl(u, kc, cc * 128)
                            B.mm(pp[:, :], l, hT[:, kc, sl], kc == 0, kc == ND - 1, [lb, hT_b[kc]], [ppb])
                        tok = t0 + su * 512
                        if kind in ("ga", "gb"):
                            e, eb = ev[evi[0] % len(ev)]
                            evi[0] += 1
                            B.act(e, pp[:, :], AF.Sigmoid, [ppb], [eb])
                            dd, ddb = (SGA_d, db["sga"]) if kind == "ga" else (SGB_d, db["sgb"])
                            B.sta(dd[ch * 128:(ch + 1) * 128, tok:tok + 512], e, [eb], [ddb])
                            continue
                        isA = kind in ("qa", "ka")
                        rt, rtb = ropeslices[(su, 0 if isA else 1)]
                        x_, xb_ = xs[evi[0] % 3]
                        a_, ab_ = ra[evi[0] % 3]
                        b_, bb_ = rb[evi[0] % 3]
                        B.cp('act', x_, pp[:, :], [ppb], [xb_])
                        B.tt('pool', a_, x_, rt[:, 0, :], ALU.mult, [xb_, rtb], [ab_])
                        hs = 64 if isA else 32
                        for g0 in range(0, 128, 2 * hs):
                            B.tt('pool', b_[g0:g0 + hs, :], x_[g0 + hs:g0 + 2 * hs, :], rt[g0 + hs:g0 + 2 * hs, 1, :],
                                 ALU.mult, [xb_, rtb], [bb_])
                            B.tt('pool', b_[g0 + hs:g0 + 2 * hs, :], x_[g0:g0 + hs, :], rt[g0:g0 + hs, 1, :],
                                 ALU.mult, [xb_, rtb], [bb_])
                        e, eb = ev[evi[0] % len(ev)]
                        evi[0] += 1
                        if isA:
                            B.tt('dve', e, a_, b_, ALU.add, [ab_, bb_], [eb])
                            if kind == "qa":
                                B.dma(QA_d[ch * 128:(ch + 1) * 128, tok:tok + 512], e, [eb], [db["qa"]])
                            else:
                                B.dma(KA_d[ch * 128:(ch + 1) * 128, loc0 + tok:loc0 + tok + 512], e, [eb], [db["ka"]])
                        else:
                            B.tt('dve', e[0:64, 0:512], a_[0:64, :], b_[0:64, :], ALU.add, [ab_, bb_], [eb])
                            e2, eb2 = ev[evi[0] % len(ev)]
                            evi[0] += 1
                            B.tt('dve', e2[0:64, 0:512], a_[64:128, :], b_[64:128, :], ALU.add, [ab_, bb_], [eb2])
                            for hh, (ee, eeb) in enumerate(((e, eb), (e2, eb2))):
                                if kind == "qb":
                                    head = ch * 2 + hh
                                    B.dma(QB_d[:, tok // 128:tok // 128 + 4, head, :],
                                          ee[0:64, 0:512].rearrange("p (a b) -> p a b", a=4), [eeb], [db["qb"]])
                                else:
                                    B.dma(KB_d[:, hh, loc0 + tok:loc0 + tok + 512], ee[0:64, 0:512], [eeb], [db["kb"]])
                wp.done(bi)

    proj_phase(S_x1c, db["x1c"], True)
    proj_phase(S_x1, db["x1"], False)

    B.phase()
    scaleA = 128.0 ** -0.5
    KT = [B.at([T2], BF16) for _ in range(2)]
    V1 = [B.at([NKT, 132], BF16) for _ in range(2)]
    QT = [B.at([T], BF16) for _ in range(2)]
    kmf = [B.at([NKB], F32) for _ in range(2)]
    kmT = [B.at([NKB], BF16) for _ in range(2)]
    gsm = [B.at([NKB], F32) for _ in range(2)]
    top8 = [B.at([8], F32) for _ in range(2)]
    SELs = [B.at([NQT, NKB], F32) for _ in range(2)]
    Pt = [B.at([256], BF16) for _ in range(8)]
    acc = [B.at([132], F32) for _ in range(4)]
    rec = [B.at([1], F32) for _ in range(2)]
    yt = [B.at([128], F32) for _ in range(2)]
    ytT = [B.at([256], BF16) for _ in range(2)]
    for vv, vb_ in V1:
        B.memset('pool', vv[:, :, 128:129], 1.0, [vb_])
    pti = [0]
    VBb = VBI[:, 0:NQT * NKB].rearrange("p (a b) -> p a b", a=NQT)
    VBv = VBI[:, NQT * NKB:2 * NQT * NKB].rearrange("p (a b) -> p a b", a=NQT)

    def load_head(h):
        kt, ktb = KT[h % 2]
        v1, v1b = V1[h % 2]
        qt, qtb = QT[h % 2]
        B.ld(kt, KA_d[h * 128:(h + 1) * 128, :], [db["ka"]], [ktb])
        B.ld(v1[:, :, 0:128], VA_d[:, h * 128:(h + 1) * 128].rearrange("(n p) d -> p n d", p=128), [db["va"]], [v1b])
        B.ld(qt, QA_d[h * 128:(h + 1) * 128, :], [db["qa"]], [qtb])

    def gate_steps(h):
        kt, ktb = KT[h % 2]
        qt, qtb = QT[h % 2]
        kf, kfb = kmf[h % 2]
        km, kmb = kmT[h % 2]
        SEL, SEL_b = SELs[h % 2]
        steps = []

        def s0():
            B.R.add('dve', lambda e: e.tensor_reduce(kf, kt.rearrange("p (n k) -> p n k", k=256), AX.X, ALU.add),
                    [ktb], [kfb])
            B.ts('dve', km, kf, 1.0 / 256, None, ALU.mult, None, [kfb], [kmb])
        steps.append(s0)
        for q in range(NQT):
            def sq_(q=q):
                pg, pgb = B.ps()
                B.mm(pg[:, 0:NKB], qt[:, q * 128:(q + 1) * 128], km, True, True, [qtb, kmb], [pgb])
                g_, gb_ = gsm[q % 2]
                t8, t8b = top8[q % 2]
                B.tt('dve', g_, pg[:, 0:NKB], VBb[:, q, :], ALU.add, [pgb, VBI_b], [gb_])
                B.R.add('dve', lambda e: e.max(t8, g_), [gb_], [t8b])
                B.ts('dve', g_, g_, t8[:, c.TOPK - 1:c.TOPK], None, ALU.is_ge, None, [gb_, t8b], [gb_])
                B.tt('dve', SEL[:, q, :], g_, VBv[:, q, :], ALU.mult, [gb_, VBI_b], [SEL_b])
            steps.append(sq_)
        return steps

    def moba_items(h):
        kt, ktb = KT[h % 2]
        v1, v1b = V1[h % 2]
        qt, qtb = QT[h % 2]
        SEL, SEL_b = SELs[h % 2]
        items = []
        for j in range(T // 256):
            nb = T // 256 + j
            q0 = j * 256
            accs = (acc[(2 * j) % 4], acc[(2 * j + 1) % 4])
            k0 = 2 * nb

            def A_own(q0=q0, k0=k0):
                s0, s0b = B.ps()
                B.mm(s0[:, 0:256], kt[:, k0 * 128:(k0 + 1) * 128], qt[:, q0:q0 + 256], True, True, [ktb, qtb], [s0b])
                B.mm(s0[:, 256:384], kt[:, (k0 + 1) * 128:(k0 + 2) * 128], qt[:, q0 + 128:q0 + 256], True, True, [ktb, qtb], [s0b])
                p0, p0b = Pt[pti[0] % 8]; pti[0] += 1
                p1, p1b = Pt[pti[0] % 8]; pti[0] += 1
                B.act(p0, s0[:, 0:256], AF.Exp, [s0b], [p0b], scale=scaleA)
                B.act(p1[:, 0:128], s0[:, 256:384], AF.Exp, [s0b], [p1b], scale=scaleA)
                B.tt('pool', p0[:, 0:128], p0[:, 0:128], TRI, ALU.mult, [p0b, consts_b], [p0b])
                B.tt('pool', p1[:, 0:128], p1[:, 0:128], TRI, ALU.mult, [p1b, consts_b], [p1b])
                return (p0, p0b, p1, p1b)

            def B_own(st, k0=k0, accs=accs):
                p0, p0b, p1, p1b = st
                oo, oob = B.ps()
                B.mm(oo[:, 0:129], p0[:, 0:128], v1[:, k0, 0:129], True, True, [p0b, v1b], [oob])
                B.mm(oo[:, 256:385], p0[:, 128:256], v1[:, k0, 0:129], True, False, [p0b, v1b], [oob])
                B.mm(oo[:, 256:385], p1[:, 0:128], v1[:, k0 + 1, 0:129], False, True, [p1b, v1b], [oob])
                B.cp('dve', accs[0][0][:, 0:129], oo[:, 0:129], [oob], [accs[0][1]])
                B.cp('dve', accs[1][0][:, 0:129], oo[:, 256:385], [oob], [accs[1][1]])
            items.append((A_own, B_own))
            for n in range(nb):
                def A_p(n=n, q0=q0):
                    sp_, spb = B.ps()
                    pk = []
                    for kk in range(2):
                        ktile = 2 * n + kk
                        B.mm(sp_[:, kk * 256:(kk + 1) * 256], kt[:, ktile * 128:(ktile + 1) * 128], qt[:, q0:q0 + 256], True, True,
                             [ktb, qtb], [spb])
                    for kk in range(2):
                        pp_, ppb = Pt[pti[0] % 8]; pti[0] += 1
                        B.act(pp_, sp_[:, kk * 256:(kk + 1) * 256], AF.Exp, [spb], [ppb], scale=scaleA)
                        pk.append((pp_, ppb, 2 * n + kk))
                    return pk

                def B_p(pk, n=n, j=j, q0=q0, accs=accs, last=(n == nb - 1)):
                    oo, oob = B.ps()
                    for qs in range(2):
                        for kk in range(2):
                            pp_, ppb, ktile = pk[kk]
                            B.mm(oo[:, qs * 256:qs * 256 + 129], pp_[:, qs * 128:(qs + 1) * 128], v1[:, ktile, 0:129],
                                 kk == 0, kk == 1, [ppb, v1b], [oob])
                    for qs in range(2):
                        aa, aab = accs[qs]
                        B.stt(aa[:, 0:129], oo[:, qs * 256:qs * 256 + 129], SEL[:, 2 * j + qs, n:n + 1], aa[:, 0:129],
                              ALU.mult, ALU.add, [oob, SEL_b, aab], [aab])
                    if last:
                        yT, yTb = ytT[j % 2]
                        for qs in range(2):
                            aa, aab = accs[qs]
                            r_, rb_ = rec[qs]
                            y_, yb_ = yt[qs]
                            B.R.add('dve', lambda e, r_=r_, aa=aa: e.reciprocal(r_, aa[:, 128:129]), [aab], [rb_])
                            B.ts('dve', y_, aa[:, 0:128], r_[:, 0:1], None, ALU.mult, None, [aab, rb_], [yb_])
                            pt_, ptb = B.ps()
                            B.tr(pt_[:, 0:128], y_, IDENT, [yb_, consts_b], [ptb])
                            B.cp('act', yT[:, qs * 128:(qs + 1) * 128], pt_[:, 0:128], [ptb], [yTb])
                        B.sta(YA_d[h * 128:(h + 1) * 128, q0:q0 + 256], yT, [yTb], [db["ya"]])
                items.append((A_p, B_p))
        return items

    load_head(0)
    for st_ in gate_steps(0):
        st_()
    for h in range(HA):
        gs_next = []
        if h + 1 < HA:
            load_head(h + 1)
            gs_next = gate_steps(h + 1)
        items = moba_items(h)
        every = max(1, len(items) // (len(gs_next) + 1)) if gs_next else 0
        st = items[0][0]()
        for k in range(len(items)):
            nst = items[k + 1][0]() if k + 1 < len(items) else None
            items[k][1](st)
            st = nst
            if gs_next and k % every == every - 1 and k > len(items) // 8:
                gs_next.pop(0)()
        while gs_next:
            gs_next.pop(0)()

    B.phase()
    scaleB = 64.0 ** -0.5
    KBT, KBT_b = B.at([KVB, T2], BF16)
    VB1, VB1_b = B.at([NKT, KVB, 66], BF16)
    QBT = [B.at([HB, 128], BF16) for _ in range(2)]
    Pb = [B.at([512], BF16) for _ in range(6)]
    den = [B.at([4], F32) for _ in range(2)]
    ybt = [B.at([HB * 64], F32) for _ in range(2)]
    ybT = [B.at([HB * 64 // 128, 128], BF16) for _ in range(2)]
    B.ld(KBT[0:64], KB_d[:, :, :], [db["kb"]], [KBT_b])
    B.memset('pool', VB1[:, :, :, 64:65], 1.0, [VB1_b])
    for g in range(KVB):
        B.ld(VB1[:, :, g, 0:64], VB_d[:, g * 64:(g + 1) * 64].rearrange("(n p) d -> p n d", p=128), [db["vb"]], [VB1_b])
    HG = min(4, G)
    pbi = [0]
    B.ld(QBT[0][0][0:64], QB_d[:, 0, :, :], [db["qb"]], [QBT[0][1]])
    sitems = []
    for i in range(NQT):
        cur = T // 128 + i
        prv = cur - 1
        for g in range(KVB):
            for hg in range(G // HG):
                first = (g == 0 and hg == 0)
                lastit = (g == KVB - 1 and hg == G // HG - 1)

                def A_s(i=i, g=g, hg=hg, cur=cur, prv=prv, first=first):
                    qb_, qbb = QBT[i % 2]
                    if first and i + 1 < NQT:
                        B.ld(QBT[(i + 1) % 2][0][0:64], QB_d[:, i + 1, :, :], [db["qb"]], [QBT[(i + 1) % 2][1]])
                    h0 = g * G + hg * HG
                    rhs = qb_[0:64, h0:h0 + HG, :]
                    ps_ = []
                    for (ktile, msk) in ((prv, MASKP0 if i == 0 else MASKP), (cur, TRI)):
                        sp_, spb = B.ps()
                        B.mm(sp_[:, 0:HG * 128], KBT[0:64, g, ktile * 128:(ktile + 1) * 128], rhs, True, True, [KBT_b, qbb], [spb])
                        pp_, ppb = Pb[pbi[0] % 6]; pbi[0] += 1
                        B.act(pp_[:, 0:HG * 128], sp_[:, 0:HG * 128], AF.Exp, [spb], [ppb], scale=scaleB)
                        pv = pp_[:, 0:HG * 128].rearrange("p (h q) -> p h q", h=HG)
                        B.tt('pool', pv, pv, msk.unsqueeze(1).to_broadcast([128, HG, 128]), ALU.mult, [ppb, consts_b], [ppb])
                        ps_.append((pp_, ppb, ktile))
                    return ps_

                def B_s(ps_, i=i, g=g, hg=hg, lastit=lastit):
                    y_, yb_ = ybt[i % 2]
                    h0 = g * G + hg * HG
                    oo, oob = B.ps()
                    for hh in range(HG):
                        for kk in range(2):
                            pp_, ppb, ktile = ps_[kk]
                            B.mm(oo[:, hh * 65:hh * 65 + 65], pp_[:, hh * 128:(hh + 1) * 128], VB1[:, ktile, g, 0:65],
                                 kk == 0, kk == 1, [ppb, VB1_b], [oob])
                    d_, db_ = den[(g * (G // HG) + hg) % 2]
                    ov = oo[:, 0:HG * 65].rearrange("p (h d) -> p h d", h=HG)
                    B.tt('dve', d_[:, 0:HG], ov[:, :, 64], ESK[:, h0:h0 + HG], ALU.add, [oob, ESK_b], [db_])
                    B.R.add('dve', lambda e, d_=d_: e.reciprocal(d_[:, 0:HG], d_[:, 0:HG]), [db_], [db_])
                    yv = y_[:, h0 * 64:(h0 + HG) * 64].rearrange("p (h d) -> p h d", h=HG)
                    B.tt('dve', yv, ov[:, :, 0:64], d_[:, 0:HG].unsqueeze(2).to_broadcast([128, HG, 64]), ALU.mult,
                         [oob, db_], [yb_])
                    if lastit:
                        yT, yTb = ybT[i % 2]
                        for ch in range(HB * 64 // 128):
                            pt_, ptb = B.ps()
                            B.tr(pt_[:, 0:128], y_[:, ch * 128:(ch + 1) * 128], IDENT, [yb_, consts_b], [ptb])
                            B.cp('act', yT[:, ch, :], pt_[:, 0:128], [ptb], [yTb])
                        B.sta(YB_d[:, i * 128:(i + 1) * 128].rearrange("(c p) n -> p c n", p=128), yT, [yTb], [db["yb"]])
                sitems.append((A_s, B_s))
    st = sitems[0][0]()
    for k in range(len(sitems)):
        nst = sitems[k + 1][0]() if k + 1 < len(sitems) else None
        sitems[k][1](st)
        st = nst

    B.phase()
    NCA, NCB = HA, HB * 64 // 128
    yaT, yaT_b = B.at([NCA, TT], BF16)
    ybT2, ybT2_b = B.at([NCB, TT], BF16)
    mT, _ = B.at([ND, TT], BF16)
    mT_b = [Buf() for _ in range(ND)]
    sg = [B.at([2, 512], BF16) for _ in range(3)]
    tm = [B.at([2, 512], F32) for _ in range(3)]
    xo = [B.at([512], F32) for _ in range(4)]
    Gcol = AB[:, 1, 2, :]
    k_i = [0]
    for ti in range(T // TT):
        t0 = ti * TT
        B.ld(yaT, YA_d[:, t0:t0 + TT].rearrange("(c p) n -> p c n", p=128), [db["ya"]], [yaT_b])
        B.ld(ybT2, YB_d[:, t0:t0 + TT].rearrange("(c p) n -> p c n", p=128), [db["yb"]], [ybT2_b])
        specs = []
        for d2 in range(ND // 2):
            specs.append((w_bm, 0, NCA, d2 * 256, 256))
            specs.append((w_bs, 0, NCB, d2 * 256, 256))
        wp = WPipe(B, specs)
        for d2 in range(ND // 2):
            um = wp.get(2 * d2)
            us = wp.get(2 * d2 + 1)
            for cc in range(2):
                dc = d2 * 2 + cc
                for su in range(NSUB):
                    sl = slice(su * 512, (su + 1) * 512)
                    tok = t0 + su * 512
                    s_, sb_ = sg[k_i[0] % 3]
                    t_, tb_ = tm[k_i[0] % 3]
                    k_i[0] += 1
                    B.ld(s_[:, 0, :], SGA_d[dc * 128:(dc + 1) * 128, tok:tok + 512], [db["sga"]], [sb_])
                    B.ld(s_[:, 1, :], SGB_d[dc * 128:(dc + 1) * 128, tok:tok + 512], [db["sgb"]], [sb_])
                    pm, pmb = B.ps()
                    pq, pqb = B.ps()
                    for kc in range(NCA):
                        l, lb = B.wl(um, kc, cc * 128)
                        B.mm(pm[:, :], l, yaT[:, kc, sl], kc == 0, kc == NCA - 1, [lb, yaT_b], [pmb])
                    for kc in range(NCB):
                        l, lb = B.wl(us, kc, cc * 128)
                        B.mm(pq[:, :], l, ybT2[:, kc, sl], kc == 0, kc == NCB - 1, [lb, ybT2_b], [pqb])
                    B.tt('dve', t_[:, 0, :], pm[:, :], s_[:, 0, :], ALU.mult, [pmb, sb_], [tb_])
                    B.tt('dve', t_[:, 1, :], pq[:, :], s_[:, 1, :], ALU.mult, [pqb, sb_], [tb_])
                    B.tt('pool', mT[:, dc, sl], t_[:, 0, :], t_[:, 1, :], ALU.add, [tb_], [mT_b[dc]])
            wp.done(2 * d2)
            wp.done(2 * d2 + 1)
        specs = [(w_out, 0, ND, d2 * 256, 256) for d2 in range(ND // 2)]
        wp = WPipe(B, specs)
        for d2 in range(ND // 2):
            u = wp.get(d2)
            for cc in range(2):
                dc = d2 * 2 + cc
                for su in range(NSUB):
                    sl = slice(t0 + su * 512, t0 + (su + 1) * 512)
                    xt, xtb = xo[(dc * NSUB + su) % len(xo)]
                    B.ld(xt, S_x1[dc * 128:(dc + 1) * 128, sl], [db["x1"]], [xtb])
                    pd, pdb = B.ps()
                    for kc in range(ND):
                        l, lb = B.wl(u, kc, cc * 128)
                        B.mm(pd[:, :], l, mT[:, kc, su * 512:(su + 1) * 512], kc == 0, kc == ND - 1, [lb, mT_b[kc]], [pdb])
                    B.stt(xt, pd[:, :], Gcol[:, dc:dc + 1], xt, ALU.mult, ALU.add, [pdb, xtb, AB_b], [xtb])
                    B.dma(S_x2[dc * 128:(dc + 1) * 128, sl], xt, [xtb], [db["x2"]])
            wp.done(d2)

    ffn_phase([(S_x2, db["x2"], S_x3, db["x3"], t * TT) for t in range(T // TT)], 2, f2g, f2u, f2d)
    B.phase()
    xr = [B.at([TT], F32) for _ in range(4)]
    sq = [B.at([TT], BF16) for _ in range(2)]
    rstd, rstd_b = B.at([TT], F32)
    tmpf = [B.at([512], F32) for _ in range(4)]
    for ti in range(T // TT):
        norm_apply(S_x3, db["x3"], ti * TT, None, None, GN[:, 3 * ND:4 * ND], None, xr, sq, rstd, rstd_b, tmpf,
                   out_dram=outT, out_b=db["out"])
    B.R.finish(nc)
    return nc


def rope_tables(cfg, half):
    T = cfg.T
    pos_ctx = np.arange(0, T, dtype=np.float64)
    pos_own = np.arange(half * T, (half + 1) * T, dtype=np.float64)
    pos = np.concatenate([pos_ctx, pos_own])
    out = []
    for hs in (64, 32):
        inv = 10000.0 ** (-np.arange(hs, dtype=np.float64) / hs)
        ang = (pos.astype(np.float32)[None, :] * inv.astype(np.float32)[:, None]).astype(np.float32)
        cos = np.cos(ang).astype(np.float32)
        sin = np.sin(ang).astype(np.float32)
        reps = 128 // (2 * hs)
        cosT = np.concatenate([cos, cos] * reps, axis=0)
        sinT = np.concatenate([sin, -sin] * reps, axis=0)
        out.append(np.stack([cosT, sinT]).astype(np.float32))
    return out


def const_masks(cfg, half):
    k = np.arange(128)[:, None]
    q = np.arange(128)[None, :]
    tri = (k <= q).astype(np.float32)
    mp = (k > q).astype(np.float32)
    mp0 = mp * float(half)
    ident = np.eye(128, dtype=np.float32)
    cm = np.concatenate([tri, mp, mp0, ident], axis=1)
    NQT, NKB = cfg.NQT, cfg.NKB
    valid = np.zeros((NQT, NKB), np.float32)
    for qt in range(NQT):
        j = qt // 2
        for n in range(NKB):
            if n < NKB // 2:
                valid[qt, n] = float(half)
            elif n < NKB // 2 + j:
                valid[qt, n] = 1.0
    bias = np.where(valid > 0, 0.0, NEG).astype(np.float32)
    vb = np.concatenate([bias.reshape(-1), valid.reshape(-1)])[None, :].repeat(128, 0).astype(np.float32)
    return cm, vb


def colT(v, n):
    return np.ascontiguousarray(np.asarray(v, np.float32).reshape(n, 128).T)


def make_in_maps(cfg, inp):
    D, ND, T = cfg.D, cfg.ND, cfg.T
    f = lambda a: np.ascontiguousarray(np.asarray(a, np.float32))
    x = np.asarray(inp["x"], np.float32)
    shared = {
        "w_ada": f(inp["w_ada"][0]), "b_adaT": colT(inp["b_ada"][0], 9 * ND),
        "gains": np.concatenate([colT(inp["norm_ffn1"][0], ND), colT(inp["norm_mix"][0], ND),
                                 colT(inp["norm_ffn2"][0], ND), colT(inp["norm_final"], ND)], axis=1),
        "f1g": f(inp["ffn1_gate"][0]), "f1u": f(inp["ffn1_up"][0]), "f1d": f(inp["ffn1_down"][0]),
        "w_in": f(inp["w_in"][0]),
        "sinks": np.ascontiguousarray(np.broadcast_to(np.asarray(inp["swa_sinks"][0], np.float32)[None, :], (128, cfg.HB))),
        "w_bm": f(inp["w_branch_moba"][0]), "w_bs": f(inp["w_branch_swa"][0]), "w_out": f(inp["w_out"][0]),
        "f2g": f(inp["ffn2_gate"][0]), "f2u": f(inp["ffn2_up"][0]), "f2d": f(inp["ffn2_down"][0]),
    }
    maps = []
    for core in range(2 * cfg.BATCH):
        b, half = core // 2, core % 2
        rA, rB = rope_tables(cfg, half)
        cm, vb = const_masks(cfg, half)
        m = dict(shared)
        m["xT_own"] = np.ascontiguousarray(x[b, half * T:(half + 1) * T, :].T)
        m["xT_ctx"] = np.ascontiguousarray(x[b, 0:T, :].T)
        m["cT"] = colT(inp["c"][b], ND)
        m["ropeA"], m["ropeB"], m["cmask"], m["vbias"] = rA, rB, cm, vb
        maps.append(m)
    return maps


def assemble(cfg, results):
    out = np.empty((cfg.BATCH, cfg.SEQ, cfg.D), np.float32)
    for core, r in enumerate(results):
        b, half = core // 2, core % 2
        out[b, half * cfg.T:(half + 1) * cfg.T, :] = np.asarray(r["outT"], np.float32).T
    return out


def kernel(**inputs):
    cfg = Cfg()
    nc = build(cfg)
    maps = make_in_maps(cfg, inputs)
    res = run_bass_kernel_spmd(nc, maps, core_ids=list(range(8)))
    return assemble(cfg, res.results)
```
